# Optimizing a Trainium2 kernel written in Bass

```python
import math
import jax
import jax.numpy as jnp
from jax import lax
import numpy as np

D_MODEL = 1024
BATCH = 2
SEQ = 16384
DEPTH = 2

GRID_W = 64
CTX_LEN = 256
Q_BLOCK = 128
HEAD_DIM = 64
ROPE_THETA = 10000.0
A_HEADS = 4
A_KV_HEADS = 2
DIFF_HEADS = 4
DIFF_DIM = 32
WIN_HEADS = 4
WIN_KV_HEADS = 2
WINDOW = 128
RET_HEADS = 4
RET_DK = 64
RET_DV = 64
RET_CHUNK = 128
N_BRANCH = 4
BRANCH_W = 256
PIECES = (
    A_HEADS * HEAD_DIM, A_KV_HEADS * HEAD_DIM, A_KV_HEADS * HEAD_DIM,
    2 * DIFF_HEADS * DIFF_DIM, 2 * DIFF_HEADS * DIFF_DIM, DIFF_HEADS * 2 * DIFF_DIM,
    WIN_HEADS * HEAD_DIM, WIN_KV_HEADS * HEAD_DIM, WIN_KV_HEADS * HEAD_DIM,
    RET_HEADS * RET_DK, RET_HEADS * RET_DK, RET_HEADS * RET_DV, RET_HEADS * RET_DV,
)
MIX_COLS = sum(PIECES)
IN_COLS = MIX_COLS + N_BRANCH * D_MODEL
PEER_HEADS = 8
PEER_NK = 128
PEER_N = PEER_NK * PEER_NK
PEER_TOPK = 16
PEER_DQ = 128
PEER_BLOCK = 128
DEEPNORM_ALPHA = (2 * DEPTH) ** 0.25
DEEPNORM_BETA = (8 * DEPTH) ** -0.25
LN_EPS = 1e-5
RMS_EPS = 1e-6
F32 = jnp.float32

kernel_name = 'hybrid_dit_gqa_diff_window_retention_peer'


def layer_norm(x, p):
    xf = x.astype(F32)
    mu = jnp.mean(xf, -1, keepdims=True)
    var = jnp.mean(jnp.square(xf - mu), -1, keepdims=True)
    return ((xf - mu) * lax.rsqrt(var + LN_EPS) * p[0].astype(F32) + p[1].astype(F32)).astype(x.dtype)


def rms_norm(x, g):
    xf = x.astype(F32)
    return (xf * lax.rsqrt(jnp.mean(xf * xf, -1, keepdims=True) + RMS_EPS) * g.astype(F32)).astype(x.dtype)


def to_heads(t, n):
    b, l, _ = t.shape
    return t.reshape(b, l, n, -1).transpose(0, 2, 1, 3)


def from_heads(t):
    b, h, l, d = t.shape
    return t.transpose(0, 2, 1, 3).reshape(b, l, h * d)


def split_pieces(p):
    out, start = [], 0
    for w in PIECES:
        out.append(p[..., start:start + w])
        start += w
    return out


def axial_tables(row, col, d):
    quarter = d // 4
    inv = ROPE_THETA ** (-jnp.arange(quarter, dtype=F32) / quarter)
    ang = jnp.stack([row[:, None] * inv, col[:, None] * inv], axis=1)
    return jnp.cos(ang), jnp.sin(ang)


def apply_axial(x, cs):
    cos, sin = cs
    d = x.shape[-1]
    xs = x.astype(F32).reshape(x.shape[:-1] + (2, 2, d // 4))
    x1, x2 = xs[..., 0, :], xs[..., 1, :]
    out = jnp.stack([x1 * cos - x2 * sin, x2 * cos + x1 * sin], axis=-2)
    return out.reshape(x.shape).astype(x.dtype)


def rope1d_tables(pos, d):
    half = d // 2
    inv = ROPE_THETA ** (-jnp.arange(half, dtype=F32) / half)
    ang = pos[:, None] * inv
    return jnp.cos(ang), jnp.sin(ang)


def apply_rope1d(x, cs):
    cos, sin = cs
    xf = x.astype(F32)
    half = x.shape[-1] // 2
    x1, x2 = xf[..., :half], xf[..., half:]
    return jnp.concatenate([x1 * cos - x2 * sin, x2 * cos + x1 * sin], -1).astype(x.dtype)


def sink_softmax(s, sink):
    m = jnp.maximum(jnp.max(s, -1, keepdims=True), sink)
    e = jnp.exp(s - m)
    return e / (jnp.sum(e, -1, keepdims=True) + jnp.exp(sink - m))


def gqa_blocked(q, k, v):
    b, hq, s, d = q.shape
    hkv = k.shape[1]
    g = hq // hkv
    nb = s // Q_BLOCK
    scale = d ** -0.5
    qb = q.reshape(b, hkv, g, nb, Q_BLOCK, d).transpose(3, 0, 1, 2, 4, 5)

    def one(qblk):
        sc = jnp.einsum('bkgqd,bkld->bkgql', qblk, k, preferred_element_type=F32) * scale
        p = jax.nn.softmax(sc, axis=-1).astype(v.dtype)
        return jnp.einsum('bkgql,bkle->bkgqe', p, v)

    o = lax.map(one, qb)
    return o.transpose(1, 2, 3, 0, 4, 5).reshape(b, hq, s, v.shape[-1])


def diff_blocked(q1, q2, k1, k2, v, lam):
    b, h, s, d = q1.shape
    nb = s // Q_BLOCK
    scale = d ** -0.5

    def blocks(t):
        return t.reshape(b, h, nb, Q_BLOCK, d).transpose(2, 0, 1, 3, 4)

    def one(qs):
        a1, a2 = qs
        p1 = jax.nn.softmax(jnp.einsum('bhqd,bhkd->bhqk', a1, k1, preferred_element_type=F32) * scale, axis=-1)
        p2 = jax.nn.softmax(jnp.einsum('bhqd,bhkd->bhqk', a2, k2, preferred_element_type=F32) * scale, axis=-1)
        return jnp.einsum('bhqk,bhke->bhqe', (p1 - lam * p2).astype(v.dtype), v)

    o = lax.map(one, (blocks(q1), blocks(q2)))
    return o.transpose(1, 2, 0, 3, 4).reshape(b, h, s, v.shape[-1])


def window_attn(q, k, v, kc, vc, sink):
    b, hq, s, d = q.shape
    hkv = k.shape[1]
    g = hq // hkv
    nb = s // Q_BLOCK
    scale = d ** -0.5
    qb = q.reshape(b, hkv, g, nb, Q_BLOCK, d)

    def band(t):
        tp = jnp.pad(t, ((0, 0), (0, 0), (Q_BLOCK, Q_BLOCK), (0, 0)))
        tp = tp.reshape(b, hkv, nb + 2, Q_BLOCK, t.shape[-1])
        return jnp.concatenate([tp[:, :, :-2], tp[:, :, 1:-1], tp[:, :, 2:]], axis=3)

    kw, vw = band(k), band(v)
    qi = jnp.arange(Q_BLOCK)[:, None]
    kj = jnp.arange(3 * Q_BLOCK)[None, :]
    in_band = jnp.abs(Q_BLOCK + qi - kj) <= WINDOW
    kpos = (jnp.arange(nb)[:, None] - 1) * Q_BLOCK + kj
    mask = in_band[None] & ((kpos >= 0) & (kpos < s))[:, None, :]
    s_loc = jnp.einsum('bkgnqd,bknld->bkgnql', qb, kw, preferred_element_type=F32) * scale
    s_loc = jnp.where(mask, s_loc, -jnp.inf)
    s_ctx = jnp.einsum('bkgnqd,bkcd->bkgnqc', qb, kc, preferred_element_type=F32) * scale
    p = sink_softmax(jnp.concatenate([s_loc, s_ctx], -1), sink.astype(F32).reshape(1, hkv, g, 1, 1, 1))
    p = p.astype(v.dtype)
    nl = 3 * Q_BLOCK
    o = (jnp.einsum('bkgnql,bknle->bkgnqe', p[..., :nl], vw)
         + jnp.einsum('bkgnqc,bkce->bkgnqe', p[..., nl:], vc))
    return o.reshape(b, hq, s, v.shape[-1])


def ctx_sink_attn(q, kc, vc, sink):
    b, hq, l, d = q.shape
    hkv = kc.shape[1]
    g = hq // hkv
    sc = jnp.einsum('bkgqd,bkcd->bkgqc', q.reshape(b, hkv, g, l, d), kc, preferred_element_type=F32) * d ** -0.5
    p = sink_softmax(sc, sink.astype(F32).reshape(1, hkv, g, 1, 1)).astype(vc.dtype)
    return jnp.einsum('bkgqc,bkce->bkgqe', p, vc).reshape(b, hq, l, vc.shape[-1])


def retention_chunked(q, k, v, log_gamma, s0):
    b, h, l, _ = q.shape
    n = l // RET_CHUNK
    q, k, v = q.astype(F32), k.astype(F32), v.astype(F32)
    idx = jnp.arange(RET_CHUNK, dtype=F32)
    diff = idx[:, None] - idx[None, :]
    lg = log_gamma.astype(F32)
    dmat = jnp.exp(jnp.where(diff[None] >= 0, diff[None] * lg[:, None, None], -jnp.inf))
    xi = jnp.exp((idx + 1.0)[None] * lg[:, None])
    zeta = jnp.exp((RET_CHUNK - 1.0 - idx)[None] * lg[:, None])
    g_chunk = jnp.exp(RET_CHUNK * lg)

    def chunks(t):
        return t.reshape(b, h, n, RET_CHUNK, t.shape[-1]).transpose(2, 0, 1, 3, 4)

    def step(st, inp):
        qc, kc, vc = inp
        inner = jnp.einsum('bhid,bhjd->bhij', qc, kc) * dmat
        o = jnp.einsum('bhij,bhjv->bhiv', inner, vc) + jnp.einsum('bhid,bhdv->bhiv', qc, st) * xi[..., None]
        st = st * g_chunk[:, None, None] + jnp.einsum('bhjd,bhjv->bhdv', kc * zeta[..., None], vc)
        return st, o

    s_fin, o = lax.scan(step, s0.astype(F32), (chunks(q), chunks(k), chunks(v)))
    return o.transpose(1, 2, 0, 3, 4).reshape(b, h, l, v.shape[-1]), s_fin


def mixer_gqa(lat, cx, gain, ax, need_ctx):
    q = apply_axial(rms_norm(to_heads(lat[0], A_HEADS), gain[0]), ax)
    k = apply_axial(rms_norm(to_heads(lat[1], A_KV_HEADS), gain[1]), ax)
    v = to_heads(lat[2], A_KV_HEADS)
    kc = rms_norm(to_heads(cx[1], A_KV_HEADS), gain[1])
    vc = to_heads(cx[2], A_KV_HEADS)
    o = gqa_blocked(q, jnp.concatenate([kc, k], 2), jnp.concatenate([vc, v], 2))
    oc = None
    if need_ctx:
        oc = from_heads(gqa_blocked(rms_norm(to_heads(cx[0], A_HEADS), gain[0]), kc, vc))
    return from_heads(o), oc


def diff_heads(t):
    b, l, _ = t.shape
    t = t.reshape(b, l, DIFF_HEADS, 2, DIFF_DIM).transpose(3, 0, 2, 1, 4)
    return t[0], t[1]


def mixer_diff(lat, cx, lam_p, subln, layer, ax, need_ctx):
    lam_init = 0.8 - 0.6 * math.exp(-0.3 * layer)
    lp = lam_p.astype(F32)
    lam = jnp.exp(jnp.sum(lp[0] * lp[1])) - jnp.exp(jnp.sum(lp[2] * lp[3])) + lam_init
    q1, q2 = diff_heads(lat[0])
    k1, k2 = diff_heads(lat[1])
    q1, q2, k1, k2 = apply_axial(q1, ax), apply_axial(q2, ax), apply_axial(k1, ax), apply_axial(k2, ax)
    v = to_heads(lat[2], DIFF_HEADS)
    ck1, ck2 = diff_heads(cx[1])
    cv = to_heads(cx[2], DIFF_HEADS)

    def post(o):
        return from_heads(rms_norm(o, subln) * (1.0 - lam_init))

    o = diff_blocked(q1, q2, jnp.concatenate([ck1, k1], 2), jnp.concatenate([ck2, k2], 2),
                     jnp.concatenate([cv, v], 2), lam)
    oc = None
    if need_ctx:
        cq1, cq2 = diff_heads(cx[0])
        oc = post(diff_blocked(cq1, cq2, ck1, ck2, cv, lam))
    return post(o), oc


def mixer_window(lat, cx, sink, ax, need_ctx):
    q = apply_axial(to_heads(lat[0], WIN_HEADS), ax)
    k = apply_axial(to_heads(lat[1], WIN_KV_HEADS), ax)
    v = to_heads(lat[2], WIN_KV_HEADS)
    kc = to_heads(cx[1], WIN_KV_HEADS)
    vc = to_heads(cx[2], WIN_KV_HEADS)
    o = from_heads(window_attn(q, k, v, kc, vc, sink))
    oc = None
    if need_ctx:
        oc = from_heads(ctx_sink_attn(to_heads(cx[0], WIN_HEADS), kc, vc, sink))
    return o, oc


def mixer_retention(lat, cx, decay, norm, rope_lat, rope_ctx, need_ctx):
    lg = jax.nn.log_sigmoid(decay.astype(F32))

    def qkv(p, rope):
        q = apply_rope1d(to_heads(p[0], RET_HEADS), rope)
        k = apply_rope1d(to_heads(p[1], RET_HEADS), rope) * (RET_DK ** -0.5)
        return q, k, to_heads(p[2], RET_HEADS)

    def flip(t):
        return jnp.flip(t, axis=2)

    def finish(o, g):
        mu = jnp.mean(o, -1, keepdims=True)
        var = jnp.mean(jnp.square(o - mu), -1, keepdims=True)
        on = from_heads((o - mu) * lax.rsqrt(var + LN_EPS)) * norm[0].astype(F32) + norm[1].astype(F32)
        return (on * jax.nn.silu(g.astype(F32))).astype(g.dtype)

    qc, kc, vc = qkv(cx, rope_ctx)
    ql, kl, vl = qkv(lat, rope_lat)
    zero = jnp.zeros((qc.shape[0], RET_HEADS, RET_DK, RET_DV), F32)
    ocf, sf = retention_chunked(qc, kc, vc, lg[0], zero)
    ocb, sb = retention_chunked(flip(qc), flip(kc), flip(vc), lg[1], zero)
    olf, _ = retention_chunked(ql, kl, vl, lg[0], sf)
    olb, _ = retention_chunked(flip(ql), flip(kl), flip(vl), lg[1], sb)
    o = finish(olf + flip(olb), lat[3])
    oc = finish(ocf + flip(ocb), cx[3]) if need_ctx else None
    return o, oc


def merge_branches(u, outs, w_gate, w_branch, w_out):
    d = u.shape[-1]
    m = sum(jax.nn.sigmoid(u @ w_gate[:, i * d:(i + 1) * d]) * (o @ w_branch[i]) for i, o in enumerate(outs))
    return m @ w_out


def peer_ffn(u, wq, subkeys, tab_u, tab_v):
    b, l, d = u.shape
    tok = u.reshape(-1, PEER_BLOCK, d)

    def block(xb):
        q = (xb @ wq).reshape(PEER_BLOCK, PEER_HEADS, 2, PEER_DQ)
        s = jnp.einsum('thpk,hpnk->thpn', q, subkeys, preferred_element_type=F32)
        sv, si = lax.top_k(s, PEER_TOPK)
        cand = (sv[:, :, 0, :, None] + sv[:, :, 1, None, :]).reshape(PEER_BLOCK, PEER_HEADS, PEER_TOPK * PEER_TOPK)
        cidx = (si[:, :, 0, :, None] * PEER_NK + si[:, :, 1, None, :]).reshape(PEER_BLOCK, PEER_HEADS, PEER_TOPK * PEER_TOPK)
        fv, fi = lax.top_k(cand, PEER_TOPK)
        e = jnp.take_along_axis(cidx, fi, axis=-1)
        gate = jax.nn.softmax(fv, axis=-1)
        act = jax.nn.gelu(jnp.einsum('thkd,td->thk', tab_u[e], xb, preferred_element_type=F32))
        return jnp.einsum('thk,thkd->td', (gate * act).astype(xb.dtype), tab_v[e])

    return lax.map(block, tok).reshape(b, l, d)


def setup_inputs(seed: int = 0) -> dict:
    key = jax.random.key(seed)
    ks = jax.random.split(key, 24)
    d = D_MODEL

    def nrm(k, shape, s):
        return s * jax.random.normal(k, shape, F32)

    base_decay = np.log(2.0 ** (5 + np.arange(RET_HEADS)) - 1.0).astype(np.float32)
    return {
        'x': nrm(ks[0], (BATCH, SEQ, d), 1.0),
        'c': nrm(ks[1], (BATCH, d), 1.0),
        'ctx': nrm(ks[2], (BATCH, CTX_LEN, d), 1.0),
        'c_ctx': nrm(ks[3], (d,), 1.0),
        'w_mod': nrm(ks[4], (DEPTH, d, 6 * d), 0.5 * d ** -0.5),
        'b_mod': nrm(ks[5], (DEPTH, 6 * d), 0.01),
        'w_in': nrm(ks[6], (DEPTH, d, IN_COLS), d ** -0.5),
        'qk_gain': 1.0 + nrm(ks[7], (DEPTH, 2, HEAD_DIM), 0.05),
        'diff_lambda': nrm(ks[8], (DEPTH, 4, DIFF_DIM), 0.1),
        'diff_subln': 1.0 + nrm(ks[9], (DEPTH, 2 * DIFF_DIM), 0.05),
        'win_sink': nrm(ks[10], (DEPTH, WIN_HEADS), 0.5),
        'ret_decay': jnp.asarray(base_decay) + nrm(ks[11], (DEPTH, 2, RET_HEADS), 0.1),
        'ret_norm': jnp.stack([1.0 + nrm(ks[12], (DEPTH, RET_HEADS * RET_DV), 0.05),
                               nrm(ks[13], (DEPTH, RET_HEADS * RET_DV), 0.02)], axis=1),
        'w_branch': nrm(ks[14], (DEPTH, N_BRANCH, BRANCH_W, d), DEEPNORM_BETA * BRANCH_W ** -0.5),
        'w_out': nrm(ks[15], (DEPTH, d, d), DEEPNORM_BETA * d ** -0.5),
        'ln_attn': jnp.stack([1.0 + nrm(ks[16], (DEPTH, d), 0.05), nrm(ks[17], (DEPTH, d), 0.02)], axis=1),
        'ln_ffn': jnp.stack([1.0 + nrm(ks[18], (DEPTH, d), 0.05), nrm(ks[19], (DEPTH, d), 0.02)], axis=1),
        'peer_wq': nrm(ks[20], (DEPTH, d, PEER_HEADS * 2 * PEER_DQ), d ** -0.5),
        'peer_subkeys': nrm(ks[21], (DEPTH, PEER_HEADS, 2, PEER_NK, PEER_DQ), PEER_DQ ** -0.5),
        'peer_u': nrm(ks[22], (DEPTH, PEER_N, d), d ** -0.5),
        'peer_v': nrm(ks[23], (DEPTH, PEER_N, d), DEEPNORM_BETA),
    }


def reference(x, c, ctx, c_ctx, w_mod, b_mod, w_in, qk_gain, diff_lambda, diff_subln, win_sink,
              ret_decay, ret_norm, w_branch, w_out, ln_attn, ln_ffn, peer_wq, peer_subkeys, peer_u, peer_v):
    b, s, d = x.shape
    n_ctx = ctx.shape[1]
    rows = s // GRID_W
    row = jnp.broadcast_to(jnp.arange(rows, dtype=F32)[:, None], (rows, GRID_W)).reshape(-1)
    col = jnp.broadcast_to(jnp.arange(GRID_W, dtype=F32)[None, :], (rows, GRID_W)).reshape(-1)
    ax64 = axial_tables(row, col, HEAD_DIM)
    ax32 = axial_tables(row, col, DIFF_DIM)
    rope_ctx = rope1d_tables(jnp.arange(n_ctx, dtype=F32), RET_DK)
    rope_lat = rope1d_tables(n_ctx + jnp.arange(s, dtype=F32), RET_DK)
    cond = jax.nn.silu(c)
    cond_ctx = jax.nn.silu(c_ctx)
    h, hc = x, ctx
    for l in range(DEPTH):
        need_ctx = l < DEPTH - 1
        mod = (cond @ w_mod[l] + b_mod[l]).reshape(b, 6, 1, d)
        modc = (cond_ctx @ w_mod[l] + b_mod[l]).reshape(6, d)
        u = h * (1 + mod[:, 1]) + mod[:, 0]
        uc = hc * (1 + modc[1]) + modc[0]
        w_mix = w_in[l, :, :MIX_COLS]
        w_gate = w_in[l, :, MIX_COLS:]
        pl = split_pieces(u @ w_mix)
        pc = split_pieces(uc @ w_mix)
        oa, oac = mixer_gqa(pl[0:3], pc[0:3], qk_gain[l], ax64, need_ctx)
        ob, obc = mixer_diff(pl[3:6], pc[3:6], diff_lambda[l], diff_subln[l], l, ax32, need_ctx)
        oc, occ = mixer_window(pl[6:9], pc[6:9], win_sink[l], ax64, need_ctx)
        od, odc = mixer_retention(pl[9:13], pc[9:13], ret_decay[l], ret_norm[l], rope_lat, rope_ctx, need_ctx)
        y = merge_branches(u, (oa, ob, oc, od), w_gate, w_branch[l], w_out[l])
        h = layer_norm(DEEPNORM_ALPHA * h + mod[:, 2] * y, ln_attn[l])
        if need_ctx:
            yc = merge_branches(uc, (oac, obc, occ, odc), w_gate, w_branch[l], w_out[l])
            hc = layer_norm(DEEPNORM_ALPHA * hc + modc[2] * yc, ln_attn[l])
        u2 = h * (1 + mod[:, 4]) + mod[:, 3]
        y2 = peer_ffn(u2, peer_wq[l], peer_subkeys[l], peer_u[l], peer_v[l])
        h = layer_norm(DEEPNORM_ALPHA * h + mod[:, 5] * y2, ln_ffn[l])
        if need_ctx:
            uc2 = hc * (1 + modc[4]) + modc[3]
            yc2 = peer_ffn(uc2, peer_wq[l], peer_subkeys[l], peer_u[l], peer_v[l])
            hc = layer_norm(DEEPNORM_ALPHA * hc + modc[5] * yc2, ln_ffn[l])
    return h
```

```python
import math
from contextlib import ExitStack
import numpy as np
import concourse.bass as bass
import concourse.mybir as mybir
from concourse.bass_utils import run_bass_kernel_spmd

F32 = mybir.dt.float32
BF16 = mybir.dt.bfloat16
U32 = mybir.dt.uint32
I32 = mybir.dt.int32
AF = mybir.ActivationFunctionType
ALU = mybir.AluOpType
AX = mybir.AxisListType

D = 1024
NCTX = 256
GRID_W = 64
MIX = 2816
DEPTH = 2
ALPHA = (2 * DEPTH) ** 0.25
LN_EPS = 1e-5
RMS_EPS = 1e-6
NCORES = 8


class Tl:
    def __init__(self, t, name):
        self.t, self.name = t, name
        self.w = {}
        self.r = {}
        self.dkey = None
        self.is_dram = False

    def __getitem__(self, k):
        return self.t[k]


class Em:
    def __init__(self, nc, st):
        self.nc, self.gst = nc, st
        self.st = st
        self.eng = {"pe": nc.tensor, "dve": nc.vector, "act": nc.scalar, "pool": nc.gpsimd, "sp": nc.sync}
        self.sem, self.cnt = {}, {}
        for k in ("pe", "dve", "act", "pool"):
            self.sem[k] = st.enter_context(nc.semaphore("s_" + k))
            self.cnt[k] = 0
        self.seen = {k: {} for k in self.eng}
        self.nd = 0
        self.free_dkeys = []
        self.stage_tiles = []
        self.ninst = 0

    def sb(self, name, shape, dtype=F32):
        self.nalloc = getattr(self, "nalloc", 0) + 1
        t = Tl(self.st.enter_context(self.nc.sbuf_tensor("%s_%d" % (name, self.nalloc), list(shape), dtype)), name)
        self.stage_tiles.append(t)
        return t

    def ps(self, name, shape=(128, 512), dtype=F32):
        return Tl(self.gst.enter_context(self.nc.psum_tensor(name, list(shape), dtype)), name)

    def dram(self, name, shape, dtype=F32, kind="ExternalInput"):
        t = Tl(self.nc.dram_tensor(name, list(shape), dtype, kind=kind).ap(), name)
        t.is_dram = True
        if kind == "ExternalInput":
            self.inputs = getattr(self, "inputs", []) + [name]
        return t

    def begin_stage(self):
        self.st = ExitStack()
        self.stage_tiles = []

    def end_stage(self):
        self.barrier()
        for t in self.stage_tiles:
            if t.dkey is not None:
                self.free_dkeys.append(t.dkey)
        self.st.close()
        self.st = self.gst
        self.stage_tiles = []

    def _dkey(self, tl):
        if tl.dkey is None:
            if self.free_dkeys:
                tl.dkey = self.free_dkeys.pop()
            else:
                tl.dkey = "d%d" % self.nd
                self.nd += 1
                self.sem[tl.dkey] = self.gst.enter_context(self.nc.semaphore("s_" + tl.dkey))
                self.cnt[tl.dkey] = 0
        return tl.dkey

    def _waits(self, e, r, w):
        need = {}

        def nd(d):
            for k, c in d.items():
                if e == "pe" and k == "pe":
                    continue
                if c > need.get(k, 0):
                    need[k] = c

        for t in r:
            if not t.is_dram:
                nd(t.w)
        for t in w:
            if not t.is_dram:
                nd(t.w)
                nd(t.r)
        seen = self.seen[e]
        for k, c in need.items():
            if seen.get(k, 0) >= c:
                continue
            seen[k] = c
            self.eng[e].wait_ge(self.sem[k], c)
            self.ninst += 1

    def _record(self, key, c, r, w):
        for t in w:
            if not t.is_dram:
                t.w = {key: c}
                t.r = {}
        for t in r:
            if not t.is_dram and t not in w:
                if c > t.r.get(key, 0):
                    t.r[key] = c

    def op(self, e, fn, r=(), w=()):
        self._waits(e, r, w)
        ins = fn(self.eng[e])
        self.cnt[e] += 1
        self.ninst += 1
        ins.then_inc(self.sem[e], 1)
        self._record(e, self.cnt[e], r, w)

    def V(self, fn, r=(), w=()):
        self.op("dve", fn, r, w)

    def A(self, fn, r=(), w=()):
        self.op("act", fn, r, w)

    def G(self, fn, r=(), w=()):
        self.op("pool", fn, r, w)

    def P(self, fn, r=(), w=()):
        self.op("pe", fn, r, w)

    def dma(self, out_ap, in_ap, r, w, q="sp", fn=None):
        sbt = None
        for t in list(w) + list(r):
            if not t.is_dram:
                sbt = t
                break
        self._waits(q, r, w)
        key = self._dkey(sbt)
        if fn is None:
            ins = self.eng[q].dma_start(out=out_ap, in_=in_ap)
        else:
            ins = fn(self.eng[q])
        self.cnt[key] += 16
        self.ninst += 1
        ins.then_inc(self.sem[key], 16)
        self._record(key, self.cnt[key], r, w)

    def barrier(self):
        for e in ("sp", "pe", "dve", "act", "pool"):
            seen = self.seen[e]
            for k, c in self.cnt.items():
                if c > 0 and seen.get(k, 0) < c:
                    seen[k] = c
                    self.eng[e].wait_ge(self.sem[k], c)
                    self.ninst += 1

    def finish(self):
        self.barrier()


def act(e, out, in_, func, **kw):
    return e.activation(out=out, in_=in_, func=func, **kw)


def load_bcast_row(em, dst, src_ap, n, parts=128):
    em.dma(dst[0:parts, 0:n], src_ap.partition_broadcast(parts), r=[], w=[dst])


def emit_modulate(em, U, H, SC1, SH, TMP):
    em.V(lambda e: e.tensor_tensor(out=TMP[:, :], in0=H[:, :], in1=SC1[:, :], op=ALU.mult), r=[H, SC1], w=[TMP])
    em.G(lambda e: e.tensor_tensor(out=U[:, :], in0=TMP[:, :], in1=SH[:, :], op=ALU.add), r=[TMP, SH], w=[U])


def emit_transpose8(em, UT, U, IDN, PSA, PSB, ncols=D):
    nk = ncols // 128
    k = 0
    flip = 0
    while k < nk:
        ps = PSA if flip == 0 else PSB
        nn = min(4, nk - k)
        for j in range(nn):
            em.P(lambda e, j=j, k=k, ps=ps: e.transpose(out=ps[:, j * 128:(j + 1) * 128], in_=U[:, (k + j) * 128:(k + j + 1) * 128],
                                                         identity=IDN[:, :]), r=[U, IDN], w=[ps])
        fn = (lambda e, k=k, nn=nn, ps=ps: act(e, UT[:, k:k + nn, :], ps[:, 0:nn * 128].rearrange("p (a b) -> p a b", b=128), AF.Copy))
        if flip == 0:
            em.A(fn, r=[ps], w=[UT])
        else:
            em.V(lambda e, k=k, nn=nn, ps=ps: e.tensor_copy(out=UT[:, k:k + nn, :], in_=ps[:, 0:nn * 128].rearrange("p (a b) -> p a b", b=128)),
                 r=[ps], w=[UT])
        k += nn
        flip ^= 1


def emit_layernorm(em, OUT, R, GAM, BET, ST, MV, RS, TMP):
    for c in range(2):
        em.V(lambda e, c=c: e.bn_stats(out=ST[:, c * 6:(c + 1) * 6], in_=R[:, c * 512:(c + 1) * 512]), r=[R], w=[ST])
    em.V(lambda e: e.bn_aggr(out=MV[:, 0:2], in_=ST[:, 0:12]), r=[ST], w=[MV])
    em.V(lambda e: e.tensor_scalar(out=RS[:, 0:1], in0=MV[:, 1:2], scalar1=LN_EPS, scalar2=None, op0=ALU.add), r=[MV], w=[RS])
    em.A(lambda e: act(e, RS[:, 0:1], RS[:, 0:1], AF.Sqrt), r=[RS], w=[RS])
    em.V(lambda e: e.reciprocal(out=RS[:, 0:1], in_=RS[:, 0:1]), r=[RS], w=[RS])
    em.V(lambda e: e.tensor_scalar(out=TMP[:, :], in0=R[:, :], scalar1=MV[:, 0:1], scalar2=RS[:, 0:1], op0=ALU.subtract, op1=ALU.mult),
         r=[R, MV, RS], w=[TMP])
    em.G(lambda e: e.tensor_tensor(out=TMP[:, :], in0=TMP[:, :], in1=GAM[:, :], op=ALU.mult), r=[TMP, GAM], w=[TMP])
    em.V(lambda e: e.tensor_tensor(out=OUT[:, :], in0=TMP[:, :], in1=BET[:, :], op=ALU.add), r=[TMP, BET], w=[OUT])


def load_weight_bf16(em, W, src_ap, nk, ncols, STG, col0=0, dummy=None):
    i = 0
    stw = STG[0].t.shape[1]
    for k in range(nk):
        c = 0
        while c < ncols:
            cw = min(stw, ncols - c)
            stg = STG[i % len(STG)]
            em.dma(stg[:, 0:cw], src_ap[k * 128:(k + 1) * 128, col0 + c:col0 + c + cw], r=[], w=[stg])
            if i % 2 == 0:
                em.A(lambda e, k=k, c=c, cw=cw, stg=stg: act(e, W[:, k, c:c + cw], stg[:, 0:cw], AF.Copy), r=[stg], w=[W])
            else:
                em.V(lambda e, k=k, c=c, cw=cw, stg=stg: e.tensor_copy(out=W[:, k, c:c + cw], in_=stg[:, 0:cw]), r=[stg], w=[W])
            c += cw
            i += 1


class Ctx:
    pass


ROT_GROUPS = [
    (0, 6, 2, 16, 0, 32),
    (512, 16, 2, 8, 64, 80),
    (1280, 6, 2, 16, 0, 32),
    (1792, 8, 1, 32, 96, 128),
]
COPY_COLS = [(384, 512), (1024, 1280), (1664, 1792), (2304, 2816)]
NRM_GROUPS = [(0, 6, 64, 0), (512, 16, 32, 6), (1280, 6, 64, 22)]
XT_BLOCKS = [0, 128, 256, 512, 640, 768, 896, 1280, 1408, 1536, 1792, 1920, 2048, 2176]
V_GROUPS = [(384, 2, 0), (1024, 4, 2), (1664, 2, 6)]


def hrows(C, l, blk):
    if l == 0:
        if blk < 2:
            return C.ctx.t[blk * 128:(blk + 1) * 128, :]
        return C.x.t[(blk - 2) * 128:(blk - 1) * 128, :]
    return C.H2.t[blk * 128:(blk + 1) * 128, :]


def stage_mod(C):
    em = C.em
    em.begin_stage()
    CT = em.sb("CT", [128, 8, 2])
    WS = [em.sb("WS%d" % i, [128, 8, 512]) for i in range(2)]
    BB = em.sb("BB", [2, 6 * D])
    OO = em.sb("OO", [2, 6 * D])
    em.dma(CT[:, :, :], C.cT.t.rearrange("(k p) r -> p k r", p=128), r=[], w=[CT])
    em.A(lambda e: act(e, CT[:, :, :], CT[:, :, :], AF.Silu), r=[CT], w=[CT])
    it = 0
    for l in range(C.depth):
        em.dma(BB[:, :], C.b_mod.t[l, :].partition_broadcast(2), r=[], w=[BB])
        for g in range(12):
            ws, ps = WS[it % 2], C.PS[it % 2]
            em.dma(ws[:, :, :], C.w_mod.t[l, :, g * 512:(g + 1) * 512].rearrange("(k p) c -> p k c", p=128), r=[], w=[ws])
            for k in range(8):
                em.P(lambda e: e.matmul(ps[0:2, :], lhsT=CT[:, k, :], rhs=ws[:, k, :], start=(k == 0), stop=(k == 7)), r=[CT, ws], w=[ps])
            em.V(lambda e: e.tensor_tensor(out=OO[:, g * 512:(g + 1) * 512], in0=ps[0:2, :], in1=BB[:, g * 512:(g + 1) * 512], op=ALU.add),
                 r=[ps, BB], w=[OO])
            it += 1
        em.dma(C.mod.t[l, :, :], OO[:, :], r=[OO], w=[C.mod])
    em.end_stage()


def load_mod_rows(C, l, idxs, tiles, plus1=()):
    em = C.em
    for ty in range(2):
        for i, mi in enumerate(idxs):
            t = tiles[ty][i]
            em.dma(t[:, :], C.mod.t[l, ty, mi * D:(mi + 1) * D].partition_broadcast(128), r=[], w=[t])
            if mi in plus1:
                em.V(lambda e: e.tensor_scalar(out=t[:, :], in0=t[:, :], scalar1=1.0, scalar2=None, op0=ALU.add), r=[t], w=[t])


def stage_proj(C, l):
    em = C.em
    em.begin_stage()
    PS = C.PS
    W = em.sb("W", [128, 8, MIX], BF16)
    STG = [em.sb("STG%d" % i, [128, 2048]) for i in range(2)]
    GN = em.sb("GN", [128, 384])
    MR = [[em.sb("MR%d_%d" % (ty, i), [128, D]) for i in range(2)] for ty in range(2)]
    HB = [em.sb("HB%d" % i, [128, D]) for i in range(2)]
    TB = [em.sb("TB%d" % i, [128, 160]) for i in range(2)]
    TMP = em.sb("TMP", [128, D])
    U = em.sb("U", [128, D])
    UT = em.sb("UT", [128, 8, 128], BF16)
    PSB = em.sb("PSB", [128, MIX])
    PO = [em.sb("PO%d" % i, [128, MIX]) for i in range(2)]
    NR = em.sb("NR", [128, 28])
    RM = em.sb("RM", [128, 28])
    XTo = [em.sb("XTo%d" % i, [128, 14, 128]) for i in range(2)]
    VPo = [em.sb("VPo%d" % i, [128, 8, 65]) for i in range(2)]
    SQ = em.sb("SQ", [128, 512])
    SS = em.sb("SS", [128, 8])
    T1 = em.sb("T1", [128, 256])
    T2 = em.sb("T2", [128, 256])
    T3 = em.sb("T3", [128, 256])
    T4 = em.sb("T4", [128, 256])
    RX = em.sb("RX", [28, 1])
    RR = em.sb("RR", [1, 28])

    load_bcast_row(em, GN, C.gain.t[l, :], 384)
    load_mod_rows(C, l, [0, 1], MR, plus1=(1,))
    for i in range(2):
        em.G(lambda e: e.memset(VPo[i][:, :, :], 1.0), w=[VPo[i]])
    load_weight_bf16(em, W, C.w_in.t[l], 8, MIX, STG)

    pmi = 0
    xt4 = C.XT.t.rearrange("p (j e) l -> p j e l", e=2)
    for b in range(C.NBx):
        ty = 1 if b < 2 else 0
        H, T, PO_, XTo_, VPo_ = HB[b % 2], TB[b % 2], PO[b % 2], XTo[b % 2], VPo[b % 2]
        em.dma(H[:, :], hrows(C, l, b), r=[], w=[H])
        em.dma(T[:, :], C.tab.t[b * 128:(b + 1) * 128, :], r=[], w=[T])
        emit_modulate(em, U, H, MR[ty][1], MR[ty][0], TMP)
        emit_transpose8(em, UT, U, C.IDN, PS[0], PS[1])
        c = 0
        while c < MIX:
            cw = min(512, MIX - c)
            ps = PS[2 + pmi % 3]
            pmi += 1
            for k in range(8):
                em.P(lambda e: e.matmul(ps[:, 0:cw], lhsT=UT[:, k, :], rhs=W[:, k, c:c + cw], start=(k == 0), stop=(k == 7)), r=[UT, W], w=[ps])
            em.A(lambda e: act(e, PSB[:, c:c + cw], ps[:, 0:cw], AF.Copy), r=[ps], w=[PSB])
            c += cw
        v384 = PSB[:, 0:384].rearrange("p (h d) -> p h d", d=64)
        em.A(lambda e: act(e, SQ[:, 0:384], PSB[:, 0:384], AF.Square), r=[PSB], w=[SQ])
        em.V(lambda e: e.tensor_reduce(out=SS[:, 0:6], in_=SQ[:, 0:384].rearrange("p (h d) -> p h d", d=64), axis=AX.X, op=ALU.add), r=[SQ], w=[SS])
        em.V(lambda e: e.tensor_scalar(out=SS[:, 0:6], in0=SS[:, 0:6], scalar1=1.0 / 64, scalar2=RMS_EPS, op0=ALU.mult, op1=ALU.add), r=[SS], w=[SS])
        em.A(lambda e: act(e, SS[:, 0:6], SS[:, 0:6], AF.Sqrt), r=[SS], w=[SS])
        em.V(lambda e: e.reciprocal(out=SS[:, 0:6], in_=SS[:, 0:6]), r=[SS], w=[SS])
        em.V(lambda e: e.tensor_tensor(out=v384, in0=v384, in1=SS[:, 0:6].unsqueeze(2).to_broadcast([128, 6, 64]), op=ALU.mult), r=[PSB, SS], w=[PSB])
        em.V(lambda e: e.tensor_tensor(out=PSB[:, 0:384], in0=PSB[:, 0:384], in1=GN[:, :], op=ALU.mult), r=[PSB, GN], w=[PSB])
        for (c0, c1) in COPY_COLS:
            em.G(lambda e: e.tensor_copy(out=PO_[:, c0:c1], in_=PSB[:, c0:c1]), r=[PSB], w=[PO_])
        for (c0, nh, a, q, co, so) in ROT_GROUPS:
            n = nh * 2 * a * q

            def xv(tl, f):
                return tl[:, c0:c0 + n].rearrange("p (h a f q) -> p h a f q", a=a, f=2, q=q)[:, :, :, f, :]

            def tv(off):
                return T[:, off:off + a * q].rearrange("p (a q) -> p a q", a=a).unsqueeze(1).to_broadcast([128, nh, a, q])

            def tmpv(tl):
                return tl[:, 0:n // 2].rearrange("p (h a q) -> p h a q", a=a, q=q)

            em.V(lambda e: e.tensor_tensor(out=tmpv(T1), in0=xv(PSB, 0), in1=tv(co), op=ALU.mult), r=[PSB, T], w=[T1])
            em.G(lambda e: e.tensor_tensor(out=tmpv(T2), in0=xv(PSB, 1), in1=tv(so), op=ALU.mult), r=[PSB, T], w=[T2])
            em.V(lambda e: e.tensor_tensor(out=xv(PO_, 0), in0=tmpv(T1), in1=tmpv(T2), op=ALU.subtract), r=[T1, T2], w=[PO_])
            em.G(lambda e: e.tensor_tensor(out=tmpv(T3), in0=xv(PSB, 1), in1=tv(co), op=ALU.mult), r=[PSB, T], w=[T3])
            em.V(lambda e: e.tensor_tensor(out=tmpv(T4), in0=xv(PSB, 0), in1=tv(so), op=ALU.mult), r=[PSB, T], w=[T4])
            em.V(lambda e: e.tensor_tensor(out=xv(PO_, 1), in0=tmpv(T3), in1=tmpv(T4), op=ALU.add), r=[T3, T4], w=[PO_])
        for (c0, nh, d, o0) in NRM_GROUPS:
            n = nh * d
            em.A(lambda e: act(e, SQ[:, 0:n], PO_[:, c0:c0 + n], AF.Square), r=[PO_], w=[SQ])
            em.V(lambda e: e.tensor_reduce(out=NR[:, o0:o0 + nh], in_=SQ[:, 0:n].rearrange("p (h d) -> p h d", d=d), axis=AX.X, op=ALU.add), r=[SQ], w=[NR])
        if b == 0:
            em.V(lambda e: e.tensor_copy(out=RM[:, :], in_=NR[:, :]), r=[NR], w=[RM])
        else:
            em.V(lambda e: e.tensor_tensor(out=RM[:, :], in0=RM[:, :], in1=NR[:, :], op=ALU.max), r=[RM, NR], w=[RM])
        em.dma(C.P.t[b * 128:(b + 1) * 128, :], PO_[:, :], r=[PO_], w=[C.P])
        j = 0
        bi = 0
        while j < 14:
            nn = min(4, 14 - j)
            ps = PS[5 + bi % 2]
            bi += 1
            for jj in range(nn):
                c0 = XT_BLOCKS[j + jj]
                em.P(lambda e: e.transpose(out=ps[:, jj * 128:(jj + 1) * 128], in_=PO_[:, c0:c0 + 128], identity=C.IDN[:, :]), r=[PO_, C.IDN], w=[ps])
            em.A(lambda e: act(e, XTo_[:, j:j + nn, :], ps[:, 0:nn * 128].rearrange("p (a b) -> p a b", b=128), AF.Copy), r=[ps], w=[XTo_])
            j += nn
        em.dma(xt4[:, :, 0, b * 128:(b + 1) * 128], XTo_[0:64, :, :], r=[XTo_], w=[C.XT])
        em.dma(xt4[:, :, 1, b * 128:(b + 1) * 128], XTo_[64:128, :, :], r=[XTo_], w=[C.XT])
        for (c0, nh, h0) in V_GROUPS:
            em.G(lambda e: e.tensor_copy(out=VPo_[:, h0:h0 + nh, 0:64], in_=PO_[:, c0:c0 + nh * 64].rearrange("p (h d) -> p h d", d=64)), r=[PO_], w=[VPo_])
        em.dma(C.VPs.t[b * 128:(b + 1) * 128, :, :], VPo_[:, :, :], r=[VPo_], w=[C.VPs])
    em.P(lambda e: e.transpose(out=PS[0][0:28, 0:128], in_=RM[:, 0:28], identity=C.IDN[:, :]), r=[RM, C.IDN], w=[PS[0]])
    em.V(lambda e: e.tensor_reduce(out=RX[:, 0:1], in_=PS[0][0:28, 0:128], axis=AX.X, op=ALU.max), r=[PS[0]], w=[RX])
    em.P(lambda e: e.transpose(out=PS[1][0:1, 0:28], in_=RX[0:28, 0:1], identity=C.IDN[0:28, 0:28]), r=[RX, C.IDN], w=[PS[1]])
    em.V(lambda e: e.tensor_copy(out=RR[:, :], in_=PS[1][0:1, 0:28]), r=[PS[1]], w=[RR])
    em.P(lambda e: e.matmul(PS[2][:, 0:28], lhsT=C.ONES[0:1, 0:128], rhs=RR[0:1, 0:28], start=True, stop=True), r=[C.ONES, RR], w=[PS[2]])
    em.V(lambda e: e.tensor_copy(out=C.RMB[:, :], in_=PS[2][:, 0:28]), r=[PS[2]], w=[C.RMB])
    em.end_stage()


def stage_attn(C, l, kind, p=0):
    em = C.em
    em.begin_stage()
    PS = C.PS
    Lx, NBx, S = C.Lx, C.NBx, C.S
    need_ctx = l < C.depth - 1
    dh = 32 if kind == "B" else 64
    scale = dh ** -0.5
    nqf = 2 if kind == "B" else 4
    if kind == "A":
        qh0, kh0, vh0, qn0, kn0, OT, oh0 = 0, 4, 0, 0, 4, C.OTA, 0
    elif kind == "B":
        qh0, kh0, vh0, qn0, kn0, OT, oh0 = 6 + 2 * p, 10 + 2 * p, 2 + 2 * p, 6 + 4 * p, 14 + 4 * p, C.OTB, 2 * p
    else:
        qh0, kh0, vh0, qn0, kn0, OT, oh0 = 14, 18, 6, 22, 26, C.OTC, 0
    nout = nqf
    lam_init = 0.8 - 0.6 * math.exp(-0.3 * l)
    GQ = 512

    KB = em.sb("KB", [64, 2, Lx], BF16)
    VB = em.sb("VB", [128, NBx, 2, 65], BF16)
    STG = [em.sb("STG%d" % i, [128, 2080]) for i in range(2)]
    NEGM = em.sb("NEGM", [128, 4])
    QS = [em.sb("QS%d" % i, [64, nqf, GQ]) for i in range(2)]
    QB = [em.sb("QB%d" % i, [64, nqf, GQ], BF16) for i in range(2)]
    PTl = [em.sb("PT%d" % i, [128, 512], BF16) for i in range(3)]
    OU = [em.sb("OU%d" % i, [65, 512]) for i in range(4)]
    RZ = em.sb("RZ", [65, 512])
    OUT = [em.sb("OUT%d" % i, [64, nout, GQ]) for i in range(2)]
    NRM = [em.sb("NRM%d" % i, [64, 512]) for i in range(2)]
    SQ = em.sb("SQ", [64, 512])
    SPS = [PS[0], PS[1], PS[2]]
    OPS = [PS[3], PS[4], PS[5]]
    BPS = [PS[6], PS[7]]
    ONES = C.ONES

    if kind == "B":
        em.V(lambda e: e.tensor_tensor(out=NEGM[:, :], in0=C.RMB[:, qn0:qn0 + 4], in1=C.RMB[:, kn0:kn0 + 4], op=ALU.mult), r=[C.RMB], w=[NEGM])
    else:
        em.V(lambda e: e.tensor_tensor(out=NEGM[:, :].rearrange("p (a b) -> p a b", b=2), in0=C.RMB[:, qn0:qn0 + 4].rearrange("p (a b) -> p a b", b=2),
                                       in1=C.RMB[:, kn0:kn0 + 2].unsqueeze(2).to_broadcast([128, 2, 2]), op=ALU.mult), r=[C.RMB], w=[NEGM])
    em.A(lambda e: act(e, NEGM[:, :], NEGM[:, :], AF.Sqrt), r=[NEGM], w=[NEGM])
    em.V(lambda e: e.tensor_scalar(out=NEGM[:, :], in0=NEGM[:, :], scalar1=-scale, scalar2=None, op0=ALU.mult), r=[NEGM], w=[NEGM])
    if kind == "C":
        SK = em.sb("SK", [128, 4])
        ES = em.sb("ES", [128, 4])
        MKB = em.sb("MKB", [128, 6, 512], BF16)
        load_bcast_row(em, SK, C.win_sink.t[l, :], 4)
        em.V(lambda e: e.tensor_tensor(out=ES[:, :], in0=SK[:, :], in1=NEGM[:, :], op=ALU.add), r=[SK, NEGM], w=[ES])
        em.A(lambda e: act(e, ES[:, :], ES[:, :], AF.Exp), r=[ES], w=[ES])
        for t in range(6):
            stg = STG[t % 2]
            em.dma(stg[:, 0:512], C.mk.t[t, :, :], r=[], w=[stg])
            em.V(lambda e: e.tensor_copy(out=MKB[:, t, :], in_=stg[:, 0:512]), r=[stg], w=[MKB])
    if kind == "B":
        LP = em.sb("LP", [1, 128])
        LS = em.sb("LS", [1, 4])
        NEGLAM = em.sb("NEGLAM", [64, 1])
        SUBL = em.sb("SUBL", [64, 1])
        em.dma(LP[:, :], C.diff_lambda.t[l, :].partition_broadcast(1), r=[], w=[LP])
        lpv = LP[0:1, :].rearrange("p (a b d) -> p a b d", a=2, b=2)
        em.V(lambda e: e.tensor_tensor(out=lpv[:, :, 0, :], in0=lpv[:, :, 0, :], in1=lpv[:, :, 1, :], op=ALU.mult), r=[LP], w=[LP])
        em.V(lambda e: e.tensor_reduce(out=LS[:, 0:2], in_=lpv[:, :, 0, :], axis=AX.X, op=ALU.add), r=[LP], w=[LS])
        em.A(lambda e: act(e, LS[:, 0:2], LS[:, 0:2], AF.Exp), r=[LS], w=[LS])
        em.V(lambda e: e.tensor_tensor(out=LS[:, 2:3], in0=LS[:, 1:2], in1=LS[:, 0:1], op=ALU.subtract), r=[LS], w=[LS])
        em.V(lambda e: e.tensor_scalar(out=LS[:, 2:3], in0=LS[:, 2:3], scalar1=-float(lam_init), scalar2=None, op0=ALU.add), r=[LS], w=[LS])
        em.P(lambda e: e.matmul(BPS[1][0:64, 0:1], lhsT=ONES[0:1, 0:64], rhs=LS[0:1, 2:3], start=True, stop=True), r=[ONES, LS], w=[BPS[1]])
        em.V(lambda e: e.tensor_copy(out=NEGLAM[:, :], in_=BPS[1][0:64, 0:1]), r=[BPS[1]], w=[NEGLAM])
        em.dma(SUBL[:, :], C.diff_subln.t[l, :].rearrange("(p o) -> p o", o=1), r=[], w=[SUBL])
        em.V(lambda e: e.tensor_scalar(out=SUBL[:, :], in0=SUBL[:, :], scalar1=1.0 - float(lam_init), scalar2=None, op0=ALU.mult), r=[SUBL], w=[SUBL])

    i = 0
    for f in range(2):
        c = 0
        while c < Lx:
            cw = min(2048, Lx - c)
            stg = STG[i % 2]
            em.dma(stg[0:64, 0:cw], C.XT.t[:, kh0 + f, c:c + cw], r=[], w=[stg])
            if i % 2 == 0:
                em.A(lambda e: act(e, KB[:, f, c:c + cw], stg[0:64, 0:cw], AF.Copy), r=[stg], w=[KB])
            else:
                em.V(lambda e: e.tensor_copy(out=KB[:, f, c:c + cw], in_=stg[0:64, 0:cw]), r=[stg], w=[KB])
            c += cw
            i += 1
    t0 = 0
    vpv = C.VPs.t[:, vh0:vh0 + 2, :].rearrange("(t p) v c -> p t v c", p=128)
    while t0 < NBx:
        tn = min(16, NBx - t0)
        stg = STG[i % 2]
        sv = stg[:, 0:tn * 130].rearrange("p (t v c) -> p t v c", v=2, c=65)
        em.dma(sv, vpv[:, t0:t0 + tn, :, :], r=[], w=[stg])
        if i % 2 == 0:
            em.A(lambda e: act(e, VB[:, t0:t0 + tn, :, :], sv, AF.Copy), r=[stg], w=[VB])
        else:
            em.V(lambda e: e.tensor_copy(out=VB[:, t0:t0 + tn, :, :], in_=sv), r=[stg], w=[VB])
        t0 += tn
        i += 1

    groups = []
    for g in range(S // GQ):
        if kind == "C":
            tl = [(0, None), (1, None)]
            for tp in range(6):
                blk = 4 * g - 1 + tp
                if 0 <= blk < S // 128:
                    tl.append((2 + blk, tp))
        else:
            tl = [(t, None) for t in range(NBx)]
        groups.append((NCTX + g * GQ, GQ, tl))
    if need_ctx:
        groups.append((0, NCTX, [(0, None), (1, None)]))

    cnt = {"s": 0, "p": 0, "o": 0, "b": 0}
    for gi, (q0, gq, tl) in enumerate(groups):
        qs, qb, out_t = QS[gi % 2], QB[gi % 2], OUT[gi % 2]
        em.dma(qs[:, :, 0:gq], C.XT.t[:, qh0:qh0 + nqf, q0:q0 + gq], r=[], w=[qs])
        em.V(lambda e: e.tensor_copy(out=qb[:, :, 0:gq], in_=qs[:, :, 0:gq]), r=[qs], w=[qb])
        for u in range(4):
            if kind == "B":
                r0, qf, kf = (u % 2) * 32, u // 2, u // 2
            else:
                r0, qf, kf = 0, u, u // 2
            r1 = r0 + dh

            def emit_s(ti):
                sps = SPS[cnt["s"] % 3]
                cnt["s"] += 1
                t = tl[ti][0]
                em.P(lambda e: e.matmul(sps[:, 0:gq], lhsT=KB[r0:r1, kf, t * 128:(t + 1) * 128], rhs=qb[r0:r1, qf, 0:gq], start=True, stop=True),
                     r=[KB, qb], w=[sps])
                return sps

            ops = OPS[cnt["o"] % 3]
            cnt["o"] += 1
            nxt = emit_s(0)
            for ti in range(len(tl)):
                sps = nxt
                if ti + 1 < len(tl):
                    nxt = emit_s(ti + 1)
                t, mi = tl[ti]
                pt = PTl[cnt["p"] % 3]
                cnt["p"] += 1
                em.A(lambda e: act(e, pt[:, 0:gq], sps[:, 0:gq], AF.Exp, scale=scale, bias=NEGM[:, u:u + 1]), r=[sps, NEGM], w=[pt])
                if mi is not None:
                    em.V(lambda e: e.tensor_tensor(out=pt[:, 0:gq], in0=pt[:, 0:gq], in1=MKB[:, mi, 0:gq], op=ALU.mult), r=[pt, MKB], w=[pt])
                em.P(lambda e: e.matmul(ops[0:65, 0:gq], lhsT=VB[:, t, kf, :], rhs=pt[:, 0:gq], start=(ti == 0), stop=(ti == len(tl) - 1)),
                     r=[VB, pt], w=[ops])
            ou = OU[u]
            em.V(lambda e: e.tensor_copy(out=ou[0:65, 0:gq], in_=ops[0:65, 0:gq]), r=[ops], w=[ou])

        def normalize(u, dst_ap, dst_tl):
            ou = OU[u]
            if kind == "C":
                em.V(lambda e: e.tensor_scalar(out=RZ[64:65, 0:gq], in0=ou[64:65, 0:gq], scalar1=ES[64:65, u:u + 1], scalar2=None, op0=ALU.add),
                     r=[ou, ES], w=[RZ])
                em.V(lambda e: e.reciprocal(out=RZ[64:65, 0:gq], in_=RZ[64:65, 0:gq]), r=[RZ], w=[RZ])
            else:
                em.V(lambda e: e.reciprocal(out=RZ[64:65, 0:gq], in_=ou[64:65, 0:gq]), r=[ou], w=[RZ])
            bps = BPS[cnt["b"] % 2]
            cnt["b"] += 1
            em.P(lambda e: e.matmul(bps[0:64, 0:gq], lhsT=ONES[64:65, 0:64], rhs=RZ[64:65, 0:gq], start=True, stop=True), r=[ONES, RZ], w=[bps])
            em.V(lambda e: e.tensor_tensor(out=dst_ap, in0=ou[0:64, 0:gq], in1=bps[0:64, 0:gq], op=ALU.mult), r=[ou, bps], w=[dst_tl])

        if kind != "B":
            for u in range(4):
                normalize(u, out_t[:, u, 0:gq], out_t)
        else:
            for hl in range(2):
                normalize(2 * hl, NRM[0][:, 0:gq], NRM[0])
                normalize(2 * hl + 1, NRM[1][:, 0:gq], NRM[1])
                em.V(lambda e: e.scalar_tensor_tensor(out=NRM[0][:, 0:gq], in0=NRM[1][:, 0:gq], scalar=NEGLAM[:, 0:1], in1=NRM[0][:, 0:gq],
                                                      op0=ALU.mult, op1=ALU.add), r=[NRM[0], NRM[1], NEGLAM], w=[NRM[0]])
                em.A(lambda e: act(e, SQ[:, 0:gq], NRM[0][:, 0:gq], AF.Square), r=[NRM[0]], w=[SQ])
                bps = BPS[cnt["b"] % 2]
                cnt["b"] += 1
                em.P(lambda e: e.matmul(bps[0:64, 0:gq], lhsT=ONES[0:64, 0:64], rhs=SQ[:, 0:gq], start=True, stop=True), r=[ONES, SQ], w=[bps])
                em.V(lambda e: e.tensor_scalar(out=SQ[:, 0:gq], in0=bps[0:64, 0:gq], scalar1=1.0 / 64, scalar2=RMS_EPS, op0=ALU.mult, op1=ALU.add),
                     r=[bps], w=[SQ])
                em.A(lambda e: act(e, SQ[:, 0:gq], SQ[:, 0:gq], AF.Sqrt), r=[SQ], w=[SQ])
                em.V(lambda e: e.reciprocal(out=SQ[:, 0:gq], in_=SQ[:, 0:gq]), r=[SQ], w=[SQ])
                em.V(lambda e: e.tensor_tensor(out=NRM[0][:, 0:gq], in0=NRM[0][:, 0:gq], in1=SQ[:, 0:gq], op=ALU.mult), r=[NRM[0], SQ], w=[NRM[0]])
                em.V(lambda e: e.tensor_scalar(out=out_t[:, hl, 0:gq], in0=NRM[0][:, 0:gq], scalar1=SUBL[:, 0:1], scalar2=None, op0=ALU.mult),
                     r=[NRM[0], SUBL], w=[out_t])
        em.dma(OT.t[:, oh0:oh0 + nout, q0:q0 + gq], out_t[:, :, 0:gq], r=[out_t], w=[OT])
    em.end_stage()


def stage_ret(C, l, f):
    em = C.em
    em.begin_stage()
    PS = C.PS
    NBx = C.NBx
    RC = em.sb("RC", [128, 5, 128])
    PIDX = em.sb("PIDX", [128, 2])
    DC = em.sb("DC", [128, 8])
    LG = em.sb("LG", [128, 8])
    NLG = em.sb("NLG", [128, 8])
    DT = em.sb("DT", [128, 4, 128])
    XI = em.sb("XI", [64, 4, 128])
    ZE = em.sb("ZE", [128, 4])
    GC = em.sb("GC", [128, 4])
    ST = em.sb("ST", [64, 4, 64])
    QTc = [em.sb("QTc%d" % i, [64, 4, 128]) for i in range(2)]
    KTc = [em.sb("KTc%d" % i, [64, 4, 128]) for i in range(2)]
    KV = [em.sb("KV%d" % i, [128, 768]) for i in range(2)]
    OFc = [em.sb("OFc%d" % i, [128, 256]) for i in range(2)]
    QX = em.sb("QX", [64, 4, 128])
    KZ = em.sb("KZ", [128, 4, 64])
    INM = em.sb("INM", [128, 512])
    OO = [em.sb("OO%d" % i, [128, 256]) for i in range(2)]
    XN = em.sb("XN", [128, 256])
    SG = em.sb("SG", [128, 256])
    BST = em.sb("BST", [128, 24])
    MV = em.sb("MV", [128, 8])
    RS = em.sb("RS", [128, 4])
    N0 = em.sb("N0", [128, 256])
    N1 = em.sb("N1", [128, 256])

    em.dma(RC[:, :, :], C.rc.t[:, :, :], r=[], w=[RC])
    em.dma(PIDX[:, :], C.pidx.t[:, :], r=[], w=[PIDX])
    load_bcast_row(em, DC, C.ret_decay.t[l, :], 8)
    load_bcast_row(em, N0, C.ret_norm.t[l, 0, :], 256)
    load_bcast_row(em, N1, C.ret_norm.t[l, 1, :], 256)
    em.A(lambda e: act(e, LG[:, :], DC[:, :], AF.Exp, scale=-1.0), r=[DC], w=[LG])
    em.V(lambda e: e.tensor_scalar(out=LG[:, :], in0=LG[:, :], scalar1=1.0, scalar2=None, op0=ALU.add), r=[LG], w=[LG])
    em.A(lambda e: act(e, NLG[:, :], LG[:, :], AF.Ln), r=[LG], w=[NLG])
    em.V(lambda e: e.tensor_scalar(out=LG[:, :], in0=NLG[:, :], scalar1=-1.0, scalar2=None, op0=ALU.mult), r=[NLG], w=[LG])
    for h in range(4):
        col = f * 4 + h
        scl = LG if f == 0 else NLG
        em.A(lambda e: act(e, DT[:, h, :], RC[:, 0, :], AF.Exp, scale=scl[:, col:col + 1]), r=[RC, scl], w=[DT])
        em.V(lambda e: e.scalar_tensor_tensor(out=DT[:, h, :], in0=DT[:, h, :], scalar=0.125, in1=RC[:, 1 + f, :], op0=ALU.mult, op1=ALU.mult),
             r=[DT, RC], w=[DT])
        em.A(lambda e: act(e, XI[:, h, :], RC[0:64, 3 + f, :], AF.Exp, scale=LG[0:64, col:col + 1]), r=[RC, LG], w=[XI])
        em.A(lambda e: act(e, ZE[:, h:h + 1], PIDX[:, f:f + 1], AF.Exp, scale=LG[:, col:col + 1]), r=[PIDX, LG], w=[ZE])
    em.V(lambda e: e.tensor_scalar(out=ZE[:, :], in0=ZE[:, :], scalar1=0.125, scalar2=None, op0=ALU.mult), r=[ZE], w=[ZE])
    em.A(lambda e: act(e, GC[:, :], LG[:, f * 4:f * 4 + 4], AF.Exp, scale=128.0), r=[LG], w=[GC])
    em.V(lambda e: e.memset(ST[:, :, :], 0.0), w=[ST])

    order = list(range(NBx)) if f == 0 else [1, 0] + list(range(NBx - 1, 1, -1))
    for it, c in enumerate(order):
        qt_, kt_, kv, ofc, oo = QTc[it % 2], KTc[it % 2], KV[it % 2], OFc[it % 2], OO[it % 2]
        cs = slice(c * 128, (c + 1) * 128)
        em.dma(qt_[:, :, :], C.XT.t[:, 20:24, cs], r=[], w=[qt_])
        em.dma(kt_[:, :, :], C.XT.t[:, 24:28, cs], r=[], w=[kt_])
        em.dma(kv[:, :], C.P.t[cs, 2048:2816], r=[], w=[kv])
        if f == 1:
            em.dma(ofc[:, :], C.ODF.t[cs, :], r=[], w=[ofc])
        em.V(lambda e: e.tensor_tensor(out=QX[:, :, :], in0=qt_[:, :, :], in1=XI[:, :, :], op=ALU.mult), r=[qt_, XI], w=[QX])
        em.G(lambda e: e.tensor_tensor(out=KZ[:, :, :], in0=kv[:, 0:256].rearrange("p (h d) -> p h d", d=64),
                                       in1=ZE[:, 0:4].unsqueeze(2).to_broadcast([128, 4, 64]), op=ALU.mult), r=[kv, ZE], w=[KZ])
        for h in range(4):
            em.P(lambda e: e.matmul(PS[0][:, h * 128:(h + 1) * 128], lhsT=kt_[:, h, :], rhs=qt_[:, h, :], start=True, stop=True), r=[kt_, qt_], w=[PS[0]])
        em.V(lambda e: e.tensor_tensor(out=INM[:, :], in0=PS[0][:, :], in1=DT[:, :, :].rearrange("p h i -> p (h i)"), op=ALU.mult), r=[PS[0], DT], w=[INM])
        for h in range(4):
            em.P(lambda e: e.matmul(PS[1][:, h * 64:(h + 1) * 64], lhsT=INM[:, h * 128:(h + 1) * 128], rhs=kv[:, 256 + h * 64:256 + (h + 1) * 64],
                                    start=True, stop=False), r=[INM, kv], w=[PS[1]])
            em.P(lambda e: e.matmul(PS[1][:, h * 64:(h + 1) * 64], lhsT=QX[:, h, :], rhs=ST[:, h, :], start=False, stop=True), r=[QX, ST], w=[PS[1]])
        for h in range(4):
            em.P(lambda e: e.matmul(PS[2][0:64, h * 64:(h + 1) * 64], lhsT=KZ[:, h, :], rhs=kv[:, 256 + h * 64:256 + (h + 1) * 64], start=True, stop=True),
                 r=[KZ, kv], w=[PS[2]])
        em.V(lambda e: e.tensor_tensor(out=ST[:, :, :], in0=ST[:, :, :], in1=GC[0:64, 0:4].unsqueeze(2).to_broadcast([64, 4, 64]), op=ALU.mult),
             r=[ST, GC], w=[ST])
        em.V(lambda e: e.tensor_tensor(out=ST[:, :, :].rearrange("p h d -> p (h d)"), in0=ST[:, :, :].rearrange("p h d -> p (h d)"), in1=PS[2][0:64, 0:256], op=ALU.add),
             r=[ST, PS[2]], w=[ST])
        if f == 0:
            em.A(lambda e: act(e, oo[:, :], PS[1][:, 0:256], AF.Copy), r=[PS[1]], w=[oo])
            em.dma(C.ODF.t[cs, :], oo[:, :], r=[oo], w=[C.ODF])
        else:
            em.V(lambda e: e.tensor_tensor(out=oo[:, :], in0=PS[1][:, 0:256], in1=ofc[:, :], op=ALU.add), r=[PS[1], ofc], w=[oo])
            for h in range(4):
                em.V(lambda e: e.bn_stats(out=BST[:, h * 6:(h + 1) * 6], in_=oo[:, h * 64:(h + 1) * 64]), r=[oo], w=[BST])
            for h in range(4):
                em.V(lambda e: e.bn_aggr(out=MV[:, 2 * h:2 * h + 2], in_=BST[:, h * 6:(h + 1) * 6]), r=[BST], w=[MV])
            em.V(lambda e: e.tensor_scalar(out=RS[:, 0:4], in0=MV[:, :].rearrange("p (h t) -> p h t", t=2)[:, :, 1], scalar1=LN_EPS, scalar2=None, op0=ALU.add),
                 r=[MV], w=[RS])
            em.A(lambda e: act(e, RS[:, :], RS[:, :], AF.Sqrt), r=[RS], w=[RS])
            em.V(lambda e: e.reciprocal(out=RS[:, :], in_=RS[:, :]), r=[RS], w=[RS])
            for h in range(4):
                em.V(lambda e: e.tensor_scalar(out=XN[:, h * 64:(h + 1) * 64], in0=oo[:, h * 64:(h + 1) * 64], scalar1=MV[:, 2 * h:2 * h + 1],
                                               scalar2=RS[:, h:h + 1], op0=ALU.subtract, op1=ALU.mult), r=[oo, MV, RS], w=[XN])
            em.G(lambda e: e.tensor_tensor(out=XN[:, :], in0=XN[:, :], in1=N0[:, :], op=ALU.mult), r=[XN, N0], w=[XN])
            em.G(lambda e: e.tensor_tensor(out=XN[:, :], in0=XN[:, :], in1=N1[:, :], op=ALU.add), r=[XN, N1], w=[XN])
            em.A(lambda e: act(e, SG[:, :], kv[:, 512:768], AF.Silu), r=[kv], w=[SG])
            em.V(lambda e: e.tensor_tensor(out=oo[:, :], in0=XN[:, :], in1=SG[:, :], op=ALU.mult), r=[XN, SG], w=[oo])
            em.dma(C.ODO.t[cs, :], oo[:, :], r=[oo], w=[C.ODO])
    em.end_stage()


def stage_merge(C, l):
    em = C.em
    em.begin_stage()
    PS = C.PS
    need_ctx = l < C.depth - 1
    Wg = em.sb("Wg", [128, 8, 4096], BF16)
    Wb = em.sb("Wb", [128, 8, D], BF16)
    Wo = em.sb("Wo", [128, 8, D], BF16)
    STG = [em.sb("STG%d" % i, [128, 1024]) for i in range(2)]
    MR = [[em.sb("MR%d_%d" % (ty, i), [128, D]) for i in range(3)] for ty in range(2)]
    LNG = em.sb("LNG", [128, D])
    LNB = em.sb("LNB", [128, D])
    HB = [em.sb("HB%d" % i, [128, D]) for i in range(2)]
    OTt = [em.sb("OTt%d" % i, [128, 6, 128]) for i in range(2)]
    ODt = [em.sb("ODt%d" % i, [128, 256]) for i in range(2)]
    OTb = em.sb("OTb", [128, 8, 128], BF16)
    TMP = em.sb("TMP", [128, D])
    U = em.sb("U", [128, D])
    UT = em.sb("UT", [128, 8, 128], BF16)
    GS = [em.sb("GS%d" % i, [128, 512]) for i in range(2)]
    TH = em.sb("TH", [128, 512])
    M = em.sb("M", [128, D])
    MT = em.sb("MT", [128, 8, 128], BF16)
    R = em.sb("R", [128, D])
    HO = [em.sb("HO%d" % i, [128, D]) for i in range(2)]
    BST = em.sb("BST", [128, 12])
    MV = em.sb("MV", [128, 2])
    RS = em.sb("RS", [128, 1])

    load_mod_rows(C, l, [0, 1, 2], MR, plus1=(1,))
    load_bcast_row(em, LNG, C.ln_attn.t[l, 0, :], D)
    load_bcast_row(em, LNB, C.ln_attn.t[l, 1, :], D)
    load_weight_bf16(em, Wg, C.w_in.t[l], 8, 4096, STG, col0=MIX)
    load_weight_bf16(em, Wb, C.w_branch.t[l], 8, D, STG)
    load_weight_bf16(em, Wo, C.w_out.t[l], 8, D, STG)

    gi = 0
    for b in range(0 if need_ctx else 2, C.NBx):
        ty = 1 if b < 2 else 0
        cs = slice(b * 128, (b + 1) * 128)
        H, ott, odt, ho = HB[b % 2], OTt[b % 2], ODt[b % 2], HO[b % 2]
        em.dma(H[:, :], hrows(C, l, b), r=[], w=[H])
        for bi, OT in enumerate((C.OTA, C.OTB, C.OTC)):
            otv = OT.t.rearrange("p (j e) l -> p j e l", e=2)
            em.dma(ott[0:64, 2 * bi:2 * bi + 2, :], otv[:, :, 0, cs], r=[], w=[ott])
            em.dma(ott[64:128, 2 * bi:2 * bi + 2, :], otv[:, :, 1, cs], r=[], w=[ott])
        em.dma(odt[:, :], C.ODO.t[cs, :], r=[], w=[odt])
        em.A(lambda e: act(e, OTb[:, 0:6, :], ott[:, :, :], AF.Copy), r=[ott], w=[OTb])
        for j in range(2):
            em.P(lambda e: e.transpose(out=PS[0][:, j * 128:(j + 1) * 128], in_=odt[:, j * 128:(j + 1) * 128], identity=C.IDN[:, :]), r=[odt, C.IDN], w=[PS[0]])
        em.V(lambda e: e.tensor_copy(out=OTb[:, 6:8, :], in_=PS[0][:, 0:256].rearrange("p (a b) -> p a b", b=128)), r=[PS[0]], w=[OTb])
        emit_modulate(em, U, H, MR[ty][1], MR[ty][0], TMP)
        emit_transpose8(em, UT, U, C.IDN, PS[0], PS[1])
        for c in range(2):
            for i in range(4):
                pg, pb, gs = PS[2 + gi % 2], PS[4 + gi % 2], GS[gi % 2]
                gi += 1
                col = i * 1024 + c * 512
                for k in range(8):
                    em.P(lambda e: e.matmul(pg[:, :], lhsT=UT[:, k, :], rhs=Wg[:, k, col:col + 512], start=(k == 0), stop=(k == 7)), r=[UT, Wg], w=[pg])
                em.A(lambda e: act(e, gs[:, :], pg[:, :], AF.Sigmoid), r=[pg], w=[gs])
                for kk in range(2):
                    em.P(lambda e: e.matmul(pb[:, :], lhsT=OTb[:, 2 * i + kk, :], rhs=Wb[:, 2 * i + kk, c * 512:(c + 1) * 512], start=(kk == 0), stop=(kk == 1)),
                         r=[OTb, Wb], w=[pb])
                if i == 0:
                    em.V(lambda e: e.tensor_tensor(out=M[:, c * 512:(c + 1) * 512], in0=gs[:, :], in1=pb[:, :], op=ALU.mult), r=[gs, pb], w=[M])
                else:
                    em.V(lambda e: e.tensor_tensor(out=TH[:, :], in0=gs[:, :], in1=pb[:, :], op=ALU.mult), r=[gs, pb], w=[TH])
                    em.G(lambda e: e.tensor_tensor(out=M[:, c * 512:(c + 1) * 512], in0=M[:, c * 512:(c + 1) * 512], in1=TH[:, :], op=ALU.add), r=[M, TH], w=[M])
        emit_transpose8(em, MT, M, C.IDN, PS[0], PS[1])
        for c in range(2):
            py = PS[6 + c]
            for k in range(8):
                em.P(lambda e: e.matmul(py[:, :], lhsT=MT[:, k, :], rhs=Wo[:, k, c * 512:(c + 1) * 512], start=(k == 0), stop=(k == 7)), r=[MT, Wo], w=[py])
            em.V(lambda e: e.tensor_tensor(out=R[:, c * 512:(c + 1) * 512], in0=py[:, :], in1=MR[ty][2][:, c * 512:(c + 1) * 512], op=ALU.mult), r=[py, MR[ty][2]], w=[R])
        em.V(lambda e: e.scalar_tensor_tensor(out=R[:, :], in0=H[:, :], scalar=float(ALPHA), in1=R[:, :], op0=ALU.mult, op1=ALU.add), r=[H, R], w=[R])
        emit_layernorm(em, ho, R, LNG, LNB, BST, MV, RS, TMP)
        em.dma(C.H1.t[cs, :], ho[:, :], r=[ho], w=[C.H1])
    em.end_stage()


def stage_peer(C, l):
    em = C.em
    em.begin_stage()
    PS = C.PS
    need_ctx = l < C.depth - 1
    last = l == C.depth - 1
    Wq = em.sb("Wq", [128, 8, 2048])
    SKT = em.sb("SKT", [128, 16, 128])
    MR = [[em.sb("MR%d_%d" % (ty, i), [128, D]) for i in range(3)] for ty in range(2)]
    LNG = em.sb("LNG", [128, D])
    LNB = em.sb("LNB", [128, D])
    I16 = em.sb("I16", [128, 32])
    HB = [em.sb("HB%d" % i, [128, D]) for i in range(2)]
    TMP = em.sb("TMP", [128, D])
    X = em.sb("X", [128, D])
    XTt = em.sb("XTt", [128, 8, 128])
    QTt = em.sb("QTt", [128, 16, 128])
    SC = em.sb("SC", [128, 16, 128])
    SC2 = SC
    SV = em.sb("SV", [128, 16, 16])
    SI = em.sb("SI", [128, 16, 16], U32)
    SIF = em.sb("SIF", [128, 16, 16])
    CD = em.sb("CD", [128, 8, 256])
    CD2 = CD
    FV = em.sb("FV", [128, 8, 16])
    FP = em.sb("FP", [128, 8, 16], U32)
    PF = em.sb("PF", [128, 8, 16])
    PI = em.sb("PI", [128, 8, 16])
    PJ = em.sb("PJ", [128, 8, 16])
    EQ = em.sb("EQ", [128, 8, 16, 16])
    EI = em.sb("EI", [128, 8, 16])
    EJ = em.sb("EJ", [128, 8, 16])
    IDX = em.sb("IDX", [128, 128], I32)
    GT = em.sb("GT", [128, 8, 16])
    GSM = em.sb("GSM", [128, 8])
    AV = em.sb("AV", [128, 128])
    A2 = em.sb("A2", [128, 128])
    WT = em.sb("WT", [128, 128])
    JK = em.sb("JK", [128, D])
    GB = [em.sb("GB%d" % i, [128, D]) for i in range(4)]
    Y = em.sb("Y", [128, D])
    R = em.sb("R", [128, D])
    HO = [em.sb("HO0", [128, D])] * 2
    BST = em.sb("BST", [128, 12])
    MV = em.sb("MV", [128, 2])
    RS = em.sb("RS", [128, 1])

    load_mod_rows(C, l, [3, 4, 5], MR, plus1=(4,))
    load_bcast_row(em, LNG, C.ln_ffn.t[l, 0, :], D)
    load_bcast_row(em, LNB, C.ln_ffn.t[l, 1, :], D)
    load_bcast_row(em, I16, C.iota16.t[:], 32)
    em.dma(Wq[:, :, :], C.peer_wq.t[l].rearrange("(k p) c -> p k c", p=128), r=[], w=[Wq])
    em.dma(SKT[:, :, :], C.skT.t[l], r=[], w=[SKT])
    if not getattr(C, "route_only", False):
        ut, vt = C.peer_u[l].t[:, :], C.peer_v[l].t[:, :]
    gbi = 0
    for b in range(0 if need_ctx else 2, C.NBx):
        ty = 1 if b < 2 else 0
        cs = slice(b * 128, (b + 1) * 128)
        H, ho = HB[b % 2], HO[b % 2]
        em.dma(H[:, :], C.H1.t[cs, :], r=[], w=[H])
        emit_modulate(em, X, H, MR[ty][1], MR[ty][0], TMP)
        emit_transpose8(em, XTt, X, C.IDN, PS[0], PS[1])
        for c4 in range(4):
            ps = PS[2 + c4 % 2]
            for cc in range(4):
                c = c4 * 4 + cc
                for k in range(8):
                    em.P(lambda e: e.matmul(ps[:, cc * 128:(cc + 1) * 128], lhsT=Wq[:, k, c * 128:(c + 1) * 128], rhs=XTt[:, k, :], start=(k == 0), stop=(k == 7)),
                         r=[Wq, XTt], w=[ps])
            em.A(lambda e: act(e, QTt[:, c4 * 4:c4 * 4 + 4, :], ps[:, :].rearrange("p (a b) -> p a b", b=128), AF.Copy), r=[ps], w=[QTt])
        for c4 in range(4):
            ps = PS[4 + c4 % 2]
            for cc in range(4):
                c = c4 * 4 + cc
                em.P(lambda e: e.matmul(ps[:, cc * 128:(cc + 1) * 128], lhsT=QTt[:, c, :], rhs=SKT[:, c, :], start=True, stop=True), r=[QTt, SKT], w=[ps])
            em.A(lambda e: act(e, SC[:, c4 * 4:c4 * 4 + 4, :], ps[:, :].rearrange("p (a b) -> p a b", b=128), AF.Copy), r=[ps], w=[SC])
        for c in range(16):
            em.V(lambda e: e.max(out=SV[:, c, 0:8], in_=SC[:, c, :]), r=[SC], w=[SV])
            em.V(lambda e: e.max_index(out=SI[:, c, 0:8], in_max=SV[:, c, 0:8], in_values=SC[:, c, :]), r=[SV, SC], w=[SI])
            em.V(lambda e: e.match_replace(out=SC2[:, c, :], in_to_replace=SV[:, c, 0:8], in_values=SC[:, c, :], imm_value=-1e30), r=[SV, SC], w=[SC2])
            em.V(lambda e: e.max(out=SV[:, c, 8:16], in_=SC2[:, c, :]), r=[SC2], w=[SV])
            em.V(lambda e: e.max_index(out=SI[:, c, 8:16], in_max=SV[:, c, 8:16], in_values=SC2[:, c, :]), r=[SV, SC2], w=[SI])
        em.V(lambda e: e.tensor_copy(out=SIF[:, :, :], in_=SI[:, :, :]), r=[SI], w=[SIF])
        svv = SV[:, :, :].rearrange("p (h t) k -> p h t k", t=2)
        sfv = SIF[:, :, :].rearrange("p (h t) k -> p h t k", t=2)
        cdv = CD[:, :, :].rearrange("p h (i j) -> p h i j", j=16)
        for h in range(8):
            em.V(lambda e: e.tensor_tensor(out=cdv[:, h, :, :], in0=svv[:, h, 0, :].unsqueeze(2).to_broadcast([128, 16, 16]),
                                           in1=svv[:, h, 1, :].unsqueeze(1).to_broadcast([128, 16, 16]), op=ALU.add), r=[SV], w=[CD])
        for h in range(8):
            em.V(lambda e: e.max(out=FV[:, h, 0:8], in_=CD[:, h, :]), r=[CD], w=[FV])
            em.V(lambda e: e.max_index(out=FP[:, h, 0:8], in_max=FV[:, h, 0:8], in_values=CD[:, h, :]), r=[FV, CD], w=[FP])
            em.V(lambda e: e.match_replace(out=CD2[:, h, :], in_to_replace=FV[:, h, 0:8], in_values=CD[:, h, :], imm_value=-1e30), r=[FV, CD], w=[CD2])
            em.V(lambda e: e.max(out=FV[:, h, 8:16], in_=CD2[:, h, :]), r=[CD2], w=[FV])
            em.V(lambda e: e.max_index(out=FP[:, h, 8:16], in_max=FV[:, h, 8:16], in_values=CD2[:, h, :]), r=[FV, CD2], w=[FP])
        em.V(lambda e: e.tensor_copy(out=PF[:, :, :], in_=FP[:, :, :]), r=[FP], w=[PF])
        i16b = I16[:, 16:32].unsqueeze(1).unsqueeze(1).to_broadcast([128, 8, 16, 16])
        i1b = I16[:, 0:16].unsqueeze(1).unsqueeze(1).to_broadcast([128, 8, 16, 16])
        em.V(lambda e: e.tensor_tensor(out=EQ[:, :, :, :], in0=PF[:, :, :].unsqueeze(3).to_broadcast([128, 8, 16, 16]), in1=i16b, op=ALU.is_ge), r=[PF, I16], w=[EQ])
        em.V(lambda e: e.tensor_reduce(out=PI[:, :, :], in_=EQ[:, :, :, :], axis=AX.X, op=ALU.add), r=[EQ], w=[PI])
        em.V(lambda e: e.tensor_scalar(out=PI[:, :, :], in0=PI[:, :, :], scalar1=-1.0, scalar2=None, op0=ALU.add), r=[PI], w=[PI])
        em.V(lambda e: e.scalar_tensor_tensor(out=PJ[:, :, :], in0=PI[:, :, :], scalar=-16.0, in1=PF[:, :, :], op0=ALU.mult, op1=ALU.add), r=[PI, PF], w=[PJ])
        for (PX, t, EX) in ((PI, 0, EI), (PJ, 1, EJ)):
            em.V(lambda e: e.tensor_tensor(out=EQ[:, :, :, :], in0=PX[:, :, :].unsqueeze(3).to_broadcast([128, 8, 16, 16]), in1=i1b, op=ALU.is_equal), r=[PX, I16], w=[EQ])
            em.V(lambda e: e.tensor_tensor(out=EQ[:, :, :, :], in0=EQ[:, :, :, :], in1=sfv[:, :, t, :].unsqueeze(2).to_broadcast([128, 8, 16, 16]), op=ALU.mult),
                 r=[EQ, SIF], w=[EQ])
            em.V(lambda e: e.tensor_reduce(out=EX[:, :, :], in_=EQ[:, :, :, :], axis=AX.X, op=ALU.add), r=[EQ], w=[EX])
        em.V(lambda e: e.scalar_tensor_tensor(out=EI[:, :, :], in0=EI[:, :, :], scalar=128.0, in1=EJ[:, :, :], op0=ALU.mult, op1=ALU.add), r=[EI, EJ], w=[EI])
        em.V(lambda e: e.tensor_copy(out=IDX[:, :], in_=EI[:, :, :].rearrange("p h k -> p (h k)")), r=[EI], w=[IDX])
        em.V(lambda e: e.tensor_tensor(out=GT[:, :, :], in0=FV[:, :, :], in1=FV[:, :, 0:1].to_broadcast([128, 8, 16]), op=ALU.subtract), r=[FV], w=[GT])
        em.A(lambda e: act(e, GT[:, :, :], GT[:, :, :], AF.Exp), r=[GT], w=[GT])
        em.V(lambda e: e.tensor_reduce(out=GSM[:, :], in_=GT[:, :, :], axis=AX.X, op=ALU.add), r=[GT], w=[GSM])
        em.V(lambda e: e.reciprocal(out=GSM[:, :], in_=GSM[:, :]), r=[GSM], w=[GSM])
        em.V(lambda e: e.tensor_tensor(out=GT[:, :, :], in0=GT[:, :, :], in1=GSM[:, :].unsqueeze(2).to_broadcast([128, 8, 16]), op=ALU.mult), r=[GT, GSM], w=[GT])
        if getattr(C, "route_only", False):
            for (tl_, ap_, o_, n_) in ((SV, SV[:, :, :].rearrange("p a b -> p (a b)"), 0, 256), (SIF, SIF[:, :, :].rearrange("p a b -> p (a b)"), 256, 256),
                                       (FV, FV[:, :, :].rearrange("p a b -> p (a b)"), 512, 128), (PF, PF[:, :, :].rearrange("p a b -> p (a b)"), 640, 128),
                                       (EI, EI[:, :, :].rearrange("p a b -> p (a b)"), 768, 128), (GT, GT[:, :, :].rearrange("p a b -> p (a b)"), 896, 128),
                                       (PI, PI[:, :, :].rearrange("p a b -> p (a b)"), 1024, 128), (PJ, PJ[:, :, :].rearrange("p a b -> p (a b)"), 1152, 128),
                                       (X, X[:, :], 2048, 1024)):
                em.dma(C.pdbg.t[:, o_:o_ + n_], ap_, r=[tl_], w=[C.pdbg])
            break
        for r_ in range(128):
            gb = GB[gbi % 4]
            gbi += 1
            em.dma(None, None, r=[IDX], w=[gb], q="pool",
                   fn=lambda q: q.indirect_dma_start(out=gb[:, :], out_offset=None, in_=ut, in_offset=bass.IndirectOffsetOnAxis(ap=IDX[:, r_:r_ + 1], axis=0), bounds_check=C.breg, oob_is_err=False))
            em.V(lambda e: e.scalar_tensor_tensor(out=JK[:, :], in0=gb[:, :], scalar=1.0, in1=X[:, :], op0=ALU.mult, op1=ALU.mult, accum_out=AV[:, r_:r_ + 1]),
                 r=[gb, X], w=[JK, AV])
        em.V(lambda e: e.tensor_tensor(out=A2[:, :], in0=AV[:, :], in1=AV[:, :], op=ALU.mult), r=[AV], w=[A2])
        em.V(lambda e: e.tensor_tensor(out=A2[:, :], in0=A2[:, :], in1=AV[:, :], op=ALU.mult), r=[A2, AV], w=[A2])
        em.V(lambda e: e.scalar_tensor_tensor(out=A2[:, :], in0=A2[:, :], scalar=0.044715, in1=AV[:, :], op0=ALU.mult, op1=ALU.add), r=[A2, AV], w=[A2])
        em.A(lambda e: act(e, A2[:, :], A2[:, :], AF.Tanh, scale=0.7978845608028654), r=[A2], w=[A2])
        em.V(lambda e: e.scalar_tensor_tensor(out=A2[:, :], in0=A2[:, :], scalar=1.0, in1=AV[:, :], op0=ALU.add, op1=ALU.mult), r=[A2, AV], w=[A2])
        em.V(lambda e: e.scalar_tensor_tensor(out=WT[:, :], in0=A2[:, :], scalar=0.5, in1=GT[:, :, :].rearrange("p h k -> p (h k)"), op0=ALU.mult, op1=ALU.mult),
             r=[A2, GT], w=[WT])
        for r_ in range(128):
            gb = GB[gbi % 4]
            gbi += 1
            em.dma(None, None, r=[IDX], w=[gb], q="pool",
                   fn=lambda q: q.indirect_dma_start(out=gb[:, :], out_offset=None, in_=vt, in_offset=bass.IndirectOffsetOnAxis(ap=IDX[:, r_:r_ + 1], axis=0), bounds_check=C.breg, oob_is_err=False))
            if r_ == 0:
                em.V(lambda e: e.tensor_scalar(out=Y[:, :], in0=gb[:, :], scalar1=WT[:, 0:1], scalar2=None, op0=ALU.mult), r=[gb, WT], w=[Y])
            else:
                em.V(lambda e: e.scalar_tensor_tensor(out=Y[:, :], in0=gb[:, :], scalar=WT[:, r_:r_ + 1], in1=Y[:, :], op0=ALU.mult, op1=ALU.add), r=[gb, WT, Y], w=[Y])
        em.G(lambda e: e.tensor_tensor(out=R[:, :], in0=Y[:, :], in1=MR[ty][2][:, :], op=ALU.mult), r=[Y, MR[ty][2]], w=[R])
        em.V(lambda e: e.scalar_tensor_tensor(out=R[:, :], in0=H[:, :], scalar=float(ALPHA), in1=R[:, :], op0=ALU.mult, op1=ALU.add), r=[H, R], w=[R])
        emit_layernorm(em, ho, R, LNG, LNB, BST, MV, RS, TMP)
        if last:
            em.dma(C.out.t[(b - 2) * 128:(b - 1) * 128, :], ho[:, :], r=[ho], w=[C.out])
        else:
            em.dma(C.H2.t[cs, :], ho[:, :], r=[ho], w=[C.H2])
    em.end_stage()


STAGES = ["mod", "proj", "attnA", "attnB0", "attnB1", "attnC", "retf", "retb", "merge", "peer"]


def build_all(S, depth=DEPTH, debug=False, stop=None):
    nc = bass.Bass("TRN2", target_bir_lowering=False)
    Lx = NCTX + S
    with ExitStack() as st:
        em = Em(nc, st)
        C = Ctx()
        C.em, C.S, C.Lx, C.NBx, C.depth = em, S, Lx, Lx // 128, depth
        C.x = em.dram("x", [S, D])
        C.ctx = em.dram("ctx", [NCTX, D])
        C.cT = em.dram("cT", [D, 2])
        C.w_mod = em.dram("w_mod", [DEPTH, D, 6 * D])
        C.b_mod = em.dram("b_mod", [DEPTH, 6 * D])
        C.w_in = em.dram("w_in", [DEPTH, D, 6912])
        C.gain = em.dram("gain", [DEPTH, 384])
        C.diff_lambda = em.dram("diff_lambda", [DEPTH, 128])
        C.diff_subln = em.dram("diff_subln", [DEPTH, 64])
        C.win_sink = em.dram("win_sink", [DEPTH, 4])
        C.ret_decay = em.dram("ret_decay", [DEPTH, 8])
        C.ret_norm = em.dram("ret_norm", [DEPTH, 2, 256])
        C.w_branch = em.dram("w_branch", [DEPTH, 1024, D])
        C.w_out = em.dram("w_out", [DEPTH, D, D])
        C.ln_attn = em.dram("ln_attn", [DEPTH, 2, D])
        C.ln_ffn = em.dram("ln_ffn", [DEPTH, 2, D])
        C.peer_wq = em.dram("peer_wq", [DEPTH, D, 2048])
        C.skT = em.dram("skT", [DEPTH, 128, 16, 128])
        npeer = DEPTH if stop is None else (stop[0] + 1 if stop[1] == "peer" else stop[0])
        C.peer_u = [em.dram("peer_u%d" % i, [16384, D]) for i in range(npeer)]
        C.peer_v = [em.dram("peer_v%d" % i, [16384, D]) for i in range(npeer)]
        C.ident = em.dram("ident", [128, 128])
        C.tab = em.dram("tab", [Lx, 160])
        C.mk = em.dram("mk", [6, 128, 512])
        C.rc = em.dram("rc", [128, 5, 128])
        C.pidx = em.dram("pidx", [128, 2])
        C.iota16 = em.dram("iota16", [32])
        C.out = em.dram("out", [S, D], kind="ExternalOutput")
        sk = "ExternalOutput" if debug else "Internal"
        C.mod = em.dram("mod", [DEPTH, 2, 6 * D], kind=sk)
        C.P = em.dram("P", [Lx, MIX], kind=sk)
        C.XT = em.dram("XT", [64, 28, Lx], kind=sk)
        C.VPs = em.dram("VPs", [Lx, 8, 65], kind=sk)
        C.OTA = em.dram("OTA", [64, 4, Lx], kind=sk)
        C.OTB = em.dram("OTB", [64, 4, Lx], kind=sk)
        C.OTC = em.dram("OTC", [64, 4, Lx], kind=sk)
        C.ODF = em.dram("ODF", [Lx, 256], kind=sk)
        C.ODO = em.dram("ODO", [Lx, 256], kind=sk)
        C.H1 = em.dram("H1", [Lx, D], kind=sk)
        C.H2 = em.dram("H2", [Lx, D], kind=sk)
        C.route_only = stop is not None and stop[1] == "route"
        if C.route_only:
            C.pdbg = em.dram("pdbg", [128, 4096], kind="ExternalOutput")
        C.PS = [em.ps("PS%d" % i) for i in range(8)]
        C.IDN = em.sb("IDN", [128, 128])
        C.ONES = em.sb("ONES", [128, 128])
        C.RMB = em.sb("RMB", [128, 28])
        em.dma(C.IDN[:, :], C.ident.t[:, :], r=[], w=[C.IDN])
        C.breg = nc.gpsimd.to_reg(16383)
        em.V(lambda e: e.memset(C.ONES[:, :], 1.0), w=[C.ONES])

        def run_stages():
            stage_mod(C)
            if stop == (0, "mod"):
                return
            for l in range(depth):
                seq = [("proj", lambda: stage_proj(C, l)), ("attnA", lambda: stage_attn(C, l, "A")), ("attnB0", lambda: stage_attn(C, l, "B", 0)),
                       ("attnB1", lambda: stage_attn(C, l, "B", 1)), ("attnC", lambda: stage_attn(C, l, "C")), ("retf", lambda: stage_ret(C, l, 0)),
                       ("retb", lambda: stage_ret(C, l, 1)), ("merge", lambda: stage_merge(C, l)), ("route" if C.route_only else "peer", lambda: stage_peer(C, l))]
                for name, fn in seq:
                    fn()
                    if stop == (l, name):
                        return

        run_stages()
        em.finish()
        C.ninst = em.ninst
    nc._ninst = C.ninst
    nc._inputs = list(em.inputs)
    return nc


def host_tables(S):
    theta = np.float32(10000.0)
    Lx = NCTX + S
    tab = np.zeros((Lx, 160), np.float32)
    i = np.arange(S)
    row = (i // GRID_W).astype(np.float32)
    col = (i % GRID_W).astype(np.float32)

    def inv(n):
        return (theta ** (-np.arange(n, dtype=np.float32) / np.float32(n))).astype(np.float32)

    a64 = np.stack([row[:, None] * inv(16)[None], col[:, None] * inv(16)[None]], 1).astype(np.float32)
    a32 = np.stack([row[:, None] * inv(8)[None], col[:, None] * inv(8)[None]], 1).astype(np.float32)
    tab[:NCTX, 0:32] = 1.0
    tab[:NCTX, 64:80] = 1.0
    tab[NCTX:, 0:32] = np.cos(a64).reshape(S, 32)
    tab[NCTX:, 32:64] = np.sin(a64).reshape(S, 32)
    tab[NCTX:, 64:80] = np.cos(a32).reshape(S, 16)
    tab[NCTX:, 80:96] = np.sin(a32).reshape(S, 16)
    pos = np.arange(Lx, dtype=np.float32)
    ang = (pos[:, None] * inv(32)[None]).astype(np.float32)
    tab[:, 96:128] = np.cos(ang)
    tab[:, 128:160] = np.sin(ang)
    return tab


def host_consts(S):
    tab = host_tables(S)
    j = np.arange(128)[:, None]
    i = np.arange(128)[None, :]
    rc = np.zeros((128, 5, 128), np.float32)
    rc[:, 0] = i - j
    rc[:, 1] = (i >= j)
    rc[:, 2] = (j >= i)
    rc[:, 3] = np.broadcast_to(i + 1, (128, 128))
    rc[:, 4] = np.broadcast_to(128 - i, (128, 128))
    pidx = np.stack([127 - np.arange(128), np.arange(128)], 1).astype(np.float32)
    mk = np.zeros((6, 128, 512), np.float32)
    k = np.arange(128)[:, None]
    q = np.arange(128)[None, :]
    for tp in range(6):
        for qb in range(4):
            rel = (tp - 1) - qb
            if rel == -1:
                mk[tp, :, qb * 128:(qb + 1) * 128] = (k >= q)
            elif rel == 0:
                mk[tp, :, qb * 128:(qb + 1) * 128] = 1.0
            elif rel == 1:
                mk[tp, :, qb * 128:(qb + 1) * 128] = (k <= q)
    iota16 = np.concatenate([np.arange(16), 16 * np.arange(16)]).astype(np.float32)
    return {"tab": tab, "rc": rc, "pidx": pidx, "mk": mk, "iota16": iota16, "ident": np.eye(128, dtype=np.float32)}


def make_in_maps(inp, S):
    f = lambda a: np.ascontiguousarray(np.asarray(a, dtype=np.float32))
    cst = host_consts(S)
    g = np.asarray(inp["qk_gain"], np.float32)
    gain = np.stack([np.concatenate([np.tile(g[l, 0], 4), np.tile(g[l, 1], 2)]) for l in range(DEPTH)]).astype(np.float32)
    sk = np.asarray(inp["peer_subkeys"], np.float32)
    skT = np.ascontiguousarray(sk.reshape(DEPTH, 16, 128, 128).transpose(0, 3, 1, 2))
    shared = {
        "w_mod": f(inp["w_mod"]), "b_mod": f(inp["b_mod"]), "w_in": f(inp["w_in"]), "gain": gain,
        "diff_lambda": f(np.asarray(inp["diff_lambda"]).reshape(DEPTH, 128)), "diff_subln": f(inp["diff_subln"]),
        "win_sink": f(inp["win_sink"]), "ret_decay": f(np.asarray(inp["ret_decay"]).reshape(DEPTH, 8)), "ret_norm": f(inp["ret_norm"]),
        "w_branch": f(np.asarray(inp["w_branch"]).reshape(DEPTH, 1024, D)), "w_out": f(inp["w_out"]), "ln_attn": f(inp["ln_attn"]),
        "ln_ffn": f(inp["ln_ffn"]), "peer_wq": f(inp["peer_wq"]), "skT": skT,
    }
    for l in range(DEPTH):
        shared["peer_u%d" % l] = f(np.asarray(inp["peer_u"])[l])
        shared["peer_v%d" % l] = f(np.asarray(inp["peer_v"])[l])
    shared.update(cst)
    maps = []
    for b in range(2):
        m = dict(shared)
        m["x"] = f(inp["x"][b])
        m["ctx"] = f(inp["ctx"][b])
        m["cT"] = f(np.stack([np.asarray(inp["c"])[b], np.asarray(inp["c_ctx"])], 1))
        maps.append(m)
    return maps


def kernel(**inp):
    S = int(np.asarray(inp["x"]).shape[1])
    nc = build_all(S)
    maps = make_in_maps(inp, S)
    res = run_bass_kernel_spmd(nc, maps, core_ids=[0, 1])
    return np.stack([res.results[b]["out"] for b in range(2)], 0).astype(np.float32)
```

```python
import math
from contextlib import ExitStack
import numpy as np
import concourse.bass as bass
import concourse.mybir as mybir
from concourse.bass_utils import run_bass_kernel_spmd

F32 = mybir.dt.float32
BF16 = mybir.dt.bfloat16
U32 = mybir.dt.uint32
I32 = mybir.dt.int32
AF = mybir.ActivationFunctionType
ALU = mybir.AluOpType
AX = mybir.AxisListType

D = 1024
NCTX = 256
GRID_W = 64
MIX = 2816
DEPTH = 2
ALPHA = (2 * DEPTH) ** 0.25
LN_EPS = 1e-5
RMS_EPS = 1e-6
NCORES = 8


class Tl:
    def __init__(self, t, name):
        self.t, self.name = t, name
        self.w = {}
        self.r = {}
        self.dkey = None
        self.is_dram = False

    def __getitem__(self, k):
        return self.t[k]


class Em:
    def __init__(self, nc, st):
        self.nc, self.gst = nc, st
        self.st = st
        self.eng = {"pe": nc.tensor, "dve": nc.vector, "act": nc.scalar, "pool": nc.gpsimd, "sp": nc.sync}
        self.sem, self.cnt = {}, {}
        for k in ("pe", "dve", "act", "pool"):
            self.sem[k] = st.enter_context(nc.semaphore("s_" + k))
            self.cnt[k] = 0
        self.seen = {k: {} for k in self.eng}
        self.nd = 0
        self.free_dkeys = []
        self.stage_tiles = []
        self.ninst = 0

    def sb(self, name, shape, dtype=F32):
        self.nalloc = getattr(self, "nalloc", 0) + 1
        t = Tl(self.st.enter_context(self.nc.sbuf_tensor("%s_%d" % (name, self.nalloc), list(shape), dtype)), name)
        self.stage_tiles.append(t)
        return t

    def ps(self, name, shape=(128, 512), dtype=F32):
        return Tl(self.gst.enter_context(self.nc.psum_tensor(name, list(shape), dtype)), name)

    def dram(self, name, shape, dtype=F32, kind="ExternalInput"):
        t = Tl(self.nc.dram_tensor(name, list(shape), dtype, kind=kind).ap(), name)
        t.is_dram = True
        if kind == "ExternalInput":
            self.inputs = getattr(self, "inputs", []) + [name]
        return t

    def begin_stage(self):
        self.st = ExitStack()
        self.stage_tiles = []

    def end_stage(self):
        self.barrier()
        for t in self.stage_tiles:
            if t.dkey is not None:
                self.free_dkeys.append(t.dkey)
        self.st.close()
        self.st = self.gst
        self.stage_tiles = []

    def _dkey(self, tl):
        if tl.dkey is None:
            if self.free_dkeys:
                tl.dkey = self.free_dkeys.pop()
            else:
                tl.dkey = "d%d" % self.nd
                self.nd += 1
                self.sem[tl.dkey] = self.gst.enter_context(self.nc.semaphore("s_" + tl.dkey))
                self.cnt[tl.dkey] = 0
        return tl.dkey

    def _waits(self, e, r, w):
        need = {}

        def nd(d):
            for k, c in d.items():
                if e == "pe" and k == "pe":
                    continue
                if c > need.get(k, 0):
                    need[k] = c

        for t in r:
            if not t.is_dram:
                nd(t.w)
        for t in w:
            if not t.is_dram:
                nd(t.w)
                nd(t.r)
        seen = self.seen[e]
        for k, c in need.items():
            if seen.get(k, 0) >= c:
                continue
            seen[k] = c
            self.eng[e].wait_ge(self.sem[k], c)
            self.ninst += 1

    def _record(self, key, c, r, w):
        for t in w:
            if not t.is_dram:
                t.w = {key: c}
                t.r = {}
        for t in r:
            if not t.is_dram and t not in w:
                if c > t.r.get(key, 0):
                    t.r[key] = c

    def op(self, e, fn, r=(), w=()):
        self._waits(e, r, w)
        ins = fn(self.eng[e])
        self.cnt[e] += 1
        self.ninst += 1
        ins.then_inc(self.sem[e], 1)
        self._record(e, self.cnt[e], r, w)

    def V(self, fn, r=(), w=()):
        self.op("dve", fn, r, w)

    def A(self, fn, r=(), w=()):
        self.op("act", fn, r, w)

    def G(self, fn, r=(), w=()):
        self.op("pool", fn, r, w)

    def P(self, fn, r=(), w=()):
        self.op("pe", fn, r, w)

    def dma(self, out_ap, in_ap, r, w, q="sp", fn=None):
        sbt = None
        for t in list(w) + list(r):
            if not t.is_dram:
                sbt = t
                break
        self._waits(q, r, w)
        key = self._dkey(sbt)
        if fn is None:
            ins = self.eng[q].dma_start(out=out_ap, in_=in_ap)
        else:
            ins = fn(self.eng[q])
        self.cnt[key] += 16
        self.ninst += 1
        ins.then_inc(self.sem[key], 16)
        self._record(key, self.cnt[key], r, w)

    def barrier(self):
        for e in ("sp", "pe", "dve", "act", "pool"):
            seen = self.seen[e]
            for k, c in self.cnt.items():
                if c > 0 and seen.get(k, 0) < c:
                    seen[k] = c
                    self.eng[e].wait_ge(self.sem[k], c)
                    self.ninst += 1

    def finish(self):
        self.barrier()


def act(e, out, in_, func, **kw):
    return e.activation(out=out, in_=in_, func=func, **kw)


def load_bcast_row(em, dst, src_ap, n, parts=128):
    em.dma(dst[0:parts, 0:n], src_ap.partition_broadcast(parts), r=[], w=[dst])


def emit_modulate(em, U, H, SC1, SH, TMP):
    em.V(lambda e: e.tensor_tensor(out=TMP[:, :], in0=H[:, :], in1=SC1[:, :], op=ALU.mult), r=[H, SC1], w=[TMP])
    em.G(lambda e: e.tensor_tensor(out=U[:, :], in0=TMP[:, :], in1=SH[:, :], op=ALU.add), r=[TMP, SH], w=[U])


def emit_transpose8(em, UT, U, IDN, PSA, PSB, ncols=D):
    nk = ncols // 128
    k = 0
    flip = 0
    while k < nk:
        ps = PSA if flip == 0 else PSB
        nn = min(4, nk - k)
        for j in range(nn):
            em.P(lambda e, j=j, k=k, ps=ps: e.transpose(out=ps[:, j * 128:(j + 1) * 128], in_=U[:, (k + j) * 128:(k + j + 1) * 128],
                                                         identity=IDN[:, :]), r=[U, IDN], w=[ps])
        fn = (lambda e, k=k, nn=nn, ps=ps: act(e, UT[:, k:k + nn, :], ps[:, 0:nn * 128].rearrange("p (a b) -> p a b", b=128), AF.Copy))
        if flip == 0:
            em.A(fn, r=[ps], w=[UT])
        else:
            em.V(lambda e, k=k, nn=nn, ps=ps: e.tensor_copy(out=UT[:, k:k + nn, :], in_=ps[:, 0:nn * 128].rearrange("p (a b) -> p a b", b=128)),
                 r=[ps], w=[UT])
        k += nn
        flip ^= 1


def emit_layernorm(em, OUT, R, GAM, BET, ST, MV, RS, TMP):
    for c in range(2):
        em.V(lambda e, c=c: e.bn_stats(out=ST[:, c * 6:(c + 1) * 6], in_=R[:, c * 512:(c + 1) * 512]), r=[R], w=[ST])
    em.V(lambda e: e.bn_aggr(out=MV[:, 0:2], in_=ST[:, 0:12]), r=[ST], w=[MV])
    em.V(lambda e: e.tensor_scalar(out=RS[:, 0:1], in0=MV[:, 1:2], scalar1=LN_EPS, scalar2=None, op0=ALU.add), r=[MV], w=[RS])
    em.A(lambda e: act(e, RS[:, 0:1], RS[:, 0:1], AF.Sqrt), r=[RS], w=[RS])
    em.V(lambda e: e.reciprocal(out=RS[:, 0:1], in_=RS[:, 0:1]), r=[RS], w=[RS])
    em.V(lambda e: e.tensor_scalar(out=TMP[:, :], in0=R[:, :], scalar1=MV[:, 0:1], scalar2=RS[:, 0:1], op0=ALU.subtract, op1=ALU.mult),
         r=[R, MV, RS], w=[TMP])
    em.G(lambda e: e.tensor_tensor(out=TMP[:, :], in0=TMP[:, :], in1=GAM[:, :], op=ALU.mult), r=[TMP, GAM], w=[TMP])
    em.V(lambda e: e.tensor_tensor(out=OUT[:, :], in0=TMP[:, :], in1=BET[:, :], op=ALU.add), r=[TMP, BET], w=[OUT])


def load_weight_bf16(em, W, src_ap, nk, ncols, STG, col0=0, dummy=None):
    i = 0
    stw = STG[0].t.shape[1]
    for k in range(nk):
        c = 0
        while c < ncols:
            cw = min(stw, ncols - c)
            stg = STG[i % len(STG)]
            em.dma(stg[:, 0:cw], src_ap[k * 128:(k + 1) * 128, col0 + c:col0 + c + cw], r=[], w=[stg])
            if i % 2 == 0:
                em.A(lambda e, k=k, c=c, cw=cw, stg=stg: act(e, W[:, k, c:c + cw], stg[:, 0:cw], AF.Copy), r=[stg], w=[W])
            else:
                em.V(lambda e, k=k, c=c, cw=cw, stg=stg: e.tensor_copy(out=W[:, k, c:c + cw], in_=stg[:, 0:cw]), r=[stg], w=[W])
            c += cw
            i += 1


class Ctx:
    pass


ROT_GROUPS = [
    (0, 6, 2, 16, 0, 32),
    (512, 16, 2, 8, 64, 80),
    (1280, 6, 2, 16, 0, 32),
    (1792, 8, 1, 32, 96, 128),
]
COPY_COLS = [(384, 512), (1024, 1280), (1664, 1792), (2304, 2816)]
NRM_GROUPS = [(0, 6, 64, 0), (512, 16, 32, 6), (1280, 6, 64, 22)]
XT_BLOCKS = [0, 128, 256, 512, 640, 768, 896, 1280, 1408, 1536, 1792, 1920, 2048, 2176]
V_GROUPS = [(384, 2, 0), (1024, 4, 2), (1664, 2, 6)]


def hrows(C, l, blk):
    if l == 0:
        if blk < 2:
            return C.ctx.t[blk * 128:(blk + 1) * 128, :]
        return C.x.t[(blk - 2) * 128:(blk - 1) * 128, :]
    return C.H2.t[blk * 128:(blk + 1) * 128, :]


def stage_mod(C):
    em = C.em
    em.begin_stage()
    CT = em.sb("CT", [128, 8, 2])
    WS = [em.sb("WS%d" % i, [128, 8, 512]) for i in range(2)]
    BB = em.sb("BB", [2, 6 * D])
    OO = em.sb("OO", [2, 6 * D])
    em.dma(CT[:, :, :], C.cT.t.rearrange("(k p) r -> p k r", p=128), r=[], w=[CT])
    em.A(lambda e: act(e, CT[:, :, :], CT[:, :, :], AF.Silu), r=[CT], w=[CT])
    it = 0
    for l in range(C.depth):
        em.dma(BB[:, :], C.b_mod.t[l, :].partition_broadcast(2), r=[], w=[BB])
        for g in range(12):
            ws, ps = WS[it % 2], C.PS[it % 2]
            em.dma(ws[:, :, :], C.w_mod.t[l, :, g * 512:(g + 1) * 512].rearrange("(k p) c -> p k c", p=128), r=[], w=[ws])
            for k in range(8):
                em.P(lambda e: e.matmul(ps[0:2, :], lhsT=CT[:, k, :], rhs=ws[:, k, :], start=(k == 0), stop=(k == 7)), r=[CT, ws], w=[ps])
            em.V(lambda e: e.tensor_tensor(out=OO[:, g * 512:(g + 1) * 512], in0=ps[0:2, :], in1=BB[:, g * 512:(g + 1) * 512], op=ALU.add),
                 r=[ps, BB], w=[OO])
            it += 1
        em.dma(C.mod.t[l, :, :], OO[:, :], r=[OO], w=[C.mod])
    em.end_stage()


def load_mod_rows(C, l, idxs, tiles, plus1=()):
    em = C.em
    for ty in range(2):
        for i, mi in enumerate(idxs):
            t = tiles[ty][i]
            em.dma(t[:, :], C.mod.t[l, ty, mi * D:(mi + 1) * D].partition_broadcast(128), r=[], w=[t])
            if mi in plus1:
                em.V(lambda e: e.tensor_scalar(out=t[:, :], in0=t[:, :], scalar1=1.0, scalar2=None, op0=ALU.add), r=[t], w=[t])


def stage_proj(C, l):
    em = C.em
    em.begin_stage()
    PS = C.PS
    W = em.sb("W", [128, 8, MIX], BF16)
    STG = [em.sb("STG%d" % i, [128, 2048]) for i in range(2)]
    GN = em.sb("GN", [128, 384])
    MR = [[em.sb("MR%d_%d" % (ty, i), [128, D]) for i in range(2)] for ty in range(2)]
    HB = [em.sb("HB%d" % i, [128, D]) for i in range(2)]
    TB = [em.sb("TB%d" % i, [128, 160]) for i in range(2)]
    TMP = em.sb("TMP", [128, D])
    U = em.sb("U", [128, D])
    UT = em.sb("UT", [128, 8, 128], BF16)
    PSB = em.sb("PSB", [128, MIX])
    PO = [em.sb("PO%d" % i, [128, MIX]) for i in range(2)]
    NR = em.sb("NR", [128, 28])
    RM = em.sb("RM", [128, 28])
    XTo = [em.sb("XTo%d" % i, [128, 14, 128]) for i in range(2)]
    VPo = [em.sb("VPo%d" % i, [128, 8, 65]) for i in range(2)]
    SQ = em.sb("SQ", [128, 512])
    SS = em.sb("SS", [128, 8])
    T1 = em.sb("T1", [128, 256])
    T2 = em.sb("T2", [128, 256])
    T3 = em.sb("T3", [128, 256])
    T4 = em.sb("T4", [128, 256])
    RX = em.sb("RX", [28, 1])
    RR = em.sb("RR", [1, 28])

    load_bcast_row(em, GN, C.gain.t[l, :], 384)
    load_mod_rows(C, l, [0, 1], MR, plus1=(1,))
    for i in range(2):
        em.G(lambda e: e.memset(VPo[i][:, :, :], 1.0), w=[VPo[i]])
    load_weight_bf16(em, W, C.w_in.t[l], 8, MIX, STG)

    pmi = 0
    xt4 = C.XT.t.rearrange("p (j e) l -> p j e l", e=2)
    for b in range(C.NBx):
        ty = 1 if b < 2 else 0
        H, T, PO_, XTo_, VPo_ = HB[b % 2], TB[b % 2], PO[b % 2], XTo[b % 2], VPo[b % 2]
        em.dma(H[:, :], hrows(C, l, b), r=[], w=[H])
        em.dma(T[:, :], C.tab.t[b * 128:(b + 1) * 128, :], r=[], w=[T])
        emit_modulate(em, U, H, MR[ty][1], MR[ty][0], TMP)
        emit_transpose8(em, UT, U, C.IDN, PS[0], PS[1])
        c = 0
        while c < MIX:
            cw = min(512, MIX - c)
            ps = PS[2 + pmi % 3]
            pmi += 1
            for k in range(8):
                em.P(lambda e: e.matmul(ps[:, 0:cw], lhsT=UT[:, k, :], rhs=W[:, k, c:c + cw], start=(k == 0), stop=(k == 7)), r=[UT, W], w=[ps])
            em.A(lambda e: act(e, PSB[:, c:c + cw], ps[:, 0:cw], AF.Copy), r=[ps], w=[PSB])
            c += cw
        v384 = PSB[:, 0:384].rearrange("p (h d) -> p h d", d=64)
        em.A(lambda e: act(e, SQ[:, 0:384], PSB[:, 0:384], AF.Square), r=[PSB], w=[SQ])
        em.V(lambda e: e.tensor_reduce(out=SS[:, 0:6], in_=SQ[:, 0:384].rearrange("p (h d) -> p h d", d=64), axis=AX.X, op=ALU.add), r=[SQ], w=[SS])
        em.V(lambda e: e.tensor_scalar(out=SS[:, 0:6], in0=SS[:, 0:6], scalar1=1.0 / 64, scalar2=RMS_EPS, op0=ALU.mult, op1=ALU.add), r=[SS], w=[SS])
        em.A(lambda e: act(e, SS[:, 0:6], SS[:, 0:6], AF.Sqrt), r=[SS], w=[SS])
        em.V(lambda e: e.reciprocal(out=SS[:, 0:6], in_=SS[:, 0:6]), r=[SS], w=[SS])
        em.V(lambda e: e.tensor_tensor(out=v384, in0=v384, in1=SS[:, 0:6].unsqueeze(2).to_broadcast([128, 6, 64]), op=ALU.mult), r=[PSB, SS], w=[PSB])
        em.V(lambda e: e.tensor_tensor(out=PSB[:, 0:384], in0=PSB[:, 0:384], in1=GN[:, :], op=ALU.mult), r=[PSB, GN], w=[PSB])
        for (c0, c1) in COPY_COLS:
            em.G(lambda e: e.tensor_copy(out=PO_[:, c0:c1], in_=PSB[:, c0:c1]), r=[PSB], w=[PO_])
        for (c0, nh, a, q, co, so) in ROT_GROUPS:
            n = nh * 2 * a * q

            def xv(tl, f):
                return tl[:, c0:c0 + n].rearrange("p (h a f q) -> p h a f q", a=a, f=2, q=q)[:, :, :, f, :]

            def tv(off):
                return T[:, off:off + a * q].rearrange("p (a q) -> p a q", a=a).unsqueeze(1).to_broadcast([128, nh, a, q])

            def tmpv(tl):
                return tl[:, 0:n // 2].rearrange("p (h a q) -> p h a q", a=a, q=q)

            em.V(lambda e: e.tensor_tensor(out=tmpv(T1), in0=xv(PSB, 0), in1=tv(co), op=ALU.mult), r=[PSB, T], w=[T1])
            em.G(lambda e: e.tensor_tensor(out=tmpv(T2), in0=xv(PSB, 1), in1=tv(so), op=ALU.mult), r=[PSB, T], w=[T2])
            em.V(lambda e: e.tensor_tensor(out=xv(PO_, 0), in0=tmpv(T1), in1=tmpv(T2), op=ALU.subtract), r=[T1, T2], w=[PO_])
            em.G(lambda e: e.tensor_tensor(out=tmpv(T3), in0=xv(PSB, 1), in1=tv(co), op=ALU.mult), r=[PSB, T], w=[T3])
            em.V(lambda e: e.tensor_tensor(out=tmpv(T4), in0=xv(PSB, 0), in1=tv(so), op=ALU.mult), r=[PSB, T], w=[T4])
            em.V(lambda e: e.tensor_tensor(out=xv(PO_, 1), in0=tmpv(T3), in1=tmpv(T4), op=ALU.add), r=[T3, T4], w=[PO_])
        for (c0, nh, d, o0) in NRM_GROUPS:
            n = nh * d
            em.A(lambda e: act(e, SQ[:, 0:n], PO_[:, c0:c0 + n], AF.Square), r=[PO_], w=[SQ])
            em.V(lambda e: e.tensor_reduce(out=NR[:, o0:o0 + nh], in_=SQ[:, 0:n].rearrange("p (h d) -> p h d", d=d), axis=AX.X, op=ALU.add), r=[SQ], w=[NR])
        if b == 0:
            em.V(lambda e: e.tensor_copy(out=RM[:, :], in_=NR[:, :]), r=[NR], w=[RM])
        else:
            em.V(lambda e: e.tensor_tensor(out=RM[:, :], in0=RM[:, :], in1=NR[:, :], op=ALU.max), r=[RM, NR], w=[RM])
        em.dma(C.P.t[b * 128:(b + 1) * 128, :], PO_[:, :], r=[PO_], w=[C.P])
        j = 0
        bi = 0
        while j < 14:
            nn = min(4, 14 - j)
            ps = PS[5 + bi % 2]
            bi += 1
            for jj in range(nn):
                c0 = XT_BLOCKS[j + jj]
                em.P(lambda e: e.transpose(out=ps[:, jj * 128:(jj + 1) * 128], in_=PO_[:, c0:c0 + 128], identity=C.IDN[:, :]), r=[PO_, C.IDN], w=[ps])
            em.A(lambda e: act(e, XTo_[:, j:j + nn, :], ps[:, 0:nn * 128].rearrange("p (a b) -> p a b", b=128), AF.Copy), r=[ps], w=[XTo_])
            j += nn
        em.dma(xt4[:, :, 0, b * 128:(b + 1) * 128], XTo_[0:64, :, :], r=[XTo_], w=[C.XT])
        em.dma(xt4[:, :, 1, b * 128:(b + 1) * 128], XTo_[64:128, :, :], r=[XTo_], w=[C.XT])
        for (c0, nh, h0) in V_GROUPS:
            em.G(lambda e: e.tensor_copy(out=VPo_[:, h0:h0 + nh, 0:64], in_=PO_[:, c0:c0 + nh * 64].rearrange("p (h d) -> p h d", d=64)), r=[PO_], w=[VPo_])
        em.dma(C.VPs.t[b * 128:(b + 1) * 128, :, :], VPo_[:, :, :], r=[VPo_], w=[C.VPs])
    em.P(lambda e: e.transpose(out=PS[0][0:28, 0:128], in_=RM[:, 0:28], identity=C.IDN[:, :]), r=[RM, C.IDN], w=[PS[0]])
    em.V(lambda e: e.tensor_reduce(out=RX[:, 0:1], in_=PS[0][0:28, 0:128], axis=AX.X, op=ALU.max), r=[PS[0]], w=[RX])
    em.P(lambda e: e.transpose(out=PS[1][0:1, 0:28], in_=RX[0:28, 0:1], identity=C.IDN[0:28, 0:28]), r=[RX, C.IDN], w=[PS[1]])
    em.V(lambda e: e.tensor_copy(out=RR[:, :], in_=PS[1][0:1, 0:28]), r=[PS[1]], w=[RR])
    em.P(lambda e: e.matmul(PS[2][:, 0:28], lhsT=C.ONES[0:1, 0:128], rhs=RR[0:1, 0:28], start=True, stop=True), r=[C.ONES, RR], w=[PS[2]])
    em.V(lambda e: e.tensor_copy(out=C.RMB[:, :], in_=PS[2][:, 0:28]), r=[PS[2]], w=[C.RMB])
    em.end_stage()


def stage_attn(C, l, kind, p=0):
    em = C.em
    em.begin_stage()
    PS = C.PS
    Lx, NBx, S = C.Lx, C.NBx, C.S
    need_ctx = l < C.depth - 1
    dh = 32 if kind == "B" else 64
    scale = dh ** -0.5
    nqf = 2 if kind == "B" else 4
    if kind == "A":
        qh0, kh0, vh0, qn0, kn0, OT, oh0 = 0, 4, 0, 0, 4, C.OTA, 0
    elif kind == "B":
        qh0, kh0, vh0, qn0, kn0, OT, oh0 = 6 + 2 * p, 10 + 2 * p, 2 + 2 * p, 6 + 4 * p, 14 + 4 * p, C.OTB, 2 * p
    else:
        qh0, kh0, vh0, qn0, kn0, OT, oh0 = 14, 18, 6, 22, 26, C.OTC, 0
    nout = nqf
    lam_init = 0.8 - 0.6 * math.exp(-0.3 * l)
    GQ = 512

    KB = em.sb("KB", [64, 2, Lx], BF16)
    VB = em.sb("VB", [128, NBx, 2, 65], BF16)
    STG = [em.sb("STG%d" % i, [128, 2080]) for i in range(2)]
    NEGM = em.sb("NEGM", [128, 4])
    QS = [em.sb("QS%d" % i, [64, nqf, GQ]) for i in range(2)]
    QB = [em.sb("QB%d" % i, [64, nqf, GQ], BF16) for i in range(2)]
    PTl = [em.sb("PT%d" % i, [128, 512], BF16) for i in range(3)]
    OU = [em.sb("OU%d" % i, [65, 512]) for i in range(4)]
    RZ = em.sb("RZ", [65, 512])
    OUT = [em.sb("OUT%d" % i, [64, nout, GQ]) for i in range(2)]
    NRM = [em.sb("NRM%d" % i, [64, 512]) for i in range(2)]
    SQ = em.sb("SQ", [64, 512])
    SPS = [PS[0], PS[1], PS[2]]
    OPS = [PS[3], PS[4], PS[5]]
    BPS = [PS[6], PS[7]]
    ONES = C.ONES

    if kind == "B":
        em.V(lambda e: e.tensor_tensor(out=NEGM[:, :], in0=C.RMB[:, qn0:qn0 + 4], in1=C.RMB[:, kn0:kn0 + 4], op=ALU.mult), r=[C.RMB], w=[NEGM])
    else:
        em.V(lambda e: e.tensor_tensor(out=NEGM[:, :].rearrange("p (a b) -> p a b", b=2), in0=C.RMB[:, qn0:qn0 + 4].rearrange("p (a b) -> p a b", b=2),
                                       in1=C.RMB[:, kn0:kn0 + 2].unsqueeze(2).to_broadcast([128, 2, 2]), op=ALU.mult), r=[C.RMB], w=[NEGM])
    em.A(lambda e: act(e, NEGM[:, :], NEGM[:, :], AF.Sqrt), r=[NEGM], w=[NEGM])
    em.V(lambda e: e.tensor_scalar(out=NEGM[:, :], in0=NEGM[:, :], scalar1=-scale, scalar2=None, op0=ALU.mult), r=[NEGM], w=[NEGM])
    if kind == "C":
        SK = em.sb("SK", [128, 4])
        ES = em.sb("ES", [128, 4])
        MKB = em.sb("MKB", [128, 6, 512], BF16)
        load_bcast_row(em, SK, C.win_sink.t[l, :], 4)
        em.V(lambda e: e.tensor_tensor(out=ES[:, :], in0=SK[:, :], in1=NEGM[:, :], op=ALU.add), r=[SK, NEGM], w=[ES])
        em.A(lambda e: act(e, ES[:, :], ES[:, :], AF.Exp), r=[ES], w=[ES])
        for t in range(6):
            stg = STG[t % 2]
            em.dma(stg[:, 0:512], C.mk.t[t, :, :], r=[], w=[stg])
            em.V(lambda e: e.tensor_copy(out=MKB[:, t, :], in_=stg[:, 0:512]), r=[stg], w=[MKB])
    if kind == "B":
        LP = em.sb("LP", [1, 128])
        LS = em.sb("LS", [1, 4])
        NEGLAM = em.sb("NEGLAM", [64, 1])
        SUBL = em.sb("SUBL", [64, 1])
        em.dma(LP[:, :], C.diff_lambda.t[l, :].partition_broadcast(1), r=[], w=[LP])
        lpv = LP[0:1, :].rearrange("p (a b d) -> p a b d", a=2, b=2)
        em.V(lambda e: e.tensor_tensor(out=lpv[:, :, 0, :], in0=lpv[:, :, 0, :], in1=lpv[:, :, 1, :], op=ALU.mult), r=[LP], w=[LP])
        em.V(lambda e: e.tensor_reduce(out=LS[:, 0:2], in_=lpv[:, :, 0, :], axis=AX.X, op=ALU.add), r=[LP], w=[LS])
        em.A(lambda e: act(e, LS[:, 0:2], LS[:, 0:2], AF.Exp), r=[LS], w=[LS])
        em.V(lambda e: e.tensor_tensor(out=LS[:, 2:3], in0=LS[:, 1:2], in1=LS[:, 0:1], op=ALU.subtract), r=[LS], w=[LS])
        em.V(lambda e: e.tensor_scalar(out=LS[:, 2:3], in0=LS[:, 2:3], scalar1=-float(lam_init), scalar2=None, op0=ALU.add), r=[LS], w=[LS])
        em.P(lambda e: e.matmul(BPS[1][0:64, 0:1], lhsT=ONES[0:1, 0:64], rhs=LS[0:1, 2:3], start=True, stop=True), r=[ONES, LS], w=[BPS[1]])
        em.V(lambda e: e.tensor_copy(out=NEGLAM[:, :], in_=BPS[1][0:64, 0:1]), r=[BPS[1]], w=[NEGLAM])
        em.dma(SUBL[:, :], C.diff_subln.t[l, :].rearrange("(p o) -> p o", o=1), r=[], w=[SUBL])
        em.V(lambda e: e.tensor_scalar(out=SUBL[:, :], in0=SUBL[:, :], scalar1=1.0 - float(lam_init), scalar2=None, op0=ALU.mult), r=[SUBL], w=[SUBL])

    i = 0
    for f in range(2):
        c = 0
        while c < Lx:
            cw = min(2048, Lx - c)
            stg = STG[i % 2]
            em.dma(stg[0:64, 0:cw], C.XT.t[:, kh0 + f, c:c + cw], r=[], w=[stg])
            if i % 2 == 0:
                em.A(lambda e: act(e, KB[:, f, c:c + cw], stg[0:64, 0:cw], AF.Copy), r=[stg], w=[KB])
            else:
                em.V(lambda e: e.tensor_copy(out=KB[:, f, c:c + cw], in_=stg[0:64, 0:cw]), r=[stg], w=[KB])
            c += cw
            i += 1
    t0 = 0
    vpv = C.VPs.t[:, vh0:vh0 + 2, :].rearrange("(t p) v c -> p t v c", p=128)
    while t0 < NBx:
        tn = min(16, NBx - t0)
        stg = STG[i % 2]
        sv = stg[:, 0:tn * 130].rearrange("p (t v c) -> p t v c", v=2, c=65)
        em.dma(sv, vpv[:, t0:t0 + tn, :, :], r=[], w=[stg])
        if i % 2 == 0:
            em.A(lambda e: act(e, VB[:, t0:t0 + tn, :, :], sv, AF.Copy), r=[stg], w=[VB])
        else:
            em.V(lambda e: e.tensor_copy(out=VB[:, t0:t0 + tn, :, :], in_=sv), r=[stg], w=[VB])
        t0 += tn
        i += 1

    groups = []
    for g in range(S // GQ):
        if kind == "C":
            tl = [(0, None), (1, None)]
            for tp in range(6):
                blk = 4 * g - 1 + tp
                if 0 <= blk < S // 128:
                    tl.append((2 + blk, tp))
        else:
            tl = [(t, None) for t in range(NBx)]
        groups.append((NCTX + g * GQ, GQ, tl))
    if need_ctx:
        groups.append((0, NCTX, [(0, None), (1, None)]))

    cnt = {"s": 0, "p": 0, "o": 0, "b": 0}
    for gi, (q0, gq, tl) in enumerate(groups):
        qs, qb, out_t = QS[gi % 2], QB[gi % 2], OUT[gi % 2]
        em.dma(qs[:, :, 0:gq], C.XT.t[:, qh0:qh0 + nqf, q0:q0 + gq], r=[], w=[qs])
        em.V(lambda e: e.tensor_copy(out=qb[:, :, 0:gq], in_=qs[:, :, 0:gq]), r=[qs], w=[qb])
        for u in range(4):
            if kind == "B":
                r0, qf, kf = (u % 2) * 32, u // 2, u // 2
            else:
                r0, qf, kf = 0, u, u // 2
            r1 = r0 + dh

            def emit_s(ti):
                sps = SPS[cnt["s"] % 3]
                cnt["s"] += 1
                t = tl[ti][0]
                em.P(lambda e: e.matmul(sps[:, 0:gq], lhsT=KB[r0:r1, kf, t * 128:(t + 1) * 128], rhs=qb[r0:r1, qf, 0:gq], start=True, stop=True),
                     r=[KB, qb], w=[sps])
                return sps

            ops = OPS[cnt["o"] % 3]
            cnt["o"] += 1
            nxt = emit_s(0)
            for ti in range(len(tl)):
                sps = nxt
                if ti + 1 < len(tl):
                    nxt = emit_s(ti + 1)
                t, mi = tl[ti]
                pt = PTl[cnt["p"] % 3]
                cnt["p"] += 1
                em.A(lambda e: act(e, pt[:, 0:gq], sps[:, 0:gq], AF.Exp, scale=scale, bias=NEGM[:, u:u + 1]), r=[sps, NEGM], w=[pt])
                if mi is not None:
                    em.V(lambda e: e.tensor_tensor(out=pt[:, 0:gq], in0=pt[:, 0:gq], in1=MKB[:, mi, 0:gq], op=ALU.mult), r=[pt, MKB], w=[pt])
                em.P(lambda e: e.matmul(ops[0:65, 0:gq], lhsT=VB[:, t, kf, :], rhs=pt[:, 0:gq], start=(ti == 0), stop=(ti == len(tl) - 1)),
                     r=[VB, pt], w=[ops])
            ou = OU[u]
            em.V(lambda e: e.tensor_copy(out=ou[0:65, 0:gq], in_=ops[0:65, 0:gq]), r=[ops], w=[ou])

        def normalize(u, dst_ap, dst_tl):
            ou = OU[u]
            if kind == "C":
                em.V(lambda e: e.tensor_scalar(out=RZ[64:65, 0:gq], in0=ou[64:65, 0:gq], scalar1=ES[64:65, u:u + 1], scalar2=None, op0=ALU.add),
                     r=[ou, ES], w=[RZ])
                em.V(lambda e: e.reciprocal(out=RZ[64:65, 0:gq], in_=RZ[64:65, 0:gq]), r=[RZ], w=[RZ])
            else:
                em.V(lambda e: e.reciprocal(out=RZ[64:65, 0:gq], in_=ou[64:65, 0:gq]), r=[ou], w=[RZ])
            bps = BPS[cnt["b"] % 2]
            cnt["b"] += 1
            em.P(lambda e: e.matmul(bps[0:64, 0:gq], lhsT=ONES[64:65, 0:64], rhs=RZ[64:65, 0:gq], start=True, stop=True), r=[ONES, RZ], w=[bps])
            em.V(lambda e: e.tensor_tensor(out=dst_ap, in0=ou[0:64, 0:gq], in1=bps[0:64, 0:gq], op=ALU.mult), r=[ou, bps], w=[dst_tl])

        if kind != "B":
            for u in range(4):
                normalize(u, out_t[:, u, 0:gq], out_t)
        else:
            for hl in range(2):
                normalize(2 * hl, NRM[0][:, 0:gq], NRM[0])
                normalize(2 * hl + 1, NRM[1][:, 0:gq], NRM[1])
                em.V(lambda e: e.scalar_tensor_tensor(out=NRM[0][:, 0:gq], in0=NRM[1][:, 0:gq], scalar=NEGLAM[:, 0:1], in1=NRM[0][:, 0:gq],
                                                      op0=ALU.mult, op1=ALU.add), r=[NRM[0], NRM[1], NEGLAM], w=[NRM[0]])
                em.A(lambda e: act(e, SQ[:, 0:gq], NRM[0][:, 0:gq], AF.Square), r=[NRM[0]], w=[SQ])
                bps = BPS[cnt["b"] % 2]
                cnt["b"] += 1
                em.P(lambda e: e.matmul(bps[0:64, 0:gq], lhsT=ONES[0:64, 0:64], rhs=SQ[:, 0:gq], start=True, stop=True), r=[ONES, SQ], w=[bps])
                em.V(lambda e: e.tensor_scalar(out=SQ[:, 0:gq], in0=bps[0:64, 0:gq], scalar1=1.0 / 64, scalar2=RMS_EPS, op0=ALU.mult, op1=ALU.add),
                     r=[bps], w=[SQ])
                em.A(lambda e: act(e, SQ[:, 0:gq], SQ[:, 0:gq], AF.Sqrt), r=[SQ], w=[SQ])
                em.V(lambda e: e.reciprocal(out=SQ[:, 0:gq], in_=SQ[:, 0:gq]), r=[SQ], w=[SQ])
                em.V(lambda e: e.tensor_tensor(out=NRM[0][:, 0:gq], in0=NRM[0][:, 0:gq], in1=SQ[:, 0:gq], op=ALU.mult), r=[NRM[0], SQ], w=[NRM[0]])
                em.V(lambda e: e.tensor_scalar(out=out_t[:, hl, 0:gq], in0=NRM[0][:, 0:gq], scalar1=SUBL[:, 0:1], scalar2=None, op0=ALU.mult),
                     r=[NRM[0], SUBL], w=[out_t])
        em.dma(OT.t[:, oh0:oh0 + nout, q0:q0 + gq], out_t[:, :, 0:gq], r=[out_t], w=[OT])
    em.end_stage()


def stage_ret(C, l, f):
    em = C.em
    em.begin_stage()
    PS = C.PS
    NBx = C.NBx
    RC = em.sb("RC", [128, 5, 128])
    PIDX = em.sb("PIDX", [128, 2])
    DC = em.sb("DC", [128, 8])
    LG = em.sb("LG", [128, 8])
    NLG = em.sb("NLG", [128, 8])
    DT = em.sb("DT", [128, 4, 128])
    XI = em.sb("XI", [64, 4, 128])
    ZE = em.sb("ZE", [128, 4])
    GC = em.sb("GC", [128, 4])
    ST = em.sb("ST", [64, 4, 64])
    QTc = [em.sb("QTc%d" % i, [64, 4, 128]) for i in range(2)]
    KTc = [em.sb("KTc%d" % i, [64, 4, 128]) for i in range(2)]
    KV = [em.sb("KV%d" % i, [128, 768]) for i in range(2)]
    OFc = [em.sb("OFc%d" % i, [128, 256]) for i in range(2)]
    QX = em.sb("QX", [64, 4, 128])
    KZ = em.sb("KZ", [128, 4, 64])
    INM = em.sb("INM", [128, 512])
    OO = [em.sb("OO%d" % i, [128, 256]) for i in range(2)]
    XN = em.sb("XN", [128, 256])
    SG = em.sb("SG", [128, 256])
    BST = em.sb("BST", [128, 24])
    MV = em.sb("MV", [128, 8])
    RS = em.sb("RS", [128, 4])
    N0 = em.sb("N0", [128, 256])
    N1 = em.sb("N1", [128, 256])

    em.dma(RC[:, :, :], C.rc.t[:, :, :], r=[], w=[RC])
    em.dma(PIDX[:, :], C.pidx.t[:, :], r=[], w=[PIDX])
    load_bcast_row(em, DC, C.ret_decay.t[l, :], 8)
    load_bcast_row(em, N0, C.ret_norm.t[l, 0, :], 256)
    load_bcast_row(em, N1, C.ret_norm.t[l, 1, :], 256)
    em.A(lambda e: act(e, LG[:, :], DC[:, :], AF.Exp, scale=-1.0), r=[DC], w=[LG])
    em.V(lambda e: e.tensor_scalar(out=LG[:, :], in0=LG[:, :], scalar1=1.0, scalar2=None, op0=ALU.add), r=[LG], w=[LG])
    em.A(lambda e: act(e, NLG[:, :], LG[:, :], AF.Ln), r=[LG], w=[NLG])
    em.V(lambda e: e.tensor_scalar(out=LG[:, :], in0=NLG[:, :], scalar1=-1.0, scalar2=None, op0=ALU.mult), r=[NLG], w=[LG])
    for h in range(4):
        col = f * 4 + h
        scl = LG if f == 0 else NLG
        em.A(lambda e: act(e, DT[:, h, :], RC[:, 0, :], AF.Exp, scale=scl[:, col:col + 1]), r=[RC, scl], w=[DT])
        em.V(lambda e: e.scalar_tensor_tensor(out=DT[:, h, :], in0=DT[:, h, :], scalar=0.125, in1=RC[:, 1 + f, :], op0=ALU.mult, op1=ALU.mult),
             r=[DT, RC], w=[DT])
        em.A(lambda e: act(e, XI[:, h, :], RC[0:64, 3 + f, :], AF.Exp, scale=LG[0:64, col:col + 1]), r=[RC, LG], w=[XI])
        em.A(lambda e: act(e, ZE[:, h:h + 1], PIDX[:, f:f + 1], AF.Exp, scale=LG[:, col:col + 1]), r=[PIDX, LG], w=[ZE])
    em.V(lambda e: e.tensor_scalar(out=ZE[:, :], in0=ZE[:, :], scalar1=0.125, scalar2=None, op0=ALU.mult), r=[ZE], w=[ZE])
    em.A(lambda e: act(e, GC[:, :], LG[:, f * 4:f * 4 + 4], AF.Exp, scale=128.0), r=[LG], w=[GC])
    em.V(lambda e: e.memset(ST[:, :, :], 0.0), w=[ST])

    order = list(range(NBx)) if f == 0 else [1, 0] + list(range(NBx - 1, 1, -1))
    for it, c in enumerate(order):
        qt_, kt_, kv, ofc, oo = QTc[it % 2], KTc[it % 2], KV[it % 2], OFc[it % 2], OO[it % 2]
        cs = slice(c * 128, (c + 1) * 128)
        em.dma(qt_[:, :, :], C.XT.t[:, 20:24, cs], r=[], w=[qt_])
        em.dma(kt_[:, :, :], C.XT.t[:, 24:28, cs], r=[], w=[kt_])
        em.dma(kv[:, :], C.P.t[cs, 2048:2816], r=[], w=[kv])
        if f == 1:
            em.dma(ofc[:, :], C.ODF.t[cs, :], r=[], w=[ofc])
        em.V(lambda e: e.tensor_tensor(out=QX[:, :, :], in0=qt_[:, :, :], in1=XI[:, :, :], op=ALU.mult), r=[qt_, XI], w=[QX])
        em.G(lambda e: e.tensor_tensor(out=KZ[:, :, :], in0=kv[:, 0:256].rearrange("p (h d) -> p h d", d=64),
                                       in1=ZE[:, 0:4].unsqueeze(2).to_broadcast([128, 4, 64]), op=ALU.mult), r=[kv, ZE], w=[KZ])
        for h in range(4):
            em.P(lambda e: e.matmul(PS[0][:, h * 128:(h + 1) * 128], lhsT=kt_[:, h, :], rhs=qt_[:, h, :], start=True, stop=True), r=[kt_, qt_], w=[PS[0]])
        em.V(lambda e: e.tensor_tensor(out=INM[:, :], in0=PS[0][:, :], in1=DT[:, :, :].rearrange("p h i -> p (h i)"), op=ALU.mult), r=[PS[0], DT], w=[INM])
        for h in range(4):
            em.P(lambda e: e.matmul(PS[1][:, h * 64:(h + 1) * 64], lhsT=INM[:, h * 128:(h + 1) * 128], rhs=kv[:, 256 + h * 64:256 + (h + 1) * 64],
                                    start=True, stop=False), r=[INM, kv], w=[PS[1]])
            em.P(lambda e: e.matmul(PS[1][:, h * 64:(h + 1) * 64], lhsT=QX[:, h, :], rhs=ST[:, h, :], start=False, stop=True), r=[QX, ST], w=[PS[1]])
        for h in range(4):
            em.P(lambda e: e.matmul(PS[2][0:64, h * 64:(h + 1) * 64], lhsT=KZ[:, h, :], rhs=kv[:, 256 + h * 64:256 + (h + 1) * 64], start=True, stop=True),
                 r=[KZ, kv], w=[PS[2]])
        em.V(lambda e: e.tensor_tensor(out=ST[:, :, :], in0=ST[:, :, :], in1=GC[0:64, 0:4].unsqueeze(2).to_broadcast([64, 4, 64]), op=ALU.mult),
             r=[ST, GC], w=[ST])
        em.V(lambda e: e.tensor_tensor(out=ST[:, :, :].rearrange("p h d -> p (h d)"), in0=ST[:, :, :].rearrange("p h d -> p (h d)"), in1=PS[2][0:64, 0:256], op=ALU.add),
             r=[ST, PS[2]], w=[ST])
        if f == 0:
            em.A(lambda e: act(e, oo[:, :], PS[1][:, 0:256], AF.Copy), r=[PS[1]], w=[oo])
            em.dma(C.ODF.t[cs, :], oo[:, :], r=[oo], w=[C.ODF])
        else:
            em.V(lambda e: e.tensor_tensor(out=oo[:, :], in0=PS[1][:, 0:256], in1=ofc[:, :], op=ALU.add), r=[PS[1], ofc], w=[oo])
            for h in range(4):
                em.V(lambda e: e.bn_stats(out=BST[:, h * 6:(h + 1) * 6], in_=oo[:, h * 64:(h + 1) * 64]), r=[oo], w=[BST])
            for h in range(4):
                em.V(lambda e: e.bn_aggr(out=MV[:, 2 * h:2 * h + 2], in_=BST[:, h * 6:(h + 1) * 6]), r=[BST], w=[MV])
            em.V(lambda e: e.tensor_scalar(out=RS[:, 0:4], in0=MV[:, :].rearrange("p (h t) -> p h t", t=2)[:, :, 1], scalar1=LN_EPS, scalar2=None, op0=ALU.add),
                 r=[MV], w=[RS])
            em.A(lambda e: act(e, RS[:, :], RS[:, :], AF.Sqrt), r=[RS], w=[RS])
            em.V(lambda e: e.reciprocal(out=RS[:, :], in_=RS[:, :]), r=[RS], w=[RS])
            for h in range(4):
                em.V(lambda e: e.tensor_scalar(out=XN[:, h * 64:(h + 1) * 64], in0=oo[:, h * 64:(h + 1) * 64], scalar1=MV[:, 2 * h:2 * h + 1],
                                               scalar2=RS[:, h:h + 1], op0=ALU.subtract, op1=ALU.mult), r=[oo, MV, RS], w=[XN])
            em.G(lambda e: e.tensor_tensor(out=XN[:, :], in0=XN[:, :], in1=N0[:, :], op=ALU.mult), r=[XN, N0], w=[XN])
            em.G(lambda e: e.tensor_tensor(out=XN[:, :], in0=XN[:, :], in1=N1[:, :], op=ALU.add), r=[XN, N1], w=[XN])
            em.A(lambda e: act(e, SG[:, :], kv[:, 512:768], AF.Silu), r=[kv], w=[SG])
            em.V(lambda e: e.tensor_tensor(out=oo[:, :], in0=XN[:, :], in1=SG[:, :], op=ALU.mult), r=[XN, SG], w=[oo])
            em.dma(C.ODO.t[cs, :], oo[:, :], r=[oo], w=[C.ODO])
    em.end_stage()


def stage_merge(C, l):
    em = C.em
    em.begin_stage()
    PS = C.PS
    need_ctx = l < C.depth - 1
    Wg = em.sb("Wg", [128, 8, 4096], BF16)
    Wb = em.sb("Wb", [128, 8, D], BF16)
    Wo = em.sb("Wo", [128, 8, D], BF16)
    STG = [em.sb("STG%d" % i, [128, 1024]) for i in range(2)]
    MR = [[em.sb("MR%d_%d" % (ty, i), [128, D]) for i in range(3)] for ty in range(2)]
    LNG = em.sb("LNG", [128, D])
    LNB = em.sb("LNB", [128, D])
    HB = [em.sb("HB%d" % i, [128, D]) for i in range(2)]
    OTt = [em.sb("OTt%d" % i, [128, 6, 128]) for i in range(2)]
    ODt = [em.sb("ODt%d" % i, [128, 256]) for i in range(2)]
    OTb = em.sb("OTb", [128, 8, 128], BF16)
    TMP = em.sb("TMP", [128, D])
    U = em.sb("U", [128, D])
    UT = em.sb("UT", [128, 8, 128], BF16)
    GS = [em.sb("GS%d" % i, [128, 512]) for i in range(2)]
    TH = em.sb("TH", [128, 512])
    M = em.sb("M", [128, D])
    MT = em.sb("MT", [128, 8, 128], BF16)
    R = em.sb("R", [128, D])
    HO = [em.sb("HO%d" % i, [128, D]) for i in range(2)]
    BST = em.sb("BST", [128, 12])
    MV = em.sb("MV", [128, 2])
    RS = em.sb("RS", [128, 1])

    load_mod_rows(C, l, [0, 1, 2], MR, plus1=(1,))
    load_bcast_row(em, LNG, C.ln_attn.t[l, 0, :], D)
    load_bcast_row(em, LNB, C.ln_attn.t[l, 1, :], D)
    load_weight_bf16(em, Wg, C.w_in.t[l], 8, 4096, STG, col0=MIX)
    load_weight_bf16(em, Wb, C.w_branch.t[l], 8, D, STG)
    load_weight_bf16(em, Wo, C.w_out.t[l], 8, D, STG)

    gi = 0
    for b in range(0 if need_ctx else 2, C.NBx):
        ty = 1 if b < 2 else 0
        cs = slice(b * 128, (b + 1) * 128)
        H, ott, odt, ho = HB[b % 2], OTt[b % 2], ODt[b % 2], HO[b % 2]
        em.dma(H[:, :], hrows(C, l, b), r=[], w=[H])
        for bi, OT in enumerate((C.OTA, C.OTB, C.OTC)):
            otv = OT.t.rearrange("p (j e) l -> p j e l", e=2)
            em.dma(ott[0:64, 2 * bi:2 * bi + 2, :], otv[:, :, 0, cs], r=[], w=[ott])
            em.dma(ott[64:128, 2 * bi:2 * bi + 2, :], otv[:, :, 1, cs], r=[], w=[ott])
        em.dma(odt[:, :], C.ODO.t[cs, :], r=[], w=[odt])
        em.A(lambda e: act(e, OTb[:, 0:6, :], ott[:, :, :], AF.Copy), r=[ott], w=[OTb])
        for j in range(2):
            em.P(lambda e: e.transpose(out=PS[0][:, j * 128:(j + 1) * 128], in_=odt[:, j * 128:(j + 1) * 128], identity=C.IDN[:, :]), r=[odt, C.IDN], w=[PS[0]])
        em.V(lambda e: e.tensor_copy(out=OTb[:, 6:8, :], in_=PS[0][:, 0:256].rearrange("p (a b) -> p a b", b=128)), r=[PS[0]], w=[OTb])
        emit_modulate(em, U, H, MR[ty][1], MR[ty][0], TMP)
        emit_transpose8(em, UT, U, C.IDN, PS[0], PS[1])
        for c in range(2):
            for i in range(4):
                pg, pb, gs = PS[2 + gi % 2], PS[4 + gi % 2], GS[gi % 2]
                gi += 1
                col = i * 1024 + c * 512
                for k in range(8):
                    em.P(lambda e: e.matmul(pg[:, :], lhsT=UT[:, k, :], rhs=Wg[:, k, col:col + 512], start=(k == 0), stop=(k == 7)), r=[UT, Wg], w=[pg])
                em.A(lambda e: act(e, gs[:, :], pg[:, :], AF.Sigmoid), r=[pg], w=[gs])
                for kk in range(2):
                    em.P(lambda e: e.matmul(pb[:, :], lhsT=OTb[:, 2 * i + kk, :], rhs=Wb[:, 2 * i + kk, c * 512:(c + 1) * 512], start=(kk == 0), stop=(kk == 1)),
                         r=[OTb, Wb], w=[pb])
                if i == 0:
                    em.V(lambda e: e.tensor_tensor(out=M[:, c * 512:(c + 1) * 512], in0=gs[:, :], in1=pb[:, :], op=ALU.mult), r=[gs, pb], w=[M])
                else:
                    em.V(lambda e: e.tensor_tensor(out=TH[:, :], in0=gs[:, :], in1=pb[:, :], op=ALU.mult), r=[gs, pb], w=[TH])
                    em.G(lambda e: e.tensor_tensor(out=M[:, c * 512:(c + 1) * 512], in0=M[:, c * 512:(c + 1) * 512], in1=TH[:, :], op=ALU.add), r=[M, TH], w=[M])
        emit_transpose8(em, MT, M, C.IDN, PS[0], PS[1])
        for c in range(2):
            py = PS[6 + c]
            for k in range(8):
                em.P(lambda e: e.matmul(py[:, :], lhsT=MT[:, k, :], rhs=Wo[:, k, c * 512:(c + 1) * 512], start=(k == 0), stop=(k == 7)), r=[MT, Wo], w=[py])
            em.V(lambda e: e.tensor_tensor(out=R[:, c * 512:(c + 1) * 512], in0=py[:, :], in1=MR[ty][2][:, c * 512:(c + 1) * 512], op=ALU.mult), r=[py, MR[ty][2]], w=[R])
        em.V(lambda e: e.scalar_tensor_tensor(out=R[:, :], in0=H[:, :], scalar=float(ALPHA), in1=R[:, :], op0=ALU.mult, op1=ALU.add), r=[H, R], w=[R])
        emit_layernorm(em, ho, R, LNG, LNB, BST, MV, RS, TMP)
        em.dma(C.H1.t[cs, :], ho[:, :], r=[ho], w=[C.H1])
    em.end_stage()


def stage_cvt(C):
    em = C.em
    em.begin_stage()
    SF = [em.sb("SF%d" % i, [128, 4096]) for i in range(2)]
    SB = [em.sb("SB%d" % i, [128, 4096], BF16) for i in range(3)]
    it = 0
    for l in range(len(C.peer_u)):
        for (src, dst) in ((C.peer_u[l], C.TUb[l]), (C.peer_v[l], C.TVb[l])):
            sv = src.t.rearrange("(t p r) d -> t p (r d)", p=128, r=4)
            dv = dst.t.rearrange("(t p r) d -> t p (r d)", p=128, r=4)
            for t in range(32):
                sf, sbb = SF[it % 2], SB[it % 3]
                em.dma(sf[:, :], sv[t], r=[], w=[sf])
                if it % 3 == 0:
                    em.A(lambda e: act(e, sbb[:, :], sf[:, :], AF.Copy), r=[sf], w=[sbb])
                elif it % 3 == 1:
                    em.V(lambda e: e.tensor_copy(out=sbb[:, :], in_=sf[:, :]), r=[sf], w=[sbb])
                else:
                    em.G(lambda e: e.tensor_copy(out=sbb[:, :], in_=sf[:, :]), r=[sf], w=[sbb])
                em.dma(dv[t], sbb[:, :], r=[sbb], w=[dst])
                it += 1
    em.end_stage()


def stage_peer(C, l):
    em = C.em
    em.begin_stage()
    PS = C.PS
    need_ctx = l < C.depth - 1
    last = l == C.depth - 1
    Wq = em.sb("Wq", [128, 8, 2048])
    SKT = em.sb("SKT", [128, 16, 128])
    MR = [[em.sb("MR%d_%d" % (ty, i), [128, D]) for i in range(3)] for ty in range(2)]
    LNG = em.sb("LNG", [128, D])
    LNB = em.sb("LNB", [128, D])
    I16 = em.sb("I16", [128, 32])
    HB = [em.sb("HB%d" % i, [128, D]) for i in range(2)]
    TMP = em.sb("TMP", [128, D])
    X = em.sb("X", [128, D])
    XTt = em.sb("XTt", [128, 8, 128])
    QTt = em.sb("QTt", [128, 16, 128])
    SC = em.sb("SC", [128, 16, 128])
    SC2 = SC
    SV = em.sb("SV", [128, 16, 16])
    SI = em.sb("SI", [128, 16, 16], U32)
    SIF = em.sb("SIF", [128, 16, 16])
    CD = em.sb("CD", [128, 8, 256])
    CD2 = CD
    FV = em.sb("FV", [128, 8, 16])
    FP = em.sb("FP", [128, 8, 16], U32)
    PF = em.sb("PF", [128, 8, 16])
    PI = em.sb("PI", [128, 8, 16])
    PJ = em.sb("PJ", [128, 8, 16])
    EQ = em.sb("EQ", [128, 8, 16, 16])
    EI = em.sb("EI", [128, 8, 16])
    EJ = em.sb("EJ", [128, 8, 16])
    IDX = em.sb("IDX", [128, 128], I32)
    GT = em.sb("GT", [128, 8, 16])
    GSM = em.sb("GSM", [128, 8])
    AV = em.sb("AV", [128, 128])
    A2 = em.sb("A2", [128, 128])
    WT = em.sb("WT", [128, 128])
    JK = em.sb("JK", [128, D])
    GB = [em.sb("GB%d" % i, [128, D], BF16) for i in range(8)]
    Y = em.sb("Y", [128, D])
    R = em.sb("R", [128, D])
    HO = [em.sb("HO0", [128, D])] * 2
    BST = em.sb("BST", [128, 12])
    MV = em.sb("MV", [128, 2])
    RS = em.sb("RS", [128, 1])

    load_mod_rows(C, l, [3, 4, 5], MR, plus1=(4,))
    load_bcast_row(em, LNG, C.ln_ffn.t[l, 0, :], D)
    load_bcast_row(em, LNB, C.ln_ffn.t[l, 1, :], D)
    load_bcast_row(em, I16, C.iota16.t[:], 32)
    em.dma(Wq[:, :, :], C.peer_wq.t[l].rearrange("(k p) c -> p k c", p=128), r=[], w=[Wq])
    em.dma(SKT[:, :, :], C.skT.t[l], r=[], w=[SKT])
    if not getattr(C, "route_only", False):
        ut, vt = C.TUb[l].t[:, :], C.TVb[l].t[:, :]
    gbi = 0
    for b in range(0 if need_ctx else 2, C.NBx):
        ty = 1 if b < 2 else 0
        cs = slice(b * 128, (b + 1) * 128)
        H, ho = HB[b % 2], HO[b % 2]
        em.dma(H[:, :], C.H1.t[cs, :], r=[], w=[H])
        emit_modulate(em, X, H, MR[ty][1], MR[ty][0], TMP)
        emit_transpose8(em, XTt, X, C.IDN, PS[0], PS[1])
        for c4 in range(4):
            ps = PS[2 + c4 % 2]
            for cc in range(4):
                c = c4 * 4 + cc
                for k in range(8):
                    em.P(lambda e: e.matmul(ps[:, cc * 128:(cc + 1) * 128], lhsT=Wq[:, k, c * 128:(c + 1) * 128], rhs=XTt[:, k, :], start=(k == 0), stop=(k == 7)),
                         r=[Wq, XTt], w=[ps])
            em.A(lambda e: act(e, QTt[:, c4 * 4:c4 * 4 + 4, :], ps[:, :].rearrange("p (a b) -> p a b", b=128), AF.Copy), r=[ps], w=[QTt])
        for c4 in range(4):
            ps = PS[4 + c4 % 2]
            for cc in range(4):
                c = c4 * 4 + cc
                em.P(lambda e: e.matmul(ps[:, cc * 128:(cc + 1) * 128], lhsT=QTt[:, c, :], rhs=SKT[:, c, :], start=True, stop=True), r=[QTt, SKT], w=[ps])
            em.A(lambda e: act(e, SC[:, c4 * 4:c4 * 4 + 4, :], ps[:, :].rearrange("p (a b) -> p a b", b=128), AF.Copy), r=[ps], w=[SC])
        for c in range(16):
            em.V(lambda e: e.max(out=SV[:, c, 0:8], in_=SC[:, c, :]), r=[SC], w=[SV])
            em.V(lambda e: e.max_index(out=SI[:, c, 0:8], in_max=SV[:, c, 0:8], in_values=SC[:, c, :]), r=[SV, SC], w=[SI])
            em.V(lambda e: e.match_replace(out=SC2[:, c, :], in_to_replace=SV[:, c, 0:8], in_values=SC[:, c, :], imm_value=-1e30), r=[SV, SC], w=[SC2])
            em.V(lambda e: e.max(out=SV[:, c, 8:16], in_=SC2[:, c, :]), r=[SC2], w=[SV])
            em.V(lambda e: e.max_index(out=SI[:, c, 8:16], in_max=SV[:, c, 8:16], in_values=SC2[:, c, :]), r=[SV, SC2], w=[SI])
        em.V(lambda e: e.tensor_copy(out=SIF[:, :, :], in_=SI[:, :, :]), r=[SI], w=[SIF])
        svv = SV[:, :, :].rearrange("p (h t) k -> p h t k", t=2)
        sfv = SIF[:, :, :].rearrange("p (h t) k -> p h t k", t=2)
        cdv = CD[:, :, :].rearrange("p h (i j) -> p h i j", j=16)
        for h in range(8):
            em.V(lambda e: e.tensor_tensor(out=cdv[:, h, :, :], in0=svv[:, h, 0, :].unsqueeze(2).to_broadcast([128, 16, 16]),
                                           in1=svv[:, h, 1, :].unsqueeze(1).to_broadcast([128, 16, 16]), op=ALU.add), r=[SV], w=[CD])
        for h in range(8):
            em.V(lambda e: e.max(out=FV[:, h, 0:8], in_=CD[:, h, :]), r=[CD], w=[FV])
            em.V(lambda e: e.max_index(out=FP[:, h, 0:8], in_max=FV[:, h, 0:8], in_values=CD[:, h, :]), r=[FV, CD], w=[FP])
            em.V(lambda e: e.match_replace(out=CD2[:, h, :], in_to_replace=FV[:, h, 0:8], in_values=CD[:, h, :], imm_value=-1e30), r=[FV, CD], w=[CD2])
            em.V(lambda e: e.max(out=FV[:, h, 8:16], in_=CD2[:, h, :]), r=[CD2], w=[FV])
            em.V(lambda e: e.max_index(out=FP[:, h, 8:16], in_max=FV[:, h, 8:16], in_values=CD2[:, h, :]), r=[FV, CD2], w=[FP])
        em.V(lambda e: e.tensor_copy(out=PF[:, :, :], in_=FP[:, :, :]), r=[FP], w=[PF])
        i16b = I16[:, 16:32].unsqueeze(1).unsqueeze(1).to_broadcast([128, 8, 16, 16])
        i1b = I16[:, 0:16].unsqueeze(1).unsqueeze(1).to_broadcast([128, 8, 16, 16])
        em.V(lambda e: e.tensor_tensor(out=EQ[:, :, :, :], in0=PF[:, :, :].unsqueeze(3).to_broadcast([128, 8, 16, 16]), in1=i16b, op=ALU.is_ge), r=[PF, I16], w=[EQ])
        em.V(lambda e: e.tensor_reduce(out=PI[:, :, :], in_=EQ[:, :, :, :], axis=AX.X, op=ALU.add), r=[EQ], w=[PI])
        em.V(lambda e: e.tensor_scalar(out=PI[:, :, :], in0=PI[:, :, :], scalar1=-1.0, scalar2=None, op0=ALU.add), r=[PI], w=[PI])
        em.V(lambda e: e.scalar_tensor_tensor(out=PJ[:, :, :], in0=PI[:, :, :], scalar=-16.0, in1=PF[:, :, :], op0=ALU.mult, op1=ALU.add), r=[PI, PF], w=[PJ])
        for (PX, t, EX) in ((PI, 0, EI), (PJ, 1, EJ)):
            em.V(lambda e: e.tensor_tensor(out=EQ[:, :, :, :], in0=PX[:, :, :].unsqueeze(3).to_broadcast([128, 8, 16, 16]), in1=i1b, op=ALU.is_equal), r=[PX, I16], w=[EQ])
            em.V(lambda e: e.tensor_tensor(out=EQ[:, :, :, :], in0=EQ[:, :, :, :], in1=sfv[:, :, t, :].unsqueeze(2).to_broadcast([128, 8, 16, 16]), op=ALU.mult),
                 r=[EQ, SIF], w=[EQ])
            em.V(lambda e: e.tensor_reduce(out=EX[:, :, :], in_=EQ[:, :, :, :], axis=AX.X, op=ALU.add), r=[EQ], w=[EX])
        em.V(lambda e: e.scalar_tensor_tensor(out=EI[:, :, :], in0=EI[:, :, :], scalar=128.0, in1=EJ[:, :, :], op0=ALU.mult, op1=ALU.add), r=[EI, EJ], w=[EI])
        em.V(lambda e: e.tensor_copy(out=IDX[:, :], in_=EI[:, :, :].rearrange("p h k -> p (h k)")), r=[EI], w=[IDX])
        em.V(lambda e: e.tensor_tensor(out=GT[:, :, :], in0=FV[:, :, :], in1=FV[:, :, 0:1].to_broadcast([128, 8, 16]), op=ALU.subtract), r=[FV], w=[GT])
        em.A(lambda e: act(e, GT[:, :, :], GT[:, :, :], AF.Exp), r=[GT], w=[GT])
        em.V(lambda e: e.tensor_reduce(out=GSM[:, :], in_=GT[:, :, :], axis=AX.X, op=ALU.add), r=[GT], w=[GSM])
        em.V(lambda e: e.reciprocal(out=GSM[:, :], in_=GSM[:, :]), r=[GSM], w=[GSM])
        em.V(lambda e: e.tensor_tensor(out=GT[:, :, :], in0=GT[:, :, :], in1=GSM[:, :].unsqueeze(2).to_broadcast([128, 8, 16]), op=ALU.mult), r=[GT, GSM], w=[GT])
        if getattr(C, "route_only", False):
            for (tl_, ap_, o_, n_) in ((SV, SV[:, :, :].rearrange("p a b -> p (a b)"), 0, 256), (SIF, SIF[:, :, :].rearrange("p a b -> p (a b)"), 256, 256),
                                       (FV, FV[:, :, :].rearrange("p a b -> p (a b)"), 512, 128), (PF, PF[:, :, :].rearrange("p a b -> p (a b)"), 640, 128),
                                       (EI, EI[:, :, :].rearrange("p a b -> p (a b)"), 768, 128), (GT, GT[:, :, :].rearrange("p a b -> p (a b)"), 896, 128),
                                       (PI, PI[:, :, :].rearrange("p a b -> p (a b)"), 1024, 128), (PJ, PJ[:, :, :].rearrange("p a b -> p (a b)"), 1152, 128),
                                       (X, X[:, :], 2048, 1024)):
                em.dma(C.pdbg.t[:, o_:o_ + n_], ap_, r=[tl_], w=[C.pdbg])
            break
        for r_ in range(128):
            gb = GB[gbi % 8]
            gbi += 1
            em.dma(None, None, r=[IDX], w=[gb], q="pool",
                   fn=lambda q: q.indirect_dma_start(out=gb[:, :], out_offset=None, in_=ut, in_offset=bass.IndirectOffsetOnAxis(ap=IDX[:, r_:r_ + 1], axis=0), bounds_check=C.breg, oob_is_err=False))
            em.V(lambda e: e.scalar_tensor_tensor(out=JK[:, :], in0=gb[:, :], scalar=1.0, in1=X[:, :], op0=ALU.mult, op1=ALU.mult, accum_out=AV[:, r_:r_ + 1]),
                 r=[gb, X], w=[JK, AV])
        em.V(lambda e: e.tensor_tensor(out=A2[:, :], in0=AV[:, :], in1=AV[:, :], op=ALU.mult), r=[AV], w=[A2])
        em.V(lambda e: e.tensor_tensor(out=A2[:, :], in0=A2[:, :], in1=AV[:, :], op=ALU.mult), r=[A2, AV], w=[A2])
        em.V(lambda e: e.scalar_tensor_tensor(out=A2[:, :], in0=A2[:, :], scalar=0.044715, in1=AV[:, :], op0=ALU.mult, op1=ALU.add), r=[A2, AV], w=[A2])
        em.A(lambda e: act(e, A2[:, :], A2[:, :], AF.Tanh, scale=0.7978845608028654), r=[A2], w=[A2])
        em.V(lambda e: e.scalar_tensor_tensor(out=A2[:, :], in0=A2[:, :], scalar=1.0, in1=AV[:, :], op0=ALU.add, op1=ALU.mult), r=[A2, AV], w=[A2])
        em.V(lambda e: e.scalar_tensor_tensor(out=WT[:, :], in0=A2[:, :], scalar=0.5, in1=GT[:, :, :].rearrange("p h k -> p (h k)"), op0=ALU.mult, op1=ALU.mult),
             r=[A2, GT], w=[WT])
        for r_ in range(128):
            gb = GB[gbi % 8]
            gbi += 1
            em.dma(None, None, r=[IDX], w=[gb], q="pool",
                   fn=lambda q: q.indirect_dma_start(out=gb[:, :], out_offset=None, in_=vt, in_offset=bass.IndirectOffsetOnAxis(ap=IDX[:, r_:r_ + 1], axis=0), bounds_check=C.breg, oob_is_err=False))
            if r_ == 0:
                em.V(lambda e: e.tensor_scalar(out=Y[:, :], in0=gb[:, :], scalar1=WT[:, 0:1], scalar2=None, op0=ALU.mult), r=[gb, WT], w=[Y])
            else:
                em.V(lambda e: e.scalar_tensor_tensor(out=Y[:, :], in0=gb[:, :], scalar=WT[:, r_:r_ + 1], in1=Y[:, :], op0=ALU.mult, op1=ALU.add), r=[gb, WT, Y], w=[Y])
        em.G(lambda e: e.tensor_tensor(out=R[:, :], in0=Y[:, :], in1=MR[ty][2][:, :], op=ALU.mult), r=[Y, MR[ty][2]], w=[R])
        em.V(lambda e: e.scalar_tensor_tensor(out=R[:, :], in0=H[:, :], scalar=float(ALPHA), in1=R[:, :], op0=ALU.mult, op1=ALU.add), r=[H, R], w=[R])
        emit_layernorm(em, ho, R, LNG, LNB, BST, MV, RS, TMP)
        if last:
            em.dma(C.out.t[(b - 2) * 128:(b - 1) * 128, :], ho[:, :], r=[ho], w=[C.out])
        else:
            em.dma(C.H2.t[cs, :], ho[:, :], r=[ho], w=[C.H2])
    em.end_stage()


STAGES = ["mod", "proj", "attnA", "attnB0", "attnB1", "attnC", "retf", "retb", "merge", "peer"]


def build_all(S, depth=DEPTH, debug=False, stop=None):
    nc = bass.Bass("TRN2", target_bir_lowering=False)
    Lx = NCTX + S
    with ExitStack() as st:
        em = Em(nc, st)
        C = Ctx()
        C.em, C.S, C.Lx, C.NBx, C.depth = em, S, Lx, Lx // 128, depth
        C.x = em.dram("x", [S, D])
        C.ctx = em.dram("ctx", [NCTX, D])
        C.cT = em.dram("cT", [D, 2])
        C.w_mod = em.dram("w_mod", [DEPTH, D, 6 * D])
        C.b_mod = em.dram("b_mod", [DEPTH, 6 * D])
        C.w_in = em.dram("w_in", [DEPTH, D, 6912])
        C.gain = em.dram("gain", [DEPTH, 384])
        C.diff_lambda = em.dram("diff_lambda", [DEPTH, 128])
        C.diff_subln = em.dram("diff_subln", [DEPTH, 64])
        C.win_sink = em.dram("win_sink", [DEPTH, 4])
        C.ret_decay = em.dram("ret_decay", [DEPTH, 8])
        C.ret_norm = em.dram("ret_norm", [DEPTH, 2, 256])
        C.w_branch = em.dram("w_branch", [DEPTH, 1024, D])
        C.w_out = em.dram("w_out", [DEPTH, D, D])
        C.ln_attn = em.dram("ln_attn", [DEPTH, 2, D])
        C.ln_ffn = em.dram("ln_ffn", [DEPTH, 2, D])
        C.peer_wq = em.dram("peer_wq", [DEPTH, D, 2048])
        C.skT = em.dram("skT", [DEPTH, 128, 16, 128])
        npeer = DEPTH if stop is None else (stop[0] + 1 if stop[1] == "peer" else stop[0])
        C.peer_u = [em.dram("peer_u%d" % i, [16384, D]) for i in range(npeer)]
        C.peer_v = [em.dram("peer_v%d" % i, [16384, D]) for i in range(npeer)]
        C.TUb = [em.dram("TUb%d" % i, [16384, D], BF16, kind="Internal") for i in range(npeer)]
        C.TVb = [em.dram("TVb%d" % i, [16384, D], BF16, kind="Internal") for i in range(npeer)]
        C.ident = em.dram("ident", [128, 128])
        C.tab = em.dram("tab", [Lx, 160])
        C.mk = em.dram("mk", [6, 128, 512])
        C.rc = em.dram("rc", [128, 5, 128])
        C.pidx = em.dram("pidx", [128, 2])
        C.iota16 = em.dram("iota16", [32])
        C.out = em.dram("out", [S, D], kind="ExternalOutput")
        sk = "ExternalOutput" if debug else "Internal"
        C.mod = em.dram("mod", [DEPTH, 2, 6 * D], kind=sk)
        C.P = em.dram("P", [Lx, MIX], kind=sk)
        C.XT = em.dram("XT", [64, 28, Lx], kind=sk)
        C.VPs = em.dram("VPs", [Lx, 8, 65], kind=sk)
        C.OTA = em.dram("OTA", [64, 4, Lx], kind=sk)
        C.OTB = em.dram("OTB", [64, 4, Lx], kind=sk)
        C.OTC = em.dram("OTC", [64, 4, Lx], kind=sk)
        C.ODF = em.dram("ODF", [Lx, 256], kind=sk)
        C.ODO = em.dram("ODO", [Lx, 256], kind=sk)
        C.H1 = em.dram("H1", [Lx, D], kind=sk)
        C.H2 = em.dram("H2", [Lx, D], kind=sk)
        C.route_only = stop is not None and stop[1] == "route"
        if C.route_only:
            C.pdbg = em.dram("pdbg", [128, 4096], kind="ExternalOutput")
        C.PS = [em.ps("PS%d" % i) for i in range(8)]
        C.IDN = em.sb("IDN", [128, 128])
        C.ONES = em.sb("ONES", [128, 128])
        C.RMB = em.sb("RMB", [128, 28])
        em.dma(C.IDN[:, :], C.ident.t[:, :], r=[], w=[C.IDN])
        C.breg = nc.gpsimd.to_reg(16383)
        em.V(lambda e: e.memset(C.ONES[:, :], 1.0), w=[C.ONES])

        def run_stages():
            stage_mod(C)
            if stop == (0, "mod"):
                return
            stage_cvt(C)
            for l in range(depth):
                seq = [("proj", lambda: stage_proj(C, l)), ("attnA", lambda: stage_attn(C, l, "A")), ("attnB0", lambda: stage_attn(C, l, "B", 0)),
                       ("attnB1", lambda: stage_attn(C, l, "B", 1)), ("attnC", lambda: stage_attn(C, l, "C")), ("retf", lambda: stage_ret(C, l, 0)),
                       ("retb", lambda: stage_ret(C, l, 1)), ("merge", lambda: stage_merge(C, l)), ("route" if C.route_only else "peer", lambda: stage_peer(C, l))]
                for name, fn in seq:
                    fn()
                    if stop == (l, name):
                        return

        run_stages()
        em.finish()
        C.ninst = em.ninst
    nc._ninst = C.ninst
    nc._inputs = list(em.inputs)
    return nc


def host_tables(S):
    theta = np.float32(10000.0)
    Lx = NCTX + S
    tab = np.zeros((Lx, 160), np.float32)
    i = np.arange(S)
    row = (i // GRID_W).astype(np.float32)
    col = (i % GRID_W).astype(np.float32)

    def inv(n):
        return (theta ** (-np.arange(n, dtype=np.float32) / np.float32(n))).astype(np.float32)

    a64 = np.stack([row[:, None] * inv(16)[None], col[:, None] * inv(16)[None]], 1).astype(np.float32)
    a32 = np.stack([row[:, None] * inv(8)[None], col[:, None] * inv(8)[None]], 1).astype(np.float32)
    tab[:NCTX, 0:32] = 1.0
    tab[:NCTX, 64:80] = 1.0
    tab[NCTX:, 0:32] = np.cos(a64).reshape(S, 32)
    tab[NCTX:, 32:64] = np.sin(a64).reshape(S, 32)
    tab[NCTX:, 64:80] = np.cos(a32).reshape(S, 16)
    tab[NCTX:, 80:96] = np.sin(a32).reshape(S, 16)
    pos = np.arange(Lx, dtype=np.float32)
    ang = (pos[:, None] * inv(32)[None]).astype(np.float32)
    tab[:, 96:128] = np.cos(ang)
    tab[:, 128:160] = np.sin(ang)
    return tab


def host_consts(S):
    tab = host_tables(S)
    j = np.arange(128)[:, None]
    i = np.arange(128)[None, :]
    rc = np.zeros((128, 5, 128), np.float32)
    rc[:, 0] = i - j
    rc[:, 1] = (i >= j)
    rc[:, 2] = (j >= i)
    rc[:, 3] = np.broadcast_to(i + 1, (128, 128))
    rc[:, 4] = np.broadcast_to(128 - i, (128, 128))
    pidx = np.stack([127 - np.arange(128), np.arange(128)], 1).astype(np.float32)
    mk = np.zeros((6, 128, 512), np.float32)
    k = np.arange(128)[:, None]
    q = np.arange(128)[None, :]
    for tp in range(6):
        for qb in range(4):
            rel = (tp - 1) - qb
            if rel == -1:
                mk[tp, :, qb * 128:(qb + 1) * 128] = (k >= q)
            elif rel == 0:
                mk[tp, :, qb * 128:(qb + 1) * 128] = 1.0
            elif rel == 1:
                mk[tp, :, qb * 128:(qb + 1) * 128] = (k <= q)
    iota16 = np.concatenate([np.arange(16), 16 * np.arange(16)]).astype(np.float32)
    return {"tab": tab, "rc": rc, "pidx": pidx, "mk": mk, "iota16": iota16, "ident": np.eye(128, dtype=np.float32)}


def make_in_maps(inp, S):
    f = lambda a: np.ascontiguousarray(np.asarray(a, dtype=np.float32))
    cst = host_consts(S)
    g = np.asarray(inp["qk_gain"], np.float32)
    gain = np.stack([np.concatenate([np.tile(g[l, 0], 4), np.tile(g[l, 1], 2)]) for l in range(DEPTH)]).astype(np.float32)
    sk = np.asarray(inp["peer_subkeys"], np.float32)
    skT = np.ascontiguousarray(sk.reshape(DEPTH, 16, 128, 128).transpose(0, 3, 1, 2))
    shared = {
        "w_mod": f(inp["w_mod"]), "b_mod": f(inp["b_mod"]), "w_in": f(inp["w_in"]), "gain": gain,
        "diff_lambda": f(np.asarray(inp["diff_lambda"]).reshape(DEPTH, 128)), "diff_subln": f(inp["diff_subln"]),
        "win_sink": f(inp["win_sink"]), "ret_decay": f(np.asarray(inp["ret_decay"]).reshape(DEPTH, 8)), "ret_norm": f(inp["ret_norm"]),
        "w_branch": f(np.asarray(inp["w_branch"]).reshape(DEPTH, 1024, D)), "w_out": f(inp["w_out"]), "ln_attn": f(inp["ln_attn"]),
        "ln_ffn": f(inp["ln_ffn"]), "peer_wq": f(inp["peer_wq"]), "skT": skT,
    }
    for l in range(DEPTH):
        shared["peer_u%d" % l] = f(np.asarray(inp["peer_u"])[l])
        shared["peer_v%d" % l] = f(np.asarray(inp["peer_v"])[l])
    shared.update(cst)
    maps = []
    for b in range(2):
        m = dict(shared)
        m["x"] = f(inp["x"][b])
        m["ctx"] = f(inp["ctx"][b])
        m["cT"] = f(np.stack([np.asarray(inp["c"])[b], np.asarray(inp["c_ctx"])], 1))
        maps.append(m)
    return maps


def kernel(**inp):
    S = int(np.asarray(inp["x"]).shape[1])
    nc = build_all(S)
    maps = make_in_maps(inp, S)
    res = run_bass_kernel_spmd(nc, maps, core_ids=[0, 1])
    return np.stack([res.results[b]["out"] for b in range(2)], 0).astype(np.float32)
```

```python
import math
from contextlib import ExitStack
import numpy as np
import concourse.bass as bass
import concourse.mybir as mybir
from concourse.bass_utils import run_bass_kernel_spmd

F32 = mybir.dt.float32
BF16 = mybir.dt.bfloat16
U32 = mybir.dt.uint32
I32 = mybir.dt.int32
AF = mybir.ActivationFunctionType
ALU = mybir.AluOpType
AX = mybir.AxisListType

D = 1024
NCTX = 256
GRID_W = 64
MIX = 2816
DEPTH = 2
ALPHA = (2 * DEPTH) ** 0.25
LN_EPS = 1e-5
RMS_EPS = 1e-6
NCORES = 8


class Tl:
    def __init__(self, t, name):
        self.t, self.name = t, name
        self.w = {}
        self.r = {}
        self.dkey = None
        self.is_dram = False

    def __getitem__(self, k):
        return self.t[k]


class Em:
    def __init__(self, nc, st):
        self.nc, self.gst = nc, st
        self.st = st
        self.eng = {"pe": nc.tensor, "dve": nc.vector, "act": nc.scalar, "pool": nc.gpsimd, "sp": nc.sync}
        self.sem, self.cnt = {}, {}
        for k in ("pe", "dve", "act", "pool"):
            self.sem[k] = st.enter_context(nc.semaphore("s_" + k))
            self.cnt[k] = 0
        self.seen = {k: {} for k in self.eng}
        self.nd = 0
        self.free_dkeys = []
        self.stage_tiles = []
        self.ninst = 0

    def sb(self, name, shape, dtype=F32):
        self.nalloc = getattr(self, "nalloc", 0) + 1
        t = Tl(self.st.enter_context(self.nc.sbuf_tensor("%s_%d" % (name, self.nalloc), list(shape), dtype)), name)
        self.stage_tiles.append(t)
        return t

    def ps(self, name, shape=(128, 512), dtype=F32):
        return Tl(self.gst.enter_context(self.nc.psum_tensor(name, list(shape), dtype)), name)

    def dram(self, name, shape, dtype=F32, kind="ExternalInput"):
        t = Tl(self.nc.dram_tensor(name, list(shape), dtype, kind=kind).ap(), name)
        t.is_dram = True
        if kind == "ExternalInput":
            self.inputs = getattr(self, "inputs", []) + [name]
        return t

    def begin_stage(self):
        self.st = ExitStack()
        self.stage_tiles = []

    def end_stage(self):
        self.barrier()
        for t in self.stage_tiles:
            if t.dkey is not None:
                self.free_dkeys.append(t.dkey)
        self.st.close()
        self.st = self.gst
        self.stage_tiles = []

    def _dkey(self, tl):
        if tl.dkey is None:
            if self.free_dkeys:
                tl.dkey = self.free_dkeys.pop()
            else:
                tl.dkey = "d%d" % self.nd
                self.nd += 1
                self.sem[tl.dkey] = self.gst.enter_context(self.nc.semaphore("s_" + tl.dkey))
                self.cnt[tl.dkey] = 0
        return tl.dkey

    def _waits(self, e, r, w):
        need = {}

        def nd(d):
            for k, c in d.items():
                if e == "pe" and k == "pe":
                    continue
                if c > need.get(k, 0):
                    need[k] = c

        for t in r:
            if not t.is_dram:
                nd(t.w)
        for t in w:
            if not t.is_dram:
                nd(t.w)
                nd(t.r)
        seen = self.seen[e]
        for k, c in need.items():
            if seen.get(k, 0) >= c:
                continue
            seen[k] = c
            self.eng[e].wait_ge(self.sem[k], c)
            self.ninst += 1

    def _record(self, key, c, r, w):
        for t in w:
            if not t.is_dram:
                t.w = {key: c}
                t.r = {}
        for t in r:
            if not t.is_dram and t not in w:
                if c > t.r.get(key, 0):
                    t.r[key] = c

    def op(self, e, fn, r=(), w=()):
        self._waits(e, r, w)
        ins = fn(self.eng[e])
        self.cnt[e] += 1
        self.ninst += 1
        ins.then_inc(self.sem[e], 1)
        self._record(e, self.cnt[e], r, w)

    def V(self, fn, r=(), w=()):
        self.op("dve", fn, r, w)

    def A(self, fn, r=(), w=()):
        self.op("act", fn, r, w)

    def G(self, fn, r=(), w=()):
        self.op("pool", fn, r, w)

    def P(self, fn, r=(), w=()):
        self.op("pe", fn, r, w)

    def dma(self, out_ap, in_ap, r, w, q="sp", fn=None):
        sbt = None
        for t in list(w) + list(r):
            if not t.is_dram:
                sbt = t
                break
        self._waits(q, r, w)
        key = self._dkey(sbt)
        if fn is None:
            ins = self.eng[q].dma_start(out=out_ap, in_=in_ap)
        else:
            ins = fn(self.eng[q])
        self.cnt[key] += 16
        self.ninst += 1
        ins.then_inc(self.sem[key], 16)
        self._record(key, self.cnt[key], r, w)

    def barrier(self):
        for e in ("sp", "pe", "dve", "act", "pool"):
            seen = self.seen[e]
            for k, c in self.cnt.items():
                if c > 0 and seen.get(k, 0) < c:
                    seen[k] = c
                    self.eng[e].wait_ge(self.sem[k], c)
                    self.ninst += 1

    def finish(self):
        self.barrier()


def act(e, out, in_, func, **kw):
    return e.activation(out=out, in_=in_, func=func, **kw)


def load_bcast_row(em, dst, src_ap, n, parts=128):
    em.dma(dst[0:parts, 0:n], src_ap.partition_broadcast(parts), r=[], w=[dst])


def emit_modulate(em, U, H, SC1, SH, TMP):
    em.V(lambda e: e.tensor_tensor(out=TMP[:, :], in0=H[:, :], in1=SC1[:, :], op=ALU.mult), r=[H, SC1], w=[TMP])
    em.G(lambda e: e.tensor_tensor(out=U[:, :], in0=TMP[:, :], in1=SH[:, :], op=ALU.add), r=[TMP, SH], w=[U])


def emit_transpose8(em, UT, U, IDN, PSA, PSB, ncols=D):
    nk = ncols // 128
    k = 0
    flip = 0
    while k < nk:
        ps = PSA if flip == 0 else PSB
        nn = min(4, nk - k)
        for j in range(nn):
            em.P(lambda e, j=j, k=k, ps=ps: e.transpose(out=ps[:, j * 128:(j + 1) * 128], in_=U[:, (k + j) * 128:(k + j + 1) * 128],
                                                         identity=IDN[:, :]), r=[U, IDN], w=[ps])
        fn = (lambda e, k=k, nn=nn, ps=ps: act(e, UT[:, k:k + nn, :], ps[:, 0:nn * 128].rearrange("p (a b) -> p a b", b=128), AF.Copy))
        if flip == 0:
            em.A(fn, r=[ps], w=[UT])
        else:
            em.V(lambda e, k=k, nn=nn, ps=ps: e.tensor_copy(out=UT[:, k:k + nn, :], in_=ps[:, 0:nn * 128].rearrange("p (a b) -> p a b", b=128)),
                 r=[ps], w=[UT])
        k += nn
        flip ^= 1


def emit_layernorm(em, OUT, R, GAM, BET, ST, MV, RS, TMP):
    for c in range(2):
        em.V(lambda e, c=c: e.bn_stats(out=ST[:, c * 6:(c + 1) * 6], in_=R[:, c * 512:(c + 1) * 512]), r=[R], w=[ST])
    em.V(lambda e: e.bn_aggr(out=MV[:, 0:2], in_=ST[:, 0:12]), r=[ST], w=[MV])
    em.V(lambda e: e.tensor_scalar(out=RS[:, 0:1], in0=MV[:, 1:2], scalar1=LN_EPS, scalar2=None, op0=ALU.add), r=[MV], w=[RS])
    em.A(lambda e: act(e, RS[:, 0:1], RS[:, 0:1], AF.Sqrt), r=[RS], w=[RS])
    em.V(lambda e: e.reciprocal(out=RS[:, 0:1], in_=RS[:, 0:1]), r=[RS], w=[RS])
    em.V(lambda e: e.tensor_scalar(out=TMP[:, :], in0=R[:, :], scalar1=MV[:, 0:1], scalar2=RS[:, 0:1], op0=ALU.subtract, op1=ALU.mult),
         r=[R, MV, RS], w=[TMP])
    em.G(lambda e: e.tensor_tensor(out=TMP[:, :], in0=TMP[:, :], in1=GAM[:, :], op=ALU.mult), r=[TMP, GAM], w=[TMP])
    em.V(lambda e: e.tensor_tensor(out=OUT[:, :], in0=TMP[:, :], in1=BET[:, :], op=ALU.add), r=[TMP, BET], w=[OUT])


def load_weight_bf16(em, W, src_ap, nk, ncols, STG, col0=0, dummy=None):
    i = 0
    stw = STG[0].t.shape[1]
    for k in range(nk):
        c = 0
        while c < ncols:
            cw = min(stw, ncols - c)
            stg = STG[i % len(STG)]
            em.dma(stg[:, 0:cw], src_ap[k * 128:(k + 1) * 128, col0 + c:col0 + c + cw], r=[], w=[stg])
            if i % 2 == 0:
                em.A(lambda e, k=k, c=c, cw=cw, stg=stg: act(e, W[:, k, c:c + cw], stg[:, 0:cw], AF.Copy), r=[stg], w=[W])
            else:
                em.V(lambda e, k=k, c=c, cw=cw, stg=stg: e.tensor_copy(out=W[:, k, c:c + cw], in_=stg[:, 0:cw]), r=[stg], w=[W])
            c += cw
            i += 1


class Ctx:
    pass


ROT_GROUPS = [
    (0, 6, 2, 16, 0, 32),
    (512, 16, 2, 8, 64, 80),
    (1280, 6, 2, 16, 0, 32),
    (1792, 8, 1, 32, 96, 128),
]
COPY_COLS = [(384, 512), (1024, 1280), (1664, 1792), (2304, 2816)]
NRM_GROUPS = [(0, 6, 64, 0), (512, 16, 32, 6), (1280, 6, 64, 22)]
XT_BLOCKS = [0, 128, 256, 512, 640, 768, 896, 1280, 1408, 1536, 1792, 1920, 2048, 2176]
V_GROUPS = [(384, 2, 0), (1024, 4, 2), (1664, 2, 6)]


def hrows(C, l, blk):
    if l == 0:
        if blk < 2:
            return C.ctx.t[blk * 128:(blk + 1) * 128, :]
        return C.x.t[(blk - 2) * 128:(blk - 1) * 128, :]
    return C.H2.t[blk * 128:(blk + 1) * 128, :]


def stage_mod(C):
    em = C.em
    em.begin_stage()
    CT = em.sb("CT", [128, 8, 2])
    WS = [em.sb("WS%d" % i, [128, 8, 512]) for i in range(2)]
    BB = em.sb("BB", [2, 6 * D])
    OO = em.sb("OO", [2, 6 * D])
    em.dma(CT[:, :, :], C.cT.t.rearrange("(k p) r -> p k r", p=128), r=[], w=[CT])
    em.A(lambda e: act(e, CT[:, :, :], CT[:, :, :], AF.Silu), r=[CT], w=[CT])
    it = 0
    for l in range(C.depth):
        em.dma(BB[:, :], C.b_mod.t[l, :].partition_broadcast(2), r=[], w=[BB])
        for g in range(12):
            ws, ps = WS[it % 2], C.PS[it % 2]
            em.dma(ws[:, :, :], C.w_mod.t[l, :, g * 512:(g + 1) * 512].rearrange("(k p) c -> p k c", p=128), r=[], w=[ws])
            for k in range(8):
                em.P(lambda e: e.matmul(ps[0:2, :], lhsT=CT[:, k, :], rhs=ws[:, k, :], start=(k == 0), stop=(k == 7)), r=[CT, ws], w=[ps])
            em.V(lambda e: e.tensor_tensor(out=OO[:, g * 512:(g + 1) * 512], in0=ps[0:2, :], in1=BB[:, g * 512:(g + 1) * 512], op=ALU.add),
                 r=[ps, BB], w=[OO])
            it += 1
        em.dma(C.mod.t[l, :, :], OO[:, :], r=[OO], w=[C.mod])
    em.end_stage()


def load_mod_rows(C, l, idxs, tiles, plus1=()):
    em = C.em
    for ty in range(2):
        for i, mi in enumerate(idxs):
            t = tiles[ty][i]
            em.dma(t[:, :], C.mod.t[l, ty, mi * D:(mi + 1) * D].partition_broadcast(128), r=[], w=[t])
            if mi in plus1:
                em.V(lambda e: e.tensor_scalar(out=t[:, :], in0=t[:, :], scalar1=1.0, scalar2=None, op0=ALU.add), r=[t], w=[t])


def stage_proj(C, l):
    em = C.em
    em.begin_stage()
    PS = C.PS
    W = em.sb("W", [128, 8, MIX], BF16)
    STG = [em.sb("STG%d" % i, [128, 2048]) for i in range(2)]
    GN = em.sb("GN", [128, 384])
    MR = [[em.sb("MR%d_%d" % (ty, i), [128, D]) for i in range(2)] for ty in range(2)]
    HB = [em.sb("HB%d" % i, [128, D]) for i in range(2)]
    TB = [em.sb("TB%d" % i, [128, 160]) for i in range(2)]
    TMP = em.sb("TMP", [128, D])
    U = em.sb("U", [128, D])
    UT = em.sb("UT", [128, 8, 128], BF16)
    PSB = em.sb("PSB", [128, MIX])
    PO = [em.sb("PO%d" % i, [128, MIX]) for i in range(2)]
    NR = em.sb("NR", [128, 28])
    RM = em.sb("RM", [128, 28])
    XTo = [em.sb("XTo%d" % i, [128, 14, 128]) for i in range(2)]
    VPo = [em.sb("VPo%d" % i, [128, 8, 65]) for i in range(2)]
    SQ = em.sb("SQ", [128, 512])
    SS = em.sb("SS", [128, 8])
    T1 = em.sb("T1", [128, 256])
    T2 = em.sb("T2", [128, 256])
    T3 = em.sb("T3", [128, 256])
    T4 = em.sb("T4", [128, 256])
    RX = em.sb("RX", [28, 1])
    RR = em.sb("RR", [1, 28])

    load_bcast_row(em, GN, C.gain.t[l, :], 384)
    load_mod_rows(C, l, [0, 1], MR, plus1=(1,))
    for i in range(2):
        em.G(lambda e: e.memset(VPo[i][:, :, :], 1.0), w=[VPo[i]])
    load_weight_bf16(em, W, C.w_in.t[l], 8, MIX, STG)

    pmi = 0
    xt4 = C.XT.t.rearrange("p (j e) l -> p j e l", e=2)
    for b in range(C.NBx):
        ty = 1 if b < 2 else 0
        H, T, PO_, XTo_, VPo_ = HB[b % 2], TB[b % 2], PO[b % 2], XTo[b % 2], VPo[b % 2]
        em.dma(H[:, :], hrows(C, l, b), r=[], w=[H])
        em.dma(T[:, :], C.tab.t[b * 128:(b + 1) * 128, :], r=[], w=[T])
        emit_modulate(em, U, H, MR[ty][1], MR[ty][0], TMP)
        emit_transpose8(em, UT, U, C.IDN, PS[0], PS[1])
        c = 0
        while c < MIX:
            cw = min(512, MIX - c)
            ps = PS[2 + pmi % 3]
            pmi += 1
            for k in range(8):
                em.P(lambda e: e.matmul(ps[:, 0:cw], lhsT=UT[:, k, :], rhs=W[:, k, c:c + cw], start=(k == 0), stop=(k == 7)), r=[UT, W], w=[ps])
            em.A(lambda e: act(e, PSB[:, c:c + cw], ps[:, 0:cw], AF.Copy), r=[ps], w=[PSB])
            c += cw
        v384 = PSB[:, 0:384].rearrange("p (h d) -> p h d", d=64)
        em.A(lambda e: act(e, SQ[:, 0:384], PSB[:, 0:384], AF.Square), r=[PSB], w=[SQ])
        em.V(lambda e: e.tensor_reduce(out=SS[:, 0:6], in_=SQ[:, 0:384].rearrange("p (h d) -> p h d", d=64), axis=AX.X, op=ALU.add), r=[SQ], w=[SS])
        em.V(lambda e: e.tensor_scalar(out=SS[:, 0:6], in0=SS[:, 0:6], scalar1=1.0 / 64, scalar2=RMS_EPS, op0=ALU.mult, op1=ALU.add), r=[SS], w=[SS])
        em.A(lambda e: act(e, SS[:, 0:6], SS[:, 0:6], AF.Sqrt), r=[SS], w=[SS])
        em.V(lambda e: e.reciprocal(out=SS[:, 0:6], in_=SS[:, 0:6]), r=[SS], w=[SS])
        em.V(lambda e: e.tensor_tensor(out=v384, in0=v384, in1=SS[:, 0:6].unsqueeze(2).to_broadcast([128, 6, 64]), op=ALU.mult), r=[PSB, SS], w=[PSB])
        em.V(lambda e: e.tensor_tensor(out=PSB[:, 0:384], in0=PSB[:, 0:384], in1=GN[:, :], op=ALU.mult), r=[PSB, GN], w=[PSB])
        for (c0, c1) in COPY_COLS:
            em.G(lambda e: e.tensor_copy(out=PO_[:, c0:c1], in_=PSB[:, c0:c1]), r=[PSB], w=[PO_])
        for (c0, nh, a, q, co, so) in ROT_GROUPS:
            n = nh * 2 * a * q

            def xv(tl, f):
                return tl[:, c0:c0 + n].rearrange("p (h a f q) -> p h a f q", a=a, f=2, q=q)[:, :, :, f, :]

            def tv(off):
                return T[:, off:off + a * q].rearrange("p (a q) -> p a q", a=a).unsqueeze(1).to_broadcast([128, nh, a, q])

            def tmpv(tl):
                return tl[:, 0:n // 2].rearrange("p (h a q) -> p h a q", a=a, q=q)

            em.V(lambda e: e.tensor_tensor(out=tmpv(T1), in0=xv(PSB, 0), in1=tv(co), op=ALU.mult), r=[PSB, T], w=[T1])
            em.G(lambda e: e.tensor_tensor(out=tmpv(T2), in0=xv(PSB, 1), in1=tv(so), op=ALU.mult), r=[PSB, T], w=[T2])
            em.V(lambda e: e.tensor_tensor(out=xv(PO_, 0), in0=tmpv(T1), in1=tmpv(T2), op=ALU.subtract), r=[T1, T2], w=[PO_])
            em.G(lambda e: e.tensor_tensor(out=tmpv(T3), in0=xv(PSB, 1), in1=tv(co), op=ALU.mult), r=[PSB, T], w=[T3])
            em.V(lambda e: e.tensor_tensor(out=tmpv(T4), in0=xv(PSB, 0), in1=tv(so), op=ALU.mult), r=[PSB, T], w=[T4])
            em.V(lambda e: e.tensor_tensor(out=xv(PO_, 1), in0=tmpv(T3), in1=tmpv(T4), op=ALU.add), r=[T3, T4], w=[PO_])
        for (c0, nh, d, o0) in NRM_GROUPS:
            n = nh * d
            em.A(lambda e: act(e, SQ[:, 0:n], PO_[:, c0:c0 + n], AF.Square), r=[PO_], w=[SQ])
            em.V(lambda e: e.tensor_reduce(out=NR[:, o0:o0 + nh], in_=SQ[:, 0:n].rearrange("p (h d) -> p h d", d=d), axis=AX.X, op=ALU.add), r=[SQ], w=[NR])
        if b == 0:
            em.V(lambda e: e.tensor_copy(out=RM[:, :], in_=NR[:, :]), r=[NR], w=[RM])
        else:
            em.V(lambda e: e.tensor_tensor(out=RM[:, :], in0=RM[:, :], in1=NR[:, :], op=ALU.max), r=[RM, NR], w=[RM])
        em.dma(C.P.t[b * 128:(b + 1) * 128, :], PO_[:, :], r=[PO_], w=[C.P])
        j = 0
        bi = 0
        while j < 14:
            nn = min(4, 14 - j)
            ps = PS[5 + bi % 2]
            bi += 1
            for jj in range(nn):
                c0 = XT_BLOCKS[j + jj]
                em.P(lambda e: e.transpose(out=ps[:, jj * 128:(jj + 1) * 128], in_=PO_[:, c0:c0 + 128], identity=C.IDN[:, :]), r=[PO_, C.IDN], w=[ps])
            em.A(lambda e: act(e, XTo_[:, j:j + nn, :], ps[:, 0:nn * 128].rearrange("p (a b) -> p a b", b=128), AF.Copy), r=[ps], w=[XTo_])
            j += nn
        em.dma(xt4[:, :, 0, b * 128:(b + 1) * 128], XTo_[0:64, :, :], r=[XTo_], w=[C.XT])
        em.dma(xt4[:, :, 1, b * 128:(b + 1) * 128], XTo_[64:128, :, :], r=[XTo_], w=[C.XT])
        for (c0, nh, h0) in V_GROUPS:
            em.G(lambda e: e.tensor_copy(out=VPo_[:, h0:h0 + nh, 0:64], in_=PO_[:, c0:c0 + nh * 64].rearrange("p (h d) -> p h d", d=64)), r=[PO_], w=[VPo_])
        em.dma(C.VPs.t[b * 128:(b + 1) * 128, :, :], VPo_[:, :, :], r=[VPo_], w=[C.VPs])
    em.P(lambda e: e.transpose(out=PS[0][0:28, 0:128], in_=RM[:, 0:28], identity=C.IDN[:, :]), r=[RM, C.IDN], w=[PS[0]])
    em.V(lambda e: e.tensor_reduce(out=RX[:, 0:1], in_=PS[0][0:28, 0:128], axis=AX.X, op=ALU.max), r=[PS[0]], w=[RX])
    em.P(lambda e: e.transpose(out=PS[1][0:1, 0:28], in_=RX[0:28, 0:1], identity=C.IDN[0:28, 0:28]), r=[RX, C.IDN], w=[PS[1]])
    em.V(lambda e: e.tensor_copy(out=RR[:, :], in_=PS[1][0:1, 0:28]), r=[PS[1]], w=[RR])
    em.P(lambda e: e.matmul(PS[2][:, 0:28], lhsT=C.ONES[0:1, 0:128], rhs=RR[0:1, 0:28], start=True, stop=True), r=[C.ONES, RR], w=[PS[2]])
    em.V(lambda e: e.tensor_copy(out=C.RMB[:, :], in_=PS[2][:, 0:28]), r=[PS[2]], w=[C.RMB])
    em.end_stage()


def stage_attn(C, l, kind, p=0):
    em = C.em
    em.begin_stage()
    PS = C.PS
    Lx, NBx, S = C.Lx, C.NBx, C.S
    need_ctx = l < C.depth - 1
    dh = 32 if kind == "B" else 64
    scale = dh ** -0.5
    nqf = 2 if kind == "B" else 4
    if kind == "A":
        qh0, kh0, vh0, qn0, kn0, OT, oh0 = 0, 4, 0, 0, 4, C.OTA, 0
    elif kind == "B":
        qh0, kh0, vh0, qn0, kn0, OT, oh0 = 6 + 2 * p, 10 + 2 * p, 2 + 2 * p, 6 + 4 * p, 14 + 4 * p, C.OTB, 2 * p
    else:
        qh0, kh0, vh0, qn0, kn0, OT, oh0 = 14, 18, 6, 22, 26, C.OTC, 0
    nout = nqf
    lam_init = 0.8 - 0.6 * math.exp(-0.3 * l)
    GQ = 512

    KB = em.sb("KB", [64, 2, Lx], BF16)
    VB = em.sb("VB", [128, NBx, 2, 65], BF16)
    STG = [em.sb("STG%d" % i, [128, 2080]) for i in range(2)]
    NEGM = em.sb("NEGM", [128, 4])
    QS = [em.sb("QS%d" % i, [64, nqf, GQ]) for i in range(2)]
    QB = [em.sb("QB%d" % i, [64, nqf, GQ], BF16) for i in range(2)]
    PTl = [em.sb("PT%d" % i, [128, 512], BF16) for i in range(3)]
    OU = [em.sb("OU%d" % i, [65, 512]) for i in range(4)]
    RZ = em.sb("RZ", [65, 512])
    OUT = [em.sb("OUT%d" % i, [64, nout, GQ]) for i in range(2)]
    NRM = [em.sb("NRM%d" % i, [64, 512]) for i in range(2)]
    SQ = em.sb("SQ", [64, 512])
    SPS = [PS[0], PS[1], PS[2]]
    OPS = [PS[3], PS[4], PS[5]]
    BPS = [PS[6], PS[7]]
    ONES = C.ONES

    if kind == "B":
        em.V(lambda e: e.tensor_tensor(out=NEGM[:, :], in0=C.RMB[:, qn0:qn0 + 4], in1=C.RMB[:, kn0:kn0 + 4], op=ALU.mult), r=[C.RMB], w=[NEGM])
    else:
        em.V(lambda e: e.tensor_tensor(out=NEGM[:, :].rearrange("p (a b) -> p a b", b=2), in0=C.RMB[:, qn0:qn0 + 4].rearrange("p (a b) -> p a b", b=2),
                                       in1=C.RMB[:, kn0:kn0 + 2].unsqueeze(2).to_broadcast([128, 2, 2]), op=ALU.mult), r=[C.RMB], w=[NEGM])
    em.A(lambda e: act(e, NEGM[:, :], NEGM[:, :], AF.Sqrt), r=[NEGM], w=[NEGM])
    em.V(lambda e: e.tensor_scalar(out=NEGM[:, :], in0=NEGM[:, :], scalar1=-scale, scalar2=None, op0=ALU.mult), r=[NEGM], w=[NEGM])
    if kind == "C":
        SK = em.sb("SK", [128, 4])
        ES = em.sb("ES", [128, 4])
        MKB = em.sb("MKB", [128, 6, 512], BF16)
        load_bcast_row(em, SK, C.win_sink.t[l, :], 4)
        em.V(lambda e: e.tensor_tensor(out=ES[:, :], in0=SK[:, :], in1=NEGM[:, :], op=ALU.add), r=[SK, NEGM], w=[ES])
        em.A(lambda e: act(e, ES[:, :], ES[:, :], AF.Exp), r=[ES], w=[ES])
        for t in range(6):
            stg = STG[t % 2]
            em.dma(stg[:, 0:512], C.mk.t[t, :, :], r=[], w=[stg])
            em.V(lambda e: e.tensor_copy(out=MKB[:, t, :], in_=stg[:, 0:512]), r=[stg], w=[MKB])
    if kind == "B":
        LP = em.sb("LP", [1, 128])
        LS = em.sb("LS", [1, 4])
        NEGLAM = em.sb("NEGLAM", [64, 1])
        SUBL = em.sb("SUBL", [64, 1])
        em.dma(LP[:, :], C.diff_lambda.t[l, :].partition_broadcast(1), r=[], w=[LP])
        lpv = LP[0:1, :].rearrange("p (a b d) -> p a b d", a=2, b=2)
        em.V(lambda e: e.tensor_tensor(out=lpv[:, :, 0, :], in0=lpv[:, :, 0, :], in1=lpv[:, :, 1, :], op=ALU.mult), r=[LP], w=[LP])
        em.V(lambda e: e.tensor_reduce(out=LS[:, 0:2], in_=lpv[:, :, 0, :], axis=AX.X, op=ALU.add), r=[LP], w=[LS])
        em.A(lambda e: act(e, LS[:, 0:2], LS[:, 0:2], AF.Exp), r=[LS], w=[LS])
        em.V(lambda e: e.tensor_tensor(out=LS[:, 2:3], in0=LS[:, 1:2], in1=LS[:, 0:1], op=ALU.subtract), r=[LS], w=[LS])
        em.V(lambda e: e.tensor_scalar(out=LS[:, 2:3], in0=LS[:, 2:3], scalar1=-float(lam_init), scalar2=None, op0=ALU.add), r=[LS], w=[LS])
        em.P(lambda e: e.matmul(BPS[1][0:64, 0:1], lhsT=ONES[0:1, 0:64], rhs=LS[0:1, 2:3], start=True, stop=True), r=[ONES, LS], w=[BPS[1]])
        em.V(lambda e: e.tensor_copy(out=NEGLAM[:, :], in_=BPS[1][0:64, 0:1]), r=[BPS[1]], w=[NEGLAM])
        em.dma(SUBL[:, :], C.diff_subln.t[l, :].rearrange("(p o) -> p o", o=1), r=[], w=[SUBL])
        em.V(lambda e: e.tensor_scalar(out=SUBL[:, :], in0=SUBL[:, :], scalar1=1.0 - float(lam_init), scalar2=None, op0=ALU.mult), r=[SUBL], w=[SUBL])

    i = 0
    for f in range(2):
        c = 0
        while c < Lx:
            cw = min(2048, Lx - c)
            stg = STG[i % 2]
            em.dma(stg[0:64, 0:cw], C.XT.t[:, kh0 + f, c:c + cw], r=[], w=[stg])
            if i % 2 == 0:
                em.A(lambda e: act(e, KB[:, f, c:c + cw], stg[0:64, 0:cw], AF.Copy), r=[stg], w=[KB])
            else:
                em.V(lambda e: e.tensor_copy(out=KB[:, f, c:c + cw], in_=stg[0:64, 0:cw]), r=[stg], w=[KB])
            c += cw
            i += 1
    t0 = 0
    vpv = C.VPs.t[:, vh0:vh0 + 2, :].rearrange("(t p) v c -> p t v c", p=128)
    while t0 < NBx:
        tn = min(16, NBx - t0)
        stg = STG[i % 2]
        sv = stg[:, 0:tn * 130].rearrange("p (t v c) -> p t v c", v=2, c=65)
        em.dma(sv, vpv[:, t0:t0 + tn, :, :], r=[], w=[stg])
        if i % 2 == 0:
            em.A(lambda e: act(e, VB[:, t0:t0 + tn, :, :], sv, AF.Copy), r=[stg], w=[VB])
        else:
            em.V(lambda e: e.tensor_copy(out=VB[:, t0:t0 + tn, :, :], in_=sv), r=[stg], w=[VB])
        t0 += tn
        i += 1

    groups = []
    for g in range(S // GQ):
        if kind == "C":
            tl = [(0, None), (1, None)]
            for tp in range(6):
                blk = 4 * g - 1 + tp
                if 0 <= blk < S // 128:
                    tl.append((2 + blk, tp))
        else:
            tl = [(t, None) for t in range(NBx)]
        groups.append((NCTX + g * GQ, GQ, tl))
    if need_ctx:
        groups.append((0, NCTX, [(0, None), (1, None)]))

    cnt = {"s": 0, "p": 0, "o": 0, "b": 0}
    for gi, (q0, gq, tl) in enumerate(groups):
        qs, qb, out_t = QS[gi % 2], QB[gi % 2], OUT[gi % 2]
        em.dma(qs[:, :, 0:gq], C.XT.t[:, qh0:qh0 + nqf, q0:q0 + gq], r=[], w=[qs])
        em.V(lambda e: e.tensor_copy(out=qb[:, :, 0:gq], in_=qs[:, :, 0:gq]), r=[qs], w=[qb])
        for u in range(4):
            if kind == "B":
                r0, qf, kf = (u % 2) * 32, u // 2, u // 2
            else:
                r0, qf, kf = 0, u, u // 2
            r1 = r0 + dh

            def emit_s(ti):
                sps = SPS[cnt["s"] % 3]
                cnt["s"] += 1
                t = tl[ti][0]
                em.P(lambda e: e.matmul(sps[:, 0:gq], lhsT=KB[r0:r1, kf, t * 128:(t + 1) * 128], rhs=qb[r0:r1, qf, 0:gq], start=True, stop=True),
                     r=[KB, qb], w=[sps])
                return sps

            ops = OPS[cnt["o"] % 3]
            cnt["o"] += 1
            nxt = emit_s(0)
            for ti in range(len(tl)):
                sps = nxt
                if ti + 1 < len(tl):
                    nxt = emit_s(ti + 1)
                t, mi = tl[ti]
                pt = PTl[cnt["p"] % 3]
                cnt["p"] += 1
                em.A(lambda e: act(e, pt[:, 0:gq], sps[:, 0:gq], AF.Exp, scale=scale, bias=NEGM[:, u:u + 1]), r=[sps, NEGM], w=[pt])
                if mi is not None:
                    em.V(lambda e: e.tensor_tensor(out=pt[:, 0:gq], in0=pt[:, 0:gq], in1=MKB[:, mi, 0:gq], op=ALU.mult), r=[pt, MKB], w=[pt])
                em.P(lambda e: e.matmul(ops[0:65, 0:gq], lhsT=VB[:, t, kf, :], rhs=pt[:, 0:gq], start=(ti == 0), stop=(ti == len(tl) - 1)),
                     r=[VB, pt], w=[ops])
            ou = OU[u]
            em.V(lambda e: e.tensor_copy(out=ou[0:65, 0:gq], in_=ops[0:65, 0:gq]), r=[ops], w=[ou])

        def normalize(u, dst_ap, dst_tl):
            ou = OU[u]
            if kind == "C":
                em.V(lambda e: e.tensor_scalar(out=RZ[64:65, 0:gq], in0=ou[64:65, 0:gq], scalar1=ES[64:65, u:u + 1], scalar2=None, op0=ALU.add),
                     r=[ou, ES], w=[RZ])
                em.V(lambda e: e.reciprocal(out=RZ[64:65, 0:gq], in_=RZ[64:65, 0:gq]), r=[RZ], w=[RZ])
            else:
                em.V(lambda e: e.reciprocal(out=RZ[64:65, 0:gq], in_=ou[64:65, 0:gq]), r=[ou], w=[RZ])
            bps = BPS[cnt["b"] % 2]
            cnt["b"] += 1
            em.P(lambda e: e.matmul(bps[0:64, 0:gq], lhsT=ONES[64:65, 0:64], rhs=RZ[64:65, 0:gq], start=True, stop=True), r=[ONES, RZ], w=[bps])
            em.V(lambda e: e.tensor_tensor(out=dst_ap, in0=ou[0:64, 0:gq], in1=bps[0:64, 0:gq], op=ALU.mult), r=[ou, bps], w=[dst_tl])

        if kind != "B":
            for u in range(4):
                normalize(u, out_t[:, u, 0:gq], out_t)
        else:
            for hl in range(2):
                normalize(2 * hl, NRM[0][:, 0:gq], NRM[0])
                normalize(2 * hl + 1, NRM[1][:, 0:gq], NRM[1])
                em.V(lambda e: e.scalar_tensor_tensor(out=NRM[0][:, 0:gq], in0=NRM[1][:, 0:gq], scalar=NEGLAM[:, 0:1], in1=NRM[0][:, 0:gq],
                                                      op0=ALU.mult, op1=ALU.add), r=[NRM[0], NRM[1], NEGLAM], w=[NRM[0]])
                em.A(lambda e: act(e, SQ[:, 0:gq], NRM[0][:, 0:gq], AF.Square), r=[NRM[0]], w=[SQ])
                bps = BPS[cnt["b"] % 2]
                cnt["b"] += 1
                em.P(lambda e: e.matmul(bps[0:64, 0:gq], lhsT=ONES[0:64, 0:64], rhs=SQ[:, 0:gq], start=True, stop=True), r=[ONES, SQ], w=[bps])
                em.V(lambda e: e.tensor_scalar(out=SQ[:, 0:gq], in0=bps[0:64, 0:gq], scalar1=1.0 / 64, scalar2=RMS_EPS, op0=ALU.mult, op1=ALU.add),
                     r=[bps], w=[SQ])
                em.A(lambda e: act(e, SQ[:, 0:gq], SQ[:, 0:gq], AF.Sqrt), r=[SQ], w=[SQ])
                em.V(lambda e: e.reciprocal(out=SQ[:, 0:gq], in_=SQ[:, 0:gq]), r=[SQ], w=[SQ])
                em.V(lambda e: e.tensor_tensor(out=NRM[0][:, 0:gq], in0=NRM[0][:, 0:gq], in1=SQ[:, 0:gq], op=ALU.mult), r=[NRM[0], SQ], w=[NRM[0]])
                em.V(lambda e: e.tensor_scalar(out=out_t[:, hl, 0:gq], in0=NRM[0][:, 0:gq], scalar1=SUBL[:, 0:1], scalar2=None, op0=ALU.mult),
                     r=[NRM[0], SUBL], w=[out_t])
        em.dma(OT.t[:, oh0:oh0 + nout, q0:q0 + gq], out_t[:, :, 0:gq], r=[out_t], w=[OT])
    em.end_stage()


def stage_ret(C, l, f):
    em = C.em
    em.begin_stage()
    PS = C.PS
    NBx = C.NBx
    RC = em.sb("RC", [128, 5, 128])
    PIDX = em.sb("PIDX", [128, 2])
    DC = em.sb("DC", [128, 8])
    LG = em.sb("LG", [128, 8])
    NLG = em.sb("NLG", [128, 8])
    DT = em.sb("DT", [128, 4, 128])
    XI = em.sb("XI", [64, 4, 128])
    ZE = em.sb("ZE", [128, 4])
    GC = em.sb("GC", [128, 4])
    ST = em.sb("ST", [64, 4, 64])
    QTc = [em.sb("QTc%d" % i, [64, 4, 128]) for i in range(2)]
    KTc = [em.sb("KTc%d" % i, [64, 4, 128]) for i in range(2)]
    KV = [em.sb("KV%d" % i, [128, 768]) for i in range(2)]
    OFc = [em.sb("OFc%d" % i, [128, 256]) for i in range(2)]
    QX = em.sb("QX", [64, 4, 128])
    KZ = em.sb("KZ", [128, 4, 64])
    INM = em.sb("INM", [128, 512])
    OO = [em.sb("OO%d" % i, [128, 256]) for i in range(2)]
    XN = em.sb("XN", [128, 256])
    SG = em.sb("SG", [128, 256])
    BST = em.sb("BST", [128, 24])
    MV = em.sb("MV", [128, 8])
    RS = em.sb("RS", [128, 4])
    N0 = em.sb("N0", [128, 256])
    N1 = em.sb("N1", [128, 256])

    em.dma(RC[:, :, :], C.rc.t[:, :, :], r=[], w=[RC])
    em.dma(PIDX[:, :], C.pidx.t[:, :], r=[], w=[PIDX])
    load_bcast_row(em, DC, C.ret_decay.t[l, :], 8)
    load_bcast_row(em, N0, C.ret_norm.t[l, 0, :], 256)
    load_bcast_row(em, N1, C.ret_norm.t[l, 1, :], 256)
    em.A(lambda e: act(e, LG[:, :], DC[:, :], AF.Exp, scale=-1.0), r=[DC], w=[LG])
    em.V(lambda e: e.tensor_scalar(out=LG[:, :], in0=LG[:, :], scalar1=1.0, scalar2=None, op0=ALU.add), r=[LG], w=[LG])
    em.A(lambda e: act(e, NLG[:, :], LG[:, :], AF.Ln), r=[LG], w=[NLG])
    em.V(lambda e: e.tensor_scalar(out=LG[:, :], in0=NLG[:, :], scalar1=-1.0, scalar2=None, op0=ALU.mult), r=[NLG], w=[LG])
    for h in range(4):
        col = f * 4 + h
        scl = LG if f == 0 else NLG
        em.A(lambda e: act(e, DT[:, h, :], RC[:, 0, :], AF.Exp, scale=scl[:, col:col + 1]), r=[RC, scl], w=[DT])
        em.V(lambda e: e.scalar_tensor_tensor(out=DT[:, h, :], in0=DT[:, h, :], scalar=0.125, in1=RC[:, 1 + f, :], op0=ALU.mult, op1=ALU.mult),
             r=[DT, RC], w=[DT])
        em.A(lambda e: act(e, XI[:, h, :], RC[0:64, 3 + f, :], AF.Exp, scale=LG[0:64, col:col + 1]), r=[RC, LG], w=[XI])
        em.A(lambda e: act(e, ZE[:, h:h + 1], PIDX[:, f:f + 1], AF.Exp, scale=LG[:, col:col + 1]), r=[PIDX, LG], w=[ZE])
    em.V(lambda e: e.tensor_scalar(out=ZE[:, :], in0=ZE[:, :], scalar1=0.125, scalar2=None, op0=ALU.mult), r=[ZE], w=[ZE])
    em.A(lambda e: act(e, GC[:, :], LG[:, f * 4:f * 4 + 4], AF.Exp, scale=128.0), r=[LG], w=[GC])
    em.V(lambda e: e.memset(ST[:, :, :], 0.0), w=[ST])

    order = list(range(NBx)) if f == 0 else [1, 0] + list(range(NBx - 1, 1, -1))
    for it, c in enumerate(order):
        qt_, kt_, kv, ofc, oo = QTc[it % 2], KTc[it % 2], KV[it % 2], OFc[it % 2], OO[it % 2]
        cs = slice(c * 128, (c + 1) * 128)
        em.dma(qt_[:, :, :], C.XT.t[:, 20:24, cs], r=[], w=[qt_])
        em.dma(kt_[:, :, :], C.XT.t[:, 24:28, cs], r=[], w=[kt_])
        em.dma(kv[:, :], C.P.t[cs, 2048:2816], r=[], w=[kv])
        if f == 1:
            em.dma(ofc[:, :], C.ODF.t[cs, :], r=[], w=[ofc])
        em.V(lambda e: e.tensor_tensor(out=QX[:, :, :], in0=qt_[:, :, :], in1=XI[:, :, :], op=ALU.mult), r=[qt_, XI], w=[QX])
        em.G(lambda e: e.tensor_tensor(out=KZ[:, :, :], in0=kv[:, 0:256].rearrange("p (h d) -> p h d", d=64),
                                       in1=ZE[:, 0:4].unsqueeze(2).to_broadcast([128, 4, 64]), op=ALU.mult), r=[kv, ZE], w=[KZ])
        for h in range(4):
            em.P(lambda e: e.matmul(PS[0][:, h * 128:(h + 1) * 128], lhsT=kt_[:, h, :], rhs=qt_[:, h, :], start=True, stop=True), r=[kt_, qt_], w=[PS[0]])
        em.V(lambda e: e.tensor_tensor(out=INM[:, :], in0=PS[0][:, :], in1=DT[:, :, :].rearrange("p h i -> p (h i)"), op=ALU.mult), r=[PS[0], DT], w=[INM])
        for h in range(4):
            em.P(lambda e: e.matmul(PS[1][:, h * 64:(h + 1) * 64], lhsT=INM[:, h * 128:(h + 1) * 128], rhs=kv[:, 256 + h * 64:256 + (h + 1) * 64],
                                    start=True, stop=False), r=[INM, kv], w=[PS[1]])
            em.P(lambda e: e.matmul(PS[1][:, h * 64:(h + 1) * 64], lhsT=QX[:, h, :], rhs=ST[:, h, :], start=False, stop=True), r=[QX, ST], w=[PS[1]])
        for h in range(4):
            em.P(lambda e: e.matmul(PS[2][0:64, h * 64:(h + 1) * 64], lhsT=KZ[:, h, :], rhs=kv[:, 256 + h * 64:256 + (h + 1) * 64], start=True, stop=True),
                 r=[KZ, kv], w=[PS[2]])
        em.V(lambda e: e.tensor_tensor(out=ST[:, :, :], in0=ST[:, :, :], in1=GC[0:64, 0:4].unsqueeze(2).to_broadcast([64, 4, 64]), op=ALU.mult),
             r=[ST, GC], w=[ST])
        em.V(lambda e: e.tensor_tensor(out=ST[:, :, :].rearrange("p h d -> p (h d)"), in0=ST[:, :, :].rearrange("p h d -> p (h d)"), in1=PS[2][0:64, 0:256], op=ALU.add),
             r=[ST, PS[2]], w=[ST])
        if f == 0:
            em.A(lambda e: act(e, oo[:, :], PS[1][:, 0:256], AF.Copy), r=[PS[1]], w=[oo])
            em.dma(C.ODF.t[cs, :], oo[:, :], r=[oo], w=[C.ODF])
        else:
            em.V(lambda e: e.tensor_tensor(out=oo[:, :], in0=PS[1][:, 0:256], in1=ofc[:, :], op=ALU.add), r=[PS[1], ofc], w=[oo])
            for h in range(4):
                em.V(lambda e: e.bn_stats(out=BST[:, h * 6:(h + 1) * 6], in_=oo[:, h * 64:(h + 1) * 64]), r=[oo], w=[BST])
            for h in range(4):
                em.V(lambda e: e.bn_aggr(out=MV[:, 2 * h:2 * h + 2], in_=BST[:, h * 6:(h + 1) * 6]), r=[BST], w=[MV])
            em.V(lambda e: e.tensor_scalar(out=RS[:, 0:4], in0=MV[:, :].rearrange("p (h t) -> p h t", t=2)[:, :, 1], scalar1=LN_EPS, scalar2=None, op0=ALU.add),
                 r=[MV], w=[RS])
            em.A(lambda e: act(e, RS[:, :], RS[:, :], AF.Sqrt), r=[RS], w=[RS])
            em.V(lambda e: e.reciprocal(out=RS[:, :], in_=RS[:, :]), r=[RS], w=[RS])
            for h in range(4):
                em.V(lambda e: e.tensor_scalar(out=XN[:, h * 64:(h + 1) * 64], in0=oo[:, h * 64:(h + 1) * 64], scalar1=MV[:, 2 * h:2 * h + 1],
                                               scalar2=RS[:, h:h + 1], op0=ALU.subtract, op1=ALU.mult), r=[oo, MV, RS], w=[XN])
            em.G(lambda e: e.tensor_tensor(out=XN[:, :], in0=XN[:, :], in1=N0[:, :], op=ALU.mult), r=[XN, N0], w=[XN])
            em.G(lambda e: e.tensor_tensor(out=XN[:, :], in0=XN[:, :], in1=N1[:, :], op=ALU.add), r=[XN, N1], w=[XN])
            em.A(lambda e: act(e, SG[:, :], kv[:, 512:768], AF.Silu), r=[kv], w=[SG])
            em.V(lambda e: e.tensor_tensor(out=oo[:, :], in0=XN[:, :], in1=SG[:, :], op=ALU.mult), r=[XN, SG], w=[oo])
            em.dma(C.ODO.t[cs, :], oo[:, :], r=[oo], w=[C.ODO])
    em.end_stage()


def stage_merge(C, l):
    em = C.em
    em.begin_stage()
    PS = C.PS
    need_ctx = l < C.depth - 1
    Wg = em.sb("Wg", [128, 8, 4096], BF16)
    Wb = em.sb("Wb", [128, 8, D], BF16)
    Wo = em.sb("Wo", [128, 8, D], BF16)
    STG = [em.sb("STG%d" % i, [128, 1024]) for i in range(2)]
    MR = [[em.sb("MR%d_%d" % (ty, i), [128, D]) for i in range(3)] for ty in range(2)]
    LNG = em.sb("LNG", [128, D])
    LNB = em.sb("LNB", [128, D])
    HB = [em.sb("HB%d" % i, [128, D]) for i in range(2)]
    OTt = [em.sb("OTt%d" % i, [128, 6, 128]) for i in range(2)]
    ODt = [em.sb("ODt%d" % i, [128, 256]) for i in range(2)]
    OTb = em.sb("OTb", [128, 8, 128], BF16)
    TMP = em.sb("TMP", [128, D])
    U = em.sb("U", [128, D])
    UT = em.sb("UT", [128, 8, 128], BF16)
    GS = [em.sb("GS%d" % i, [128, 512]) for i in range(2)]
    TH = em.sb("TH", [128, 512])
    M = em.sb("M", [128, D])
    MT = em.sb("MT", [128, 8, 128], BF16)
    R = em.sb("R", [128, D])
    HO = [em.sb("HO%d" % i, [128, D]) for i in range(2)]
    BST = em.sb("BST", [128, 12])
    MV = em.sb("MV", [128, 2])
    RS = em.sb("RS", [128, 1])

    load_mod_rows(C, l, [0, 1, 2], MR, plus1=(1,))
    load_bcast_row(em, LNG, C.ln_attn.t[l, 0, :], D)
    load_bcast_row(em, LNB, C.ln_attn.t[l, 1, :], D)
    load_weight_bf16(em, Wg, C.w_in.t[l], 8, 4096, STG, col0=MIX)
    load_weight_bf16(em, Wb, C.w_branch.t[l], 8, D, STG)
    load_weight_bf16(em, Wo, C.w_out.t[l], 8, D, STG)

    gi = 0
    for b in range(0 if need_ctx else 2, C.NBx):
        ty = 1 if b < 2 else 0
        cs = slice(b * 128, (b + 1) * 128)
        H, ott, odt, ho = HB[b % 2], OTt[b % 2], ODt[b % 2], HO[b % 2]
        em.dma(H[:, :], hrows(C, l, b), r=[], w=[H])
        for bi, OT in enumerate((C.OTA, C.OTB, C.OTC)):
            otv = OT.t.rearrange("p (j e) l -> p j e l", e=2)
            em.dma(ott[0:64, 2 * bi:2 * bi + 2, :], otv[:, :, 0, cs], r=[], w=[ott])
            em.dma(ott[64:128, 2 * bi:2 * bi + 2, :], otv[:, :, 1, cs], r=[], w=[ott])
        em.dma(odt[:, :], C.ODO.t[cs, :], r=[], w=[odt])
        em.A(lambda e: act(e, OTb[:, 0:6, :], ott[:, :, :], AF.Copy), r=[ott], w=[OTb])
        for j in range(2):
            em.P(lambda e: e.transpose(out=PS[0][:, j * 128:(j + 1) * 128], in_=odt[:, j * 128:(j + 1) * 128], identity=C.IDN[:, :]), r=[odt, C.IDN], w=[PS[0]])
        em.V(lambda e: e.tensor_copy(out=OTb[:, 6:8, :], in_=PS[0][:, 0:256].rearrange("p (a b) -> p a b", b=128)), r=[PS[0]], w=[OTb])
        emit_modulate(em, U, H, MR[ty][1], MR[ty][0], TMP)
        emit_transpose8(em, UT, U, C.IDN, PS[0], PS[1])
        for c in range(2):
            for i in range(4):
                pg, pb, gs = PS[2 + gi % 2], PS[4 + gi % 2], GS[gi % 2]
                gi += 1
                col = i * 1024 + c * 512
                for k in range(8):
                    em.P(lambda e: e.matmul(pg[:, :], lhsT=UT[:, k, :], rhs=Wg[:, k, col:col + 512], start=(k == 0), stop=(k == 7)), r=[UT, Wg], w=[pg])
                em.A(lambda e: act(e, gs[:, :], pg[:, :], AF.Sigmoid), r=[pg], w=[gs])
                for kk in range(2):
                    em.P(lambda e: e.matmul(pb[:, :], lhsT=OTb[:, 2 * i + kk, :], rhs=Wb[:, 2 * i + kk, c * 512:(c + 1) * 512], start=(kk == 0), stop=(kk == 1)),
                         r=[OTb, Wb], w=[pb])
                if i == 0:
                    em.V(lambda e: e.tensor_tensor(out=M[:, c * 512:(c + 1) * 512], in0=gs[:, :], in1=pb[:, :], op=ALU.mult), r=[gs, pb], w=[M])
                else:
                    em.V(lambda e: e.tensor_tensor(out=TH[:, :], in0=gs[:, :], in1=pb[:, :], op=ALU.mult), r=[gs, pb], w=[TH])
                    em.G(lambda e: e.tensor_tensor(out=M[:, c * 512:(c + 1) * 512], in0=M[:, c * 512:(c + 1) * 512], in1=TH[:, :], op=ALU.add), r=[M, TH], w=[M])
        emit_transpose8(em, MT, M, C.IDN, PS[0], PS[1])
        for c in range(2):
            py = PS[6 + c]
            for k in range(8):
                em.P(lambda e: e.matmul(py[:, :], lhsT=MT[:, k, :], rhs=Wo[:, k, c * 512:(c + 1) * 512], start=(k == 0), stop=(k == 7)), r=[MT, Wo], w=[py])
            em.V(lambda e: e.tensor_tensor(out=R[:, c * 512:(c + 1) * 512], in0=py[:, :], in1=MR[ty][2][:, c * 512:(c + 1) * 512], op=ALU.mult), r=[py, MR[ty][2]], w=[R])
        em.V(lambda e: e.scalar_tensor_tensor(out=R[:, :], in0=H[:, :], scalar=float(ALPHA), in1=R[:, :], op0=ALU.mult, op1=ALU.add), r=[H, R], w=[R])
        emit_layernorm(em, ho, R, LNG, LNB, BST, MV, RS, TMP)
        em.dma(C.H1.t[cs, :], ho[:, :], r=[ho], w=[C.H1])
    em.end_stage()


def stage_cvt(C):
    em = C.em
    em.begin_stage()
    SF = [em.sb("SF%d" % i, [128, 4096]) for i in range(2)]
    SB = [em.sb("SB%d" % i, [128, 4096], BF16) for i in range(3)]
    it = 0
    for l in range(len(C.peer_u)):
        for (src, dst) in ((C.peer_u[l], C.TUb[l]), (C.peer_v[l], C.TVb[l])):
            sv = src.t.rearrange("(t p r) d -> t p (r d)", p=128, r=4)
            dv = dst.t.rearrange("(t p r) d -> t p (r d)", p=128, r=4)
            for t in range(32):
                sf, sbb = SF[it % 2], SB[it % 3]
                em.dma(sf[:, :], sv[t], r=[], w=[sf])
                if it % 3 == 0:
                    em.A(lambda e: act(e, sbb[:, :], sf[:, :], AF.Copy), r=[sf], w=[sbb])
                elif it % 3 == 1:
                    em.V(lambda e: e.tensor_copy(out=sbb[:, :], in_=sf[:, :]), r=[sf], w=[sbb])
                else:
                    em.G(lambda e: e.tensor_copy(out=sbb[:, :], in_=sf[:, :]), r=[sf], w=[sbb])
                em.dma(dv[t], sbb[:, :], r=[sbb], w=[dst])
                it += 1
    em.end_stage()


def stage_peer(C, l):
    em = C.em
    em.begin_stage()
    PS = C.PS
    need_ctx = l < C.depth - 1
    last = l == C.depth - 1
    Wq = em.sb("Wq", [128, 8, 2048])
    SKT = em.sb("SKT", [128, 16, 128])
    MR = [[em.sb("MR%d_%d" % (ty, i), [128, D]) for i in range(3)] for ty in range(2)]
    LNG = em.sb("LNG", [128, D])
    LNB = em.sb("LNB", [128, D])
    I16 = em.sb("I16", [128, 32])
    HB = [em.sb("HB%d" % i, [128, D]) for i in range(2)]
    TMP = em.sb("TMP", [128, D])
    X = em.sb("X", [128, D])
    XTt = em.sb("XTt", [128, 8, 128])
    QTt = em.sb("QTt", [128, 16, 128])
    SC = em.sb("SC", [128, 16, 128])
    SC2 = SC
    SV = em.sb("SV", [128, 16, 16])
    SI = em.sb("SI", [128, 16, 16], U32)
    SIF = em.sb("SIF", [128, 16, 16])
    CD = em.sb("CD", [128, 8, 256])
    CD2 = CD
    FV = em.sb("FV", [128, 8, 16])
    FP = em.sb("FP", [128, 8, 16], U32)
    PF = em.sb("PF", [128, 8, 16])
    PI = em.sb("PI", [128, 8, 16])
    PJ = em.sb("PJ", [128, 8, 16])
    EQ = em.sb("EQ", [128, 8, 16, 16])
    EI = em.sb("EI", [128, 8, 16])
    EJ = em.sb("EJ", [128, 8, 16])
    IDX = em.sb("IDX", [128, 128], I32)
    GT = em.sb("GT", [128, 8, 16])
    GSM = em.sb("GSM", [128, 8])
    AV = em.sb("AV", [128, 128])
    A2 = em.sb("A2", [128, 128])
    WT = em.sb("WT", [128, 128])
    JK = em.sb("JK", [128, D])
    GB = [em.sb("GB%d" % i, [128, D], BF16) for i in range(8)]
    IDNb = em.sb("IDNb", [128, 128], BF16)
    DG = [em.sb("DG%d" % i, [128, 128], BF16) for i in range(3)]
    R = em.sb("R", [128, D])
    HO = [em.sb("HO0", [128, D])] * 2
    BST = em.sb("BST", [128, 12])
    MV = em.sb("MV", [128, 2])
    RS = em.sb("RS", [128, 1])

    load_mod_rows(C, l, [3, 4, 5], MR, plus1=(4,))
    load_bcast_row(em, LNG, C.ln_ffn.t[l, 0, :], D)
    load_bcast_row(em, LNB, C.ln_ffn.t[l, 1, :], D)
    load_bcast_row(em, I16, C.iota16.t[:], 32)
    em.V(lambda e: e.tensor_copy(out=IDNb[:, :], in_=C.IDN[:, :]), r=[C.IDN], w=[IDNb])
    em.dma(Wq[:, :, :], C.peer_wq.t[l].rearrange("(k p) c -> p k c", p=128), r=[], w=[Wq])
    em.dma(SKT[:, :, :], C.skT.t[l], r=[], w=[SKT])
    if not getattr(C, "route_only", False):
        ut, vt = C.TUb[l].t[:, :], C.TVb[l].t[:, :]
    gbi = 0
    for b in range(0 if need_ctx else 2, C.NBx):
        ty = 1 if b < 2 else 0
        cs = slice(b * 128, (b + 1) * 128)
        H, ho = HB[b % 2], HO[b % 2]
        em.dma(H[:, :], C.H1.t[cs, :], r=[], w=[H])
        emit_modulate(em, X, H, MR[ty][1], MR[ty][0], TMP)
        emit_transpose8(em, XTt, X, C.IDN, PS[0], PS[1])
        for c4 in range(4):
            ps = PS[2 + c4 % 2]
            for cc in range(4):
                c = c4 * 4 + cc
                for k in range(8):
                    em.P(lambda e: e.matmul(ps[:, cc * 128:(cc + 1) * 128], lhsT=Wq[:, k, c * 128:(c + 1) * 128], rhs=XTt[:, k, :], start=(k == 0), stop=(k == 7)),
                         r=[Wq, XTt], w=[ps])
            em.A(lambda e: act(e, QTt[:, c4 * 4:c4 * 4 + 4, :], ps[:, :].rearrange("p (a b) -> p a b", b=128), AF.Copy), r=[ps], w=[QTt])
        for c4 in range(4):
            ps = PS[4 + c4 % 2]
            for cc in range(4):
                c = c4 * 4 + cc
                em.P(lambda e: e.matmul(ps[:, cc * 128:(cc + 1) * 128], lhsT=QTt[:, c, :], rhs=SKT[:, c, :], start=True, stop=True), r=[QTt, SKT], w=[ps])
            em.A(lambda e: act(e, SC[:, c4 * 4:c4 * 4 + 4, :], ps[:, :].rearrange("p (a b) -> p a b", b=128), AF.Copy), r=[ps], w=[SC])
        for c in range(16):
            em.V(lambda e: e.max(out=SV[:, c, 0:8], in_=SC[:, c, :]), r=[SC], w=[SV])
            em.V(lambda e: e.max_index(out=SI[:, c, 0:8], in_max=SV[:, c, 0:8], in_values=SC[:, c, :]), r=[SV, SC], w=[SI])
            em.V(lambda e: e.match_replace(out=SC2[:, c, :], in_to_replace=SV[:, c, 0:8], in_values=SC[:, c, :], imm_value=-1e30), r=[SV, SC], w=[SC2])
            em.V(lambda e: e.max(out=SV[:, c, 8:16], in_=SC2[:, c, :]), r=[SC2], w=[SV])
            em.V(lambda e: e.max_index(out=SI[:, c, 8:16], in_max=SV[:, c, 8:16], in_values=SC2[:, c, :]), r=[SV, SC2], w=[SI])
        em.V(lambda e: e.tensor_copy(out=SIF[:, :, :], in_=SI[:, :, :]), r=[SI], w=[SIF])
        svv = SV[:, :, :].rearrange("p (h t) k -> p h t k", t=2)
        sfv = SIF[:, :, :].rearrange("p (h t) k -> p h t k", t=2)
        cdv = CD[:, :, :].rearrange("p h (i j) -> p h i j", j=16)
        for h in range(8):
            em.V(lambda e: e.tensor_tensor(out=cdv[:, h, :, :], in0=svv[:, h, 0, :].unsqueeze(2).to_broadcast([128, 16, 16]),
                                           in1=svv[:, h, 1, :].unsqueeze(1).to_broadcast([128, 16, 16]), op=ALU.add), r=[SV], w=[CD])
        for h in range(8):
            em.V(lambda e: e.max(out=FV[:, h, 0:8], in_=CD[:, h, :]), r=[CD], w=[FV])
            em.V(lambda e: e.max_index(out=FP[:, h, 0:8], in_max=FV[:, h, 0:8], in_values=CD[:, h, :]), r=[FV, CD], w=[FP])
            em.V(lambda e: e.match_replace(out=CD2[:, h, :], in_to_replace=FV[:, h, 0:8], in_values=CD[:, h, :], imm_value=-1e30), r=[FV, CD], w=[CD2])
            em.V(lambda e: e.max(out=FV[:, h, 8:16], in_=CD2[:, h, :]), r=[CD2], w=[FV])
            em.V(lambda e: e.max_index(out=FP[:, h, 8:16], in_max=FV[:, h, 8:16], in_values=CD2[:, h, :]), r=[FV, CD2], w=[FP])
        em.V(lambda e: e.tensor_copy(out=PF[:, :, :], in_=FP[:, :, :]), r=[FP], w=[PF])
        i16b = I16[:, 16:32].unsqueeze(1).unsqueeze(1).to_broadcast([128, 8, 16, 16])
        i1b = I16[:, 0:16].unsqueeze(1).unsqueeze(1).to_broadcast([128, 8, 16, 16])
        em.V(lambda e: e.tensor_tensor(out=EQ[:, :, :, :], in0=PF[:, :, :].unsqueeze(3).to_broadcast([128, 8, 16, 16]), in1=i16b, op=ALU.is_ge), r=[PF, I16], w=[EQ])
        em.V(lambda e: e.tensor_reduce(out=PI[:, :, :], in_=EQ[:, :, :, :], axis=AX.X, op=ALU.add), r=[EQ], w=[PI])
        em.V(lambda e: e.tensor_scalar(out=PI[:, :, :], in0=PI[:, :, :], scalar1=-1.0, scalar2=None, op0=ALU.add), r=[PI], w=[PI])
        em.V(lambda e: e.scalar_tensor_tensor(out=PJ[:, :, :], in0=PI[:, :, :], scalar=-16.0, in1=PF[:, :, :], op0=ALU.mult, op1=ALU.add), r=[PI, PF], w=[PJ])
        for (PX, t, EX) in ((PI, 0, EI), (PJ, 1, EJ)):
            em.V(lambda e: e.tensor_tensor(out=EQ[:, :, :, :], in0=PX[:, :, :].unsqueeze(3).to_broadcast([128, 8, 16, 16]), in1=i1b, op=ALU.is_equal), r=[PX, I16], w=[EQ])
            em.V(lambda e: e.tensor_tensor(out=EQ[:, :, :, :], in0=EQ[:, :, :, :], in1=sfv[:, :, t, :].unsqueeze(2).to_broadcast([128, 8, 16, 16]), op=ALU.mult),
                 r=[EQ, SIF], w=[EQ])
            em.V(lambda e: e.tensor_reduce(out=EX[:, :, :], in_=EQ[:, :, :, :], axis=AX.X, op=ALU.add), r=[EQ], w=[EX])
        em.V(lambda e: e.scalar_tensor_tensor(out=EI[:, :, :], in0=EI[:, :, :], scalar=128.0, in1=EJ[:, :, :], op0=ALU.mult, op1=ALU.add), r=[EI, EJ], w=[EI])
        em.V(lambda e: e.tensor_copy(out=IDX[:, :], in_=EI[:, :, :].rearrange("p h k -> p (h k)")), r=[EI], w=[IDX])
        em.V(lambda e: e.tensor_tensor(out=GT[:, :, :], in0=FV[:, :, :], in1=FV[:, :, 0:1].to_broadcast([128, 8, 16]), op=ALU.subtract), r=[FV], w=[GT])
        em.A(lambda e: act(e, GT[:, :, :], GT[:, :, :], AF.Exp), r=[GT], w=[GT])
        em.V(lambda e: e.tensor_reduce(out=GSM[:, :], in_=GT[:, :, :], axis=AX.X, op=ALU.add), r=[GT], w=[GSM])
        em.V(lambda e: e.reciprocal(out=GSM[:, :], in_=GSM[:, :]), r=[GSM], w=[GSM])
        em.V(lambda e: e.tensor_tensor(out=GT[:, :, :], in0=GT[:, :, :], in1=GSM[:, :].unsqueeze(2).to_broadcast([128, 8, 16]), op=ALU.mult), r=[GT, GSM], w=[GT])
        if getattr(C, "route_only", False):
            for (tl_, ap_, o_, n_) in ((SV, SV[:, :, :].rearrange("p a b -> p (a b)"), 0, 256), (SIF, SIF[:, :, :].rearrange("p a b -> p (a b)"), 256, 256),
                                       (FV, FV[:, :, :].rearrange("p a b -> p (a b)"), 512, 128), (PF, PF[:, :, :].rearrange("p a b -> p (a b)"), 640, 128),
                                       (EI, EI[:, :, :].rearrange("p a b -> p (a b)"), 768, 128), (GT, GT[:, :, :].rearrange("p a b -> p (a b)"), 896, 128),
                                       (PI, PI[:, :, :].rearrange("p a b -> p (a b)"), 1024, 128), (PJ, PJ[:, :, :].rearrange("p a b -> p (a b)"), 1152, 128),
                                       (X, X[:, :], 2048, 1024)):
                em.dma(C.pdbg.t[:, o_:o_ + n_], ap_, r=[tl_], w=[C.pdbg])
            break
        for r_ in range(128):
            gb = GB[gbi % 8]
            gbi += 1
            em.dma(None, None, r=[IDX], w=[gb], q="pool",
                   fn=lambda q: q.indirect_dma_start(out=gb[:, :], out_offset=None, in_=ut, in_offset=bass.IndirectOffsetOnAxis(ap=IDX[:, r_:r_ + 1], axis=0), bounds_check=C.breg, oob_is_err=False))
            em.V(lambda e: e.scalar_tensor_tensor(out=JK[:, :], in0=gb[:, :], scalar=1.0, in1=X[:, :], op0=ALU.mult, op1=ALU.mult, accum_out=AV[:, r_:r_ + 1]),
                 r=[gb, X], w=[JK, AV])
        em.V(lambda e: e.tensor_tensor(out=A2[:, :], in0=AV[:, :], in1=AV[:, :], op=ALU.mult), r=[AV], w=[A2])
        em.V(lambda e: e.tensor_tensor(out=A2[:, :], in0=A2[:, :], in1=AV[:, :], op=ALU.mult), r=[A2, AV], w=[A2])
        em.V(lambda e: e.scalar_tensor_tensor(out=A2[:, :], in0=A2[:, :], scalar=0.044715, in1=AV[:, :], op0=ALU.mult, op1=ALU.add), r=[A2, AV], w=[A2])
        em.A(lambda e: act(e, A2[:, :], A2[:, :], AF.Tanh, scale=0.7978845608028654), r=[A2], w=[A2])
        em.V(lambda e: e.scalar_tensor_tensor(out=A2[:, :], in0=A2[:, :], scalar=1.0, in1=AV[:, :], op0=ALU.add, op1=ALU.mult), r=[A2, AV], w=[A2])
        em.V(lambda e: e.scalar_tensor_tensor(out=WT[:, :], in0=A2[:, :], scalar=0.5, in1=GT[:, :, :].rearrange("p h k -> p (h k)"), op0=ALU.mult, op1=ALU.mult),
             r=[A2, GT], w=[WT])
        for r_ in range(128):
            gb = GB[gbi % 8]
            gbi += 1
            em.dma(None, None, r=[IDX], w=[gb], q="pool",
                   fn=lambda q: q.indirect_dma_start(out=gb[:, :], out_offset=None, in_=vt, in_offset=bass.IndirectOffsetOnAxis(ap=IDX[:, r_:r_ + 1], axis=0), bounds_check=C.breg, oob_is_err=False))
            dg = DG[r_ % 3]
            em.V(lambda e: e.tensor_scalar(out=dg[:, :], in0=IDNb[:, :], scalar1=WT[:, r_:r_ + 1], scalar2=None, op0=ALU.mult), r=[IDNb, WT], w=[dg])
            for c in range(2):
                em.P(lambda e: e.matmul(PS[6 + c][:, :], lhsT=dg[:, :], rhs=gb[:, c * 512:(c + 1) * 512], start=(r_ == 0), stop=(r_ == 127)),
                     r=[dg, gb], w=[PS[6 + c]])
        for c in range(2):
            em.V(lambda e: e.tensor_tensor(out=R[:, c * 512:(c + 1) * 512], in0=PS[6 + c][:, :], in1=MR[ty][2][:, c * 512:(c + 1) * 512], op=ALU.mult),
                 r=[PS[6 + c], MR[ty][2]], w=[R])
        em.V(lambda e: e.scalar_tensor_tensor(out=R[:, :], in0=H[:, :], scalar=float(ALPHA), in1=R[:, :], op0=ALU.mult, op1=ALU.add), r=[H, R], w=[R])
        emit_layernorm(em, ho, R, LNG, LNB, BST, MV, RS, TMP)
        if last:
            em.dma(C.out.t[(b - 2) * 128:(b - 1) * 128, :], ho[:, :], r=[ho], w=[C.out])
        else:
            em.dma(C.H2.t[cs, :], ho[:, :], r=[ho], w=[C.H2])
    em.end_stage()


STAGES = ["mod", "proj", "attnA", "attnB0", "attnB1", "attnC", "retf", "retb", "merge", "peer"]


def build_all(S, depth=DEPTH, debug=False, stop=None):
    nc = bass.Bass("TRN2", target_bir_lowering=False)
    Lx = NCTX + S
    with ExitStack() as st:
        em = Em(nc, st)
        C = Ctx()
        C.em, C.S, C.Lx, C.NBx, C.depth = em, S, Lx, Lx // 128, depth
        C.x = em.dram("x", [S, D])
        C.ctx = em.dram("ctx", [NCTX, D])
        C.cT = em.dram("cT", [D, 2])
        C.w_mod = em.dram("w_mod", [DEPTH, D, 6 * D])
        C.b_mod = em.dram("b_mod", [DEPTH, 6 * D])
        C.w_in = em.dram("w_in", [DEPTH, D, 6912])
        C.gain = em.dram("gain", [DEPTH, 384])
        C.diff_lambda = em.dram("diff_lambda", [DEPTH, 128])
        C.diff_subln = em.dram("diff_subln", [DEPTH, 64])
        C.win_sink = em.dram("win_sink", [DEPTH, 4])
        C.ret_decay = em.dram("ret_decay", [DEPTH, 8])
        C.ret_norm = em.dram("ret_norm", [DEPTH, 2, 256])
        C.w_branch = em.dram("w_branch", [DEPTH, 1024, D])
        C.w_out = em.dram("w_out", [DEPTH, D, D])
        C.ln_attn = em.dram("ln_attn", [DEPTH, 2, D])
        C.ln_ffn = em.dram("ln_ffn", [DEPTH, 2, D])
        C.peer_wq = em.dram("peer_wq", [DEPTH, D, 2048])
        C.skT = em.dram("skT", [DEPTH, 128, 16, 128])
        npeer = DEPTH if stop is None else (stop[0] + 1 if stop[1] == "peer" else stop[0])
        C.peer_u = [em.dram("peer_u%d" % i, [16384, D]) for i in range(npeer)]
        C.peer_v = [em.dram("peer_v%d" % i, [16384, D]) for i in range(npeer)]
        C.TUb = [em.dram("TUb%d" % i, [16384, D], BF16, kind="Internal") for i in range(npeer)]
        C.TVb = [em.dram("TVb%d" % i, [16384, D], BF16, kind="Internal") for i in range(npeer)]
        C.ident = em.dram("ident", [128, 128])
        C.tab = em.dram("tab", [Lx, 160])
        C.mk = em.dram("mk", [6, 128, 512])
        C.rc = em.dram("rc", [128, 5, 128])
        C.pidx = em.dram("pidx", [128, 2])
        C.iota16 = em.dram("iota16", [32])
        C.out = em.dram("out", [S, D], kind="ExternalOutput")
        sk = "ExternalOutput" if debug else "Internal"
        C.mod = em.dram("mod", [DEPTH, 2, 6 * D], kind=sk)
        C.P = em.dram("P", [Lx, MIX], kind=sk)
        C.XT = em.dram("XT", [64, 28, Lx], kind=sk)
        C.VPs = em.dram("VPs", [Lx, 8, 65], kind=sk)
        C.OTA = em.dram("OTA", [64, 4, Lx], kind=sk)
        C.OTB = em.dram("OTB", [64, 4, Lx], kind=sk)
        C.OTC = em.dram("OTC", [64, 4, Lx], kind=sk)
        C.ODF = em.dram("ODF", [Lx, 256], kind=sk)
        C.ODO = em.dram("ODO", [Lx, 256], kind=sk)
        C.H1 = em.dram("H1", [Lx, D], kind=sk)
        C.H2 = em.dram("H2", [Lx, D], kind=sk)
        C.route_only = stop is not None and stop[1] == "route"
        if C.route_only:
            C.pdbg = em.dram("pdbg", [128, 4096], kind="ExternalOutput")
        C.PS = [em.ps("PS%d" % i) for i in range(8)]
        C.IDN = em.sb("IDN", [128, 128])
        C.ONES = em.sb("ONES", [128, 128])
        C.RMB = em.sb("RMB", [128, 28])
        em.dma(C.IDN[:, :], C.ident.t[:, :], r=[], w=[C.IDN])
        C.breg = nc.gpsimd.to_reg(16383)
        em.V(lambda e: e.memset(C.ONES[:, :], 1.0), w=[C.ONES])

        def run_stages():
            stage_mod(C)
            if stop == (0, "mod"):
                return
            stage_cvt(C)
            for l in range(depth):
                seq = [("proj", lambda: stage_proj(C, l)), ("attnA", lambda: stage_attn(C, l, "A")), ("attnB0", lambda: stage_attn(C, l, "B", 0)),
                       ("attnB1", lambda: stage_attn(C, l, "B", 1)), ("attnC", lambda: stage_attn(C, l, "C")), ("retf", lambda: stage_ret(C, l, 0)),
                       ("retb", lambda: stage_ret(C, l, 1)), ("merge", lambda: stage_merge(C, l)), ("route" if C.route_only else "peer", lambda: stage_peer(C, l))]
                for name, fn in seq:
                    fn()
                    if stop == (l, name):
                        return

        run_stages()
        em.finish()
        C.ninst = em.ninst
    nc._ninst = C.ninst
    nc._inputs = list(em.inputs)
    return nc


def host_tables(S):
    theta = np.float32(10000.0)
    Lx = NCTX + S
    tab = np.zeros((Lx, 160), np.float32)
    i = np.arange(S)
    row = (i // GRID_W).astype(np.float32)
    col = (i % GRID_W).astype(np.float32)

    def inv(n):
        return (theta ** (-np.arange(n, dtype=np.float32) / np.float32(n))).astype(np.float32)

    a64 = np.stack([row[:, None] * inv(16)[None], col[:, None] * inv(16)[None]], 1).astype(np.float32)
    a32 = np.stack([row[:, None] * inv(8)[None], col[:, None] * inv(8)[None]], 1).astype(np.float32)
    tab[:NCTX, 0:32] = 1.0
    tab[:NCTX, 64:80] = 1.0
    tab[NCTX:, 0:32] = np.cos(a64).reshape(S, 32)
    tab[NCTX:, 32:64] = np.sin(a64).reshape(S, 32)
    tab[NCTX:, 64:80] = np.cos(a32).reshape(S, 16)
    tab[NCTX:, 80:96] = np.sin(a32).reshape(S, 16)
    pos = np.arange(Lx, dtype=np.float32)
    ang = (pos[:, None] * inv(32)[None]).astype(np.float32)
    tab[:, 96:128] = np.cos(ang)
    tab[:, 128:160] = np.sin(ang)
    return tab


def host_consts(S):
    tab = host_tables(S)
    j = np.arange(128)[:, None]
    i = np.arange(128)[None, :]
    rc = np.zeros((128, 5, 128), np.float32)
    rc[:, 0] = i - j
    rc[:, 1] = (i >= j)
    rc[:, 2] = (j >= i)
    rc[:, 3] = np.broadcast_to(i + 1, (128, 128))
    rc[:, 4] = np.broadcast_to(128 - i, (128, 128))
    pidx = np.stack([127 - np.arange(128), np.arange(128)], 1).astype(np.float32)
    mk = np.zeros((6, 128, 512), np.float32)
    k = np.arange(128)[:, None]
    q = np.arange(128)[None, :]
    for tp in range(6):
        for qb in range(4):
            rel = (tp - 1) - qb
            if rel == -1:
                mk[tp, :, qb * 128:(qb + 1) * 128] = (k >= q)
            elif rel == 0:
                mk[tp, :, qb * 128:(qb + 1) * 128] = 1.0
            elif rel == 1:
                mk[tp, :, qb * 128:(qb + 1) * 128] = (k <= q)
    iota16 = np.concatenate([np.arange(16), 16 * np.arange(16)]).astype(np.float32)
    return {"tab": tab, "rc": rc, "pidx": pidx, "mk": mk, "iota16": iota16, "ident": np.eye(128, dtype=np.float32)}


def make_in_maps(inp, S):
    f = lambda a: np.ascontiguousarray(np.asarray(a, dtype=np.float32))
    cst = host_consts(S)
    g = np.asarray(inp["qk_gain"], np.float32)
    gain = np.stack([np.concatenate([np.tile(g[l, 0], 4), np.tile(g[l, 1], 2)]) for l in range(DEPTH)]).astype(np.float32)
    sk = np.asarray(inp["peer_subkeys"], np.float32)
    skT = np.ascontiguousarray(sk.reshape(DEPTH, 16, 128, 128).transpose(0, 3, 1, 2))
    shared = {
        "w_mod": f(inp["w_mod"]), "b_mod": f(inp["b_mod"]), "w_in": f(inp["w_in"]), "gain": gain,
        "diff_lambda": f(np.asarray(inp["diff_lambda"]).reshape(DEPTH, 128)), "diff_subln": f(inp["diff_subln"]),
        "win_sink": f(inp["win_sink"]), "ret_decay": f(np.asarray(inp["ret_decay"]).reshape(DEPTH, 8)), "ret_norm": f(inp["ret_norm"]),
        "w_branch": f(np.asarray(inp["w_branch"]).reshape(DEPTH, 1024, D)), "w_out": f(inp["w_out"]), "ln_attn": f(inp["ln_attn"]),
        "ln_ffn": f(inp["ln_ffn"]), "peer_wq": f(inp["peer_wq"]), "skT": skT,
    }
    for l in range(DEPTH):
        shared["peer_u%d" % l] = f(np.asarray(inp["peer_u"])[l])
        shared["peer_v%d" % l] = f(np.asarray(inp["peer_v"])[l])
    shared.update(cst)
    maps = []
    for b in range(2):
        m = dict(shared)
        m["x"] = f(inp["x"][b])
        m["ctx"] = f(inp["ctx"][b])
        m["cT"] = f(np.stack([np.asarray(inp["c"])[b], np.asarray(inp["c_ctx"])], 1))
        maps.append(m)
    return maps


def kernel(**inp):
    S = int(np.asarray(inp["x"]).shape[1])
    nc = build_all(S)
    maps = make_in_maps(inp, S)
    res = run_bass_kernel_spmd(nc, maps, core_ids=[0, 1])
    return np.stack([res.results[b]["out"] for b in range(2)], 0).astype(np.float32)
```
